# Optimizing a Trainium2 kernel written in Bass

```python
import jax
import jax.numpy as jnp
from jax import lax
import numpy as np

D_MODEL = 1024
BATCH = 8
SEQ = 8192
DEPTH = 4

N_A_LAYERS = DEPTH // 2
N_B_LAYERS = DEPTH - N_A_LAYERS
A_INNER = 2 * D_MODEL
A_HEADS = 4
A_HEAD_DIM = A_INNER // A_HEADS
A_CONV = 4
A_QKV_BLOCK = 4
A_CHUNK = 64
B_HEADS = 16
B_GROUPS = 2
B_HPG = B_HEADS // B_GROUPS
B_DK = 128
B_DV = 128
B_INNER = B_HEADS * B_DV
N_BRANCH = 3
CMP_BLK = 32
CMP_STRIDE = 16
CMP_HIDDEN = 256
SEL_BLK = 64
SEL_TOP = 16
WINDOW = 512
Q_BLOCK = 128
EPS = 1e-6

kernel_name = "yoco_mlstm_nsa_hybrid"


def rmsnorm(x, g):
    xf = x.astype(jnp.float32)
    y = xf * lax.rsqrt(jnp.mean(xf * xf, axis=-1, keepdims=True) + EPS)
    return (y * g.astype(jnp.float32)).astype(x.dtype)


def masked_softmax(s, mask):
    s = jnp.where(mask, s, -jnp.inf)
    m = jnp.max(s, axis=-1, keepdims=True)
    m = jnp.where(jnp.isfinite(m), m, 0.0)
    e = jnp.exp(s - m)
    return e / jnp.maximum(jnp.sum(e, axis=-1, keepdims=True), 1e-30)


def causal_depthwise_conv(x, w, b):
    taps = w.shape[0]
    y = lax.conv_general_dilated(x, w[:, None, :], window_strides=(1,),
                                 padding=[(taps - 1, 0)],
                                 dimension_numbers=("NWC", "WIO", "NWC"),
                                 feature_group_count=x.shape[-1])
    return y + b


def blockdiag(x, w):
    nb, bs, _ = w.shape
    xb = x.reshape(x.shape[:-1] + (nb, bs))
    return jnp.einsum("bsgi,gij->bsgj", xb, w).reshape(x.shape)


def mlstm_cell(q, k, v, i_pre, f_pre):
    bsz, seq, nh, dh = q.shape
    nc = seq // A_CHUNK

    def to_chunks(a):
        a = a.astype(jnp.float32).reshape((bsz, nc, A_CHUNK, nh) + a.shape[3:])
        return jnp.moveaxis(jnp.moveaxis(a, 1, 0), 3, 2)

    qc = to_chunks(q)
    kc = to_chunks(k) * (dh ** -0.5)
    vc = to_chunks(v)
    ic = to_chunks(i_pre)
    lfc = jax.nn.log_sigmoid(to_chunks(f_pre))
    causal = jnp.tril(jnp.ones((A_CHUNK, A_CHUNK), dtype=bool))

    def step(carry, xs):
        c_st, n_st, m_st = carry
        qj, kj, vj, ij, lfj = xs
        b = jnp.cumsum(lfj, axis=-1)
        log_d = jnp.where(causal, b[..., :, None] - b[..., None, :] + ij[..., None, :], -jnp.inf)
        m_inter = b + m_st[..., None]
        m_t = jnp.maximum(jnp.max(log_d, axis=-1), m_inter)
        d = jnp.exp(log_d - m_t[..., None])
        s = jnp.einsum("bhtd,bhsd->bhts", qj, kj) * d
        decay = jnp.exp(m_inter - m_t)
        num = decay[..., None] * jnp.einsum("bhtk,bhkv->bhtv", qj, c_st) + jnp.einsum("bhts,bhsv->bhtv", s, vj)
        den = decay * jnp.einsum("bhtk,bhk->bht", qj, n_st) + jnp.sum(s, axis=-1)
        h = num / jnp.maximum(jnp.abs(den), jnp.exp(-m_t))[..., None]
        b_last = b[..., -1]
        g = b_last[..., None] - b + ij
        m_new = jnp.maximum(b_last + m_st, jnp.max(g, axis=-1))
        w = jnp.exp(g - m_new[..., None])
        carry_decay = jnp.exp(b_last + m_st - m_new)
        c_new = carry_decay[..., None, None] * c_st + jnp.einsum("bhl,bhlk,bhlv->bhkv", w, kj, vj)
        n_new = carry_decay[..., None] * n_st + jnp.einsum("bhl,bhlk->bhk", w, kj)
        return (c_new, n_new, m_new), h

    init = (jnp.zeros((bsz, nh, dh, dh), jnp.float32),
            jnp.zeros((bsz, nh, dh), jnp.float32),
            jnp.zeros((bsz, nh), jnp.float32))
    _, hs = lax.scan(step, init, (qc, kc, vc, ic, lfc))
    hs = jnp.moveaxis(jnp.moveaxis(hs, 0, 1), 2, 3)
    return hs.reshape(bsz, seq, nh, dh)


def mlstm_mixer(h, w_in, conv_w, conv_b, w_q, w_k, w_v, w_gate, b_gate, head_norm, skip, w_out):
    bsz, seq, _ = h.shape
    xm, o_pre, z = jnp.split(h @ w_in, 3, axis=-1)
    xc = jax.nn.silu(causal_depthwise_conv(xm, conv_w, conv_b))
    q = blockdiag(xc, w_q)
    k = blockdiag(xc, w_k)
    v = blockdiag(xm, w_v)
    gates = (q @ w_gate[:A_INNER] + k @ w_gate[A_INNER:2 * A_INNER]
             + v @ w_gate[2 * A_INNER:] + b_gate).astype(jnp.float32)
    i_pre, f_pre = gates[..., :A_HEADS], gates[..., A_HEADS:]

    def heads(a):
        return a.reshape(bsz, seq, A_HEADS, A_HEAD_DIM)

    cell = mlstm_cell(heads(q), heads(k), heads(v), i_pre, f_pre)
    hc = jax.nn.sigmoid(heads(o_pre).astype(jnp.float32)) * cell
    mu = jnp.mean(hc, axis=-1, keepdims=True)
    var = jnp.mean(jnp.square(hc - mu), axis=-1, keepdims=True)
    hn = ((hc - mu) * lax.rsqrt(var + EPS)).reshape(bsz, seq, A_INNER) * head_norm
    y = (hn + skip * xc) * jax.nn.silu(z)
    return y.astype(h.dtype) @ w_out


def compress_blocks(a, pos, w1, b1, w2, b2):
    bsz, seq, g, d = a.shape
    r = CMP_BLK // CMP_STRIDE
    ch = a.reshape(bsz, seq // CMP_STRIDE, CMP_STRIDE, g, d)
    nc = seq // CMP_STRIDE - r + 1
    blk = jnp.concatenate([ch[:, i:i + nc] for i in range(r)], axis=2)
    blk = blk + pos[:, None, :]
    flat = jnp.moveaxis(blk, 3, 2).reshape(bsz, nc, g, CMP_BLK * d)
    return jax.nn.silu(flat @ w1 + b1) @ w2 + b2


def nsa_shared_kv(x, kv_norm, kv_w, cmp_pos, cmp_w1, cmp_b1, cmp_w2, cmp_b2):
    bsz, seq, _ = x.shape
    kv = (rmsnorm(x, kv_norm) @ kv_w).reshape(bsz, seq, 2 * N_BRANCH, B_GROUPS, B_DK)
    kc = compress_blocks(kv[:, :, 0], cmp_pos[0], cmp_w1[0], cmp_b1[0], cmp_w2[0], cmp_b2[0])
    vc = compress_blocks(kv[:, :, 1], cmp_pos[1], cmp_w1[1], cmp_b1[1], cmp_w2[1], cmp_b2[1])
    ns = seq // SEL_BLK

    def sel_blocks(a):
        return a.reshape(bsz, ns, SEL_BLK, B_GROUPS, B_DK).transpose(0, 3, 1, 2, 4)

    ks = sel_blocks(kv[:, :, 2])
    vs = sel_blocks(kv[:, :, 3])
    pad = ((0, 0), (WINDOW, 0), (0, 0), (0, 0))
    kw = jnp.pad(kv[:, :, 4], pad)
    vw = jnp.pad(kv[:, :, 5], pad)
    return (kc, vc, ks, vs, kw, vw)


def cmp_to_sel_overlap(nc, ns):
    i = jnp.arange(nc)[:, None] * CMP_STRIDE
    j = jnp.arange(ns)[None, :] * SEL_BLK
    ov = jnp.minimum(i + CMP_BLK, j + SEL_BLK) - jnp.maximum(i, j)
    return (jnp.maximum(ov, 0) / CMP_STRIDE).astype(jnp.float32)


def alibi_slopes():
    h = jnp.arange(1, B_HEADS + 1, dtype=jnp.float32)
    return (2.0 ** (-8.0 * h / B_HEADS)).reshape(B_GROUPS, B_HPG)


def nsa_mixer(h, kv, w_in, w_out):
    bsz, seq, _ = h.shape
    kc, vc, ks, vs, kw, vw = kv
    nqk = B_HEADS * B_DK
    ngl = N_BRANCH * B_HEADS
    proj = h @ w_in
    q = proj[..., :nqk].reshape(bsz, seq, B_GROUPS, B_HPG, B_DK) * (B_DK ** -0.5)
    gates = jax.nn.sigmoid(proj[..., nqk:nqk + ngl].astype(jnp.float32)).reshape(
        bsz, seq, N_BRANCH, B_GROUPS, B_HPG)
    zg = jax.nn.silu(proj[..., nqk + ngl:]).reshape(bsz, seq, N_BRANCH, B_GROUPS, B_HPG, B_DV)
    nc = kc.shape[1]
    ns = ks.shape[2]
    n_sel = min(SEL_TOP, ns)
    w_ov = cmp_to_sel_overlap(nc, ns)
    slopes = alibi_slopes()
    c_idx = jnp.arange(nc)
    c_end = c_idx * CMP_STRIDE + CMP_BLK - 1
    c_mid = (c_idx * CMP_STRIDE).astype(jnp.float32) + 0.5 * (CMP_BLK - 1)
    j_blk = jnp.arange(ns)
    b_idx = jnp.arange(bsz)[:, None, None, None]
    g_idx = jnp.arange(B_GROUPS)[None, None, :, None]
    nqb = seq // Q_BLOCK

    def to_blocks(a):
        return jnp.moveaxis(a.reshape((bsz, nqb, Q_BLOCK) + a.shape[2:]), 1, 0)

    def query_block(args):
        qb, gb, zb, t0 = args
        t = t0 + jnp.arange(Q_BLOCK)
        tf = t.astype(jnp.float32)
        s = jnp.einsum("bqghd,bngd->bqghn", qb, kc, preferred_element_type=jnp.float32)
        s = s - slopes[:, :, None] * (tf[:, None] - c_mid[None, :])[:, None, None, :]
        p_c = masked_softmax(s, (c_end[None, :] <= t[:, None])[:, None, None, :])
        o_c = jnp.einsum("bqghn,bngd->bqghd", p_c, vc)
        imp = jnp.einsum("bqghn,nj->bqgj", p_c, w_ov)
        cur = (t // SEL_BLK)[:, None]
        forced = (j_blk == 0) | (j_blk == cur) | (j_blk == cur - 1)
        imp = jnp.where(forced[:, None], jnp.inf, jnp.where((j_blk <= cur)[:, None], imp, -jnp.inf))
        _, idx = lax.top_k(imp, n_sel)
        k_sel = ks[b_idx, g_idx, idx]
        v_sel = vs[b_idx, g_idx, idx].reshape(bsz, Q_BLOCK, B_GROUPS, n_sel * SEL_BLK, B_DV)
        pos = idx[..., None] * SEL_BLK + jnp.arange(SEL_BLK)
        dist = t[None, :, None, None, None] - pos
        s = jnp.einsum("bqghd,bqgnld->bqghnl", qb, k_sel, preferred_element_type=jnp.float32)
        s = s - slopes[:, :, None, None] * dist[:, :, :, None].astype(jnp.float32)
        s = s.reshape(bsz, Q_BLOCK, B_GROUPS, B_HPG, n_sel * SEL_BLK)
        mask = (dist >= 0)[:, :, :, None].reshape(bsz, Q_BLOCK, B_GROUPS, 1, n_sel * SEL_BLK)
        o_s = jnp.einsum("bqghk,bqgkd->bqghd", masked_softmax(s, mask), v_sel)
        k_w = lax.dynamic_slice_in_dim(kw, t0, WINDOW + Q_BLOCK, axis=1)
        v_w = lax.dynamic_slice_in_dim(vw, t0, WINDOW + Q_BLOCK, axis=1)
        kpos = t0 - WINDOW + jnp.arange(WINDOW + Q_BLOCK)
        wd = t[:, None] - kpos[None, :]
        wmask = (wd >= 0) & (wd < WINDOW) & (kpos >= 0)[None, :]
        s = jnp.einsum("bqghd,bkgd->bqghk", qb, k_w, preferred_element_type=jnp.float32)
        s = s - slopes[:, :, None] * wd.astype(jnp.float32)[:, None, None, :]
        o_w = jnp.einsum("bqghk,bkgd->bqghd", masked_softmax(s, wmask[:, None, None, :]), v_w)
        o_all = jnp.stack([o_c, o_s, o_w], axis=2)
        o = jnp.sum(gb[..., None] * zb * o_all, axis=2)
        return o.reshape(bsz, Q_BLOCK, B_INNER)

    t0s = jnp.arange(nqb, dtype=jnp.int32) * Q_BLOCK
    out = lax.map(query_block, (to_blocks(q), to_blocks(gates), to_blocks(zg), t0s))
    out = jnp.moveaxis(out, 0, 1).reshape(bsz, seq, B_INNER)
    return out.astype(h.dtype) @ w_out


def setup_inputs(seed: int = 0) -> dict:
    key = jax.random.key(seed)
    keys = iter(jax.random.split(key, 32))

    def nrm(shape, scale):
        return jax.random.normal(next(keys), shape, jnp.float32) * scale

    na, nb = N_A_LAYERS, N_B_LAYERS
    nqb_blocks = A_INNER // A_QKV_BLOCK
    forget_bias = jnp.asarray(np.linspace(3.0, 6.0, A_HEADS), jnp.float32)
    b_in_width = B_HEADS * B_DK + N_BRANCH * B_HEADS + N_BRANCH * B_INNER
    x = nrm((BATCH, SEQ, D_MODEL), 1.0)
    norm_pre = 1.0 + nrm((DEPTH, D_MODEL), 0.02)
    norm_post = 1.0 + nrm((DEPTH, D_MODEL), 0.02)
    a_w_in = nrm((na, D_MODEL, 3 * A_INNER), D_MODEL ** -0.5)
    a_conv_w = nrm((na, A_CONV, A_INNER), A_CONV ** -0.5)
    a_conv_b = nrm((na, A_INNER), 0.01)
    a_w_q = nrm((na, nqb_blocks, A_QKV_BLOCK, A_QKV_BLOCK), A_QKV_BLOCK ** -0.5)
    a_w_k = nrm((na, nqb_blocks, A_QKV_BLOCK, A_QKV_BLOCK), A_QKV_BLOCK ** -0.5)
    a_w_v = nrm((na, nqb_blocks, A_QKV_BLOCK, A_QKV_BLOCK), A_QKV_BLOCK ** -0.5)
    a_w_gate = nrm((na, 3 * A_INNER, 2 * A_HEADS), (3 * A_INNER) ** -0.5)
    a_b_gate = jnp.concatenate([nrm((na, A_HEADS), 0.1),
                                forget_bias[None, :] + nrm((na, A_HEADS), 0.01)], axis=-1)
    a_head_norm = 1.0 + nrm((na, A_INNER), 0.02)
    a_skip = 1.0 + nrm((na, A_INNER), 0.02)
    a_w_out = nrm((na, A_INNER, D_MODEL), A_INNER ** -0.5)
    kv_norm = 1.0 + nrm((D_MODEL,), 0.02)
    kv_w = nrm((D_MODEL, 2 * N_BRANCH * B_GROUPS * B_DK), D_MODEL ** -0.5)
    cmp_pos = nrm((2, CMP_BLK, B_DK), 0.02)
    cmp_w1 = nrm((2, CMP_BLK * B_DK, CMP_HIDDEN), (CMP_BLK * B_DK) ** -0.5)
    cmp_b1 = nrm((2, CMP_HIDDEN), 0.01)
    cmp_w2 = nrm((2, CMP_HIDDEN, B_DK), CMP_HIDDEN ** -0.5)
    cmp_b2 = nrm((2, B_DK), 0.01)
    b_w_in = nrm((nb, D_MODEL, b_in_width), D_MODEL ** -0.5)
    b_w_out = nrm((nb, B_INNER, D_MODEL), B_INNER ** -0.5)
    return {"x": x, "norm_pre": norm_pre, "norm_post": norm_post,
            "a_w_in": a_w_in, "a_conv_w": a_conv_w, "a_conv_b": a_conv_b,
            "a_w_q": a_w_q, "a_w_k": a_w_k, "a_w_v": a_w_v,
            "a_w_gate": a_w_gate, "a_b_gate": a_b_gate, "a_head_norm": a_head_norm,
            "a_skip": a_skip, "a_w_out": a_w_out,
            "kv_norm": kv_norm, "kv_w": kv_w, "cmp_pos": cmp_pos,
            "cmp_w1": cmp_w1, "cmp_b1": cmp_b1, "cmp_w2": cmp_w2, "cmp_b2": cmp_b2,
            "b_w_in": b_w_in, "b_w_out": b_w_out}


def reference(x, norm_pre, norm_post, a_w_in, a_conv_w, a_conv_b, a_w_q, a_w_k, a_w_v,
              a_w_gate, a_b_gate, a_head_norm, a_skip, a_w_out, kv_norm, kv_w, cmp_pos,
              cmp_w1, cmp_b1, cmp_w2, cmp_b2, b_w_in, b_w_out):
    shared_kv = None
    for layer in range(DEPTH):
        h = rmsnorm(x, norm_pre[layer])
        if layer < N_A_LAYERS:
            y = mlstm_mixer(h, a_w_in[layer], a_conv_w[layer], a_conv_b[layer], a_w_q[layer],
                            a_w_k[layer], a_w_v[layer], a_w_gate[layer], a_b_gate[layer],
                            a_head_norm[layer], a_skip[layer], a_w_out[layer])
        else:
            lb = layer - N_A_LAYERS
            y = nsa_mixer(h, shared_kv, b_w_in[lb], b_w_out[lb])
        x = x + rmsnorm(y, norm_post[layer])
        if layer == N_A_LAYERS - 1:
            shared_kv = nsa_shared_kv(x, kv_norm, kv_w, cmp_pos, cmp_w1, cmp_b1, cmp_w2, cmp_b2)
    return x
```

```python
import numpy as np
from contextlib import ExitStack
import concourse.bass as bass
import concourse.mybir as mybir
from concourse.bass_utils import run_bass_kernel_spmd

F32 = mybir.dt.float32
BF16 = mybir.dt.bfloat16
AF = mybir.ActivationFunctionType
ALU = mybir.AluOpType
EPS = 1e-6
D = 1024
AI = 2048
NEG = -30000.0


class Sched:
    ENG = ("pe", "act", "dve", "pool", "sp")

    def __init__(self, nc, stack, ndsem=16):
        self.nc = nc
        self.sem = {e: stack.enter_context(nc.semaphore("sem_" + e)) for e in self.ENG}
        self.cnt = {e: 0 for e in self.ENG}
        self.waited = {e: {} for e in self.ENG}
        self.stream = {e: [] for e in self.ENG}
        self.lastw = {}
        self.reads = {}
        self.dsems = [stack.enter_context(nc.semaphore("semd%d" % i)) for i in range(ndsem)]
        self.dsem_cnt = [0] * ndsem
        self.dsem_rr = 0
        self.ninst = 0

    def _deps(self, eng, reads, writes):
        deps = []
        for r in reads:
            w = self.lastw.get(r)
            if w is not None:
                deps.append(w)
        for w_ in writes:
            w = self.lastw.get(w_)
            if w is not None:
                deps.append(w)
            deps.extend(self.reads.get(w_, {}).values())
        out = {}
        for (s, v, tag) in deps:
            if tag == "pe" and eng == "pe":
                continue
            key = id(s)
            if v <= self.waited[eng].get(key, 0):
                continue
            if key not in out or out[key][1] < v:
                out[key] = (s, v)
        for key, (s, v) in out.items():
            self.waited[eng][key] = v
        return list(out.values())

    def _record(self, token, reads, writes):
        for r in reads:
            self.reads.setdefault(r, {})[token[2]] = token
        for w in writes:
            self.lastw[w] = token
            self.reads[w] = {}

    @staticmethod
    def _excl(reads, writes):
        ex = [k for k in reads if k.startswith("PS:")]
        if ex:
            writes = list(writes) + ex
            reads = [k for k in reads if not k.startswith("PS:")]
        return reads, writes

    def op(self, eng, fn, reads=(), writes=(), inc=True):
        reads, writes = self._excl(reads, writes)
        waits = self._deps(eng, reads, writes)
        for (s, v) in waits:
            if s is self.sem["pe"] and v > self.cnt["pe"]:
                raise RuntimeError("wait on un-signalled PE ticket")
        ticket = self.cnt[eng] + 1
        if inc:
            self.cnt[eng] = ticket
        else:
            assert eng == "pe"
        self.stream[eng].append((waits, fn, (self.sem[eng], 1) if inc else None))
        self._record((self.sem[eng], ticket, eng), reads, writes)
        self.ninst += 1

    def dma(self, eng, fn, reads=(), writes=()):
        i = self.dsem_rr
        self.dsem_rr = (self.dsem_rr + 1) % len(self.dsems)
        s = self.dsems[i]
        waits = self._deps(eng, reads, writes)
        prev = self.dsem_cnt[i]
        if prev > self.waited[eng].get(id(s), 0):
            waits = [w for w in waits if w[0] is not s] + [(s, prev)]
            self.waited[eng][id(s)] = prev
        self.dsem_cnt[i] = prev + 16
        self.stream[eng].append((waits, fn, (s, 16)))
        self._record((s, prev + 16, "d%d" % i), reads, writes)
        self.ninst += 1

    def barrier(self):
        assert all(it[2] is not None or it[1] is None for it in self.stream["pe"][-1:]), "last PE op must inc"
        for e in self.ENG:
            waits = []
            for e2 in self.ENG:
                s = self.sem[e2]
                if e2 != e and self.cnt[e2] > self.waited[e].get(id(s), 0):
                    waits.append((s, self.cnt[e2]))
                    self.waited[e][id(s)] = self.cnt[e2]
            for i, s in enumerate(self.dsems):
                if self.dsem_cnt[i] > self.waited[e].get(id(s), 0):
                    waits.append((s, self.dsem_cnt[i]))
                    self.waited[e][id(s)] = self.dsem_cnt[i]
            self.stream[e].append((waits, None, None))
        self.lastw.clear()
        self.reads.clear()

    def simulate(self, streams):
        if not hasattr(self, "simval"):
            self.simval = {}
        val = self.simval
        pos = {e: 0 for e in self.ENG}
        progress = True
        while progress:
            progress = False
            for e in self.ENG:
                items = streams[e]
                while pos[e] < len(items):
                    waits, fn, inc = items[pos[e]]
                    if any(val.get(id(s), 0) < v for (s, v) in waits):
                        break
                    if inc is not None:
                        val[id(inc[0])] = val.get(id(inc[0]), 0) + inc[1]
                    pos[e] += 1
                    progress = True
        for e in self.ENG:
            if pos[e] < len(streams[e]):
                waits, fn, inc = streams[e][pos[e]]
                bad = [(v, val.get(id(s), 0)) for (s, v) in waits if val.get(id(s), 0) < v]
                raise RuntimeError("DEADLOCK engine %s at item %d/%d waits(need,have)=%s" % (e, pos[e], len(streams[e]), bad))

    def emit(self):
        nc = self.nc
        streams = self.stream
        self.stream = {e: [] for e in self.ENG}
        self.simulate(streams)

        def run(engobj, items):
            for (waits, fn, inc) in items:
                for (s, v) in waits:
                    engobj.wait_ge(s, v)
                if fn is not None:
                    ins = fn(engobj)
                    if inc is not None:
                        ins.then_inc(inc[0], inc[1])

        with nc.Block() as block:
            @block.tensor
            def _(e):
                run(e, streams["pe"])

            @block.scalar
            def _(e):
                run(e, streams["act"])

            @block.vector
            def _(e):
                run(e, streams["dve"])

            @block.gpsimd
            def _(e):
                run(e, streams["pool"])

            @block.sync
            def _(e):
                run(e, streams["sp"])


def MM(S, out, lhsT, rhs, start, stop, reads, writes, inc=None):
    if inc is None:
        inc = stop
    S.op("pe", lambda e: e.matmul(out, lhsT=lhsT, rhs=rhs, start=start, stop=stop), reads, writes, inc)


def TR(S, out, in_, ident, reads, writes, inc=True):
    S.op("pe", lambda e: e.transpose(out, in_, ident), reads, writes, inc)


def ACT(S, out, in_, func, reads, writes, bias=None, scale=None):
    kw = {}
    if bias is not None:
        kw["bias"] = bias
    if scale is not None:
        kw["scale"] = scale
    S.op("act", lambda e: e.activation(out=out, in_=in_, func=func, **kw), reads, writes)


def TT(S, eng, out, in0, in1, op, reads, writes):
    S.op(eng, lambda e: e.tensor_tensor(out=out, in0=in0, in1=in1, op=op), reads, writes)


def TS(S, eng, out, in0, s1, s2, op0, op1, reads, writes):
    if s2 is None:
        S.op(eng, lambda e: e.tensor_scalar(out=out, in0=in0, scalar1=s1, scalar2=None, op0=op0), reads, writes)
    else:
        S.op(eng, lambda e: e.tensor_scalar(out=out, in0=in0, scalar1=s1, scalar2=s2, op0=op0, op1=op1), reads, writes)


def STT(S, eng, out, in0, scalar, in1, op0, op1, reads, writes, accum_out=None):
    if accum_out is None:
        S.op(eng, lambda e: e.scalar_tensor_tensor(out=out, in0=in0, scalar=scalar, in1=in1, op0=op0, op1=op1),
             reads, writes)
    else:
        S.op(eng, lambda e: e.scalar_tensor_tensor(out=out, in0=in0, scalar=scalar, in1=in1, op0=op0, op1=op1,
                                                   accum_out=accum_out), reads, writes)


def CP(S, eng, out, in_, reads, writes):
    if eng == "act":
        S.op("act", lambda e: e.copy(out=out, in_=in_), reads, writes)
    else:
        S.op(eng, lambda e: e.tensor_copy(out=out, in_=in_), reads, writes)


def DMA(S, eng, out, in_, reads=(), writes=()):
    S.dma(eng, lambda e: e.dma_start(out=out, in_=in_), reads, writes)


class Ctx:
    pass


_UID = [0]


def alloc(nc, st, name, shape, dt):
    _UID[0] += 1
    return st.enter_context(nc.sbuf_tensor("%s_u%d" % (name, _UID[0]), list(shape), dt))


def palloc(nc, st, name, shape, dt):
    _UID[0] += 1
    return st.enter_context(nc.psum_tensor("%s_u%d" % (name, _UID[0]), list(shape), dt))


def alloc_normT(nc, st, C):
    R = Ctx()
    R.xt = [alloc(nc, st, "xt%d" % i, [128, 4, D], F32) for i in range(2)]
    R.junk = alloc(nc, st, "nt_junk", [128, D], BF16)
    R.ss = [alloc(nc, st, "nt_ss%d" % i, [128, 4], F32) for i in range(2)]
    R.rstd = [alloc(nc, st, "nt_rstd%d" % i, [128, 4], F32) for i in range(2)]
    R.hn = alloc(nc, st, "nt_hn", [128, 4, D], BF16)
    R.hT = [alloc(nc, st, "hT%d" % i, [128, 8, 512], BF16) for i in range(2)]
    R.ptr = [palloc(nc, st, "nt_ptr%d" % i, [128, 1024], BF16) for i in range(2)]
    R.ident = C.ident
    return R


def load_x(S, R, xsrc, st):
    b = st % 2
    DMA(S, "sp", R.xt[b][:], xsrc[st * 512:(st + 1) * 512, :].rearrange("(a p) d -> p a d", p=128),
        writes=["xt%d" % b])


def norm_T(S, R, st):
    b = st % 2
    xt = R.xt[b]
    xk = "xt%d" % b
    for a in range(4):
        STT(S, "dve", R.junk[:], xt[:, a, :], 1.0, xt[:, a, :], ALU.mult, ALU.mult, [xk], ["nt_junk", "ss%d" % b],
            accum_out=R.ss[b][:, a:a + 1])
    TS(S, "dve", R.rstd[b][:], R.ss[b][:], 1.0 / D, EPS, ALU.mult, ALU.add, ["ss%d" % b], ["rstd%d" % b])
    ACT(S, R.rstd[b][:], R.rstd[b][:], AF.Sqrt, ["rstd%d" % b], ["rstd%d" % b])
    S.op("dve", lambda e: e.reciprocal(out=R.rstd[b][:], in_=R.rstd[b][:]), ["rstd%d" % b], ["rstd%d" % b])
    for a in range(4):
        TS(S, "dve" if a % 2 == 0 else "pool", R.hn[:, a, :], xt[:, a, :], R.rstd[b][:, a:a + 1], None, ALU.mult, None,
           [xk, "rstd%d" % b], ["hn%d" % a])
    for k in range(8):
        pt = R.ptr[k % 2]
        pk = "PS:ptr%d" % (k % 2)
        for a in range(4):
            TR(S, pt[:, a * 128:(a + 1) * 128], R.hn[:, a, k * 128:(k + 1) * 128], R.ident[:],
               ["hn%d" % a], [pk], inc=(a == 3))
        CP(S, "act" if k % 2 == 0 else "dve", R.hT[b][:, k, :], pt[:, 0:512], [pk], ["hT%d_%d" % (b, k)])
    return R.hT[b], ["hT%d_%d" % (b, k) for k in range(8)]


def pass_A1(nc, S, C, T, xsrc, lw, XM, XC, G):
    NST = T // 512
    with ExitStack() as st:
        R = alloc_normT(nc, st, C)
        W = alloc(nc, st, "a1_w", [128, 8, AI], BF16)
        gf = alloc(nc, st, "a1_gf", [128, 8], F32)
        cw = alloc(nc, st, "a1_cw", [128, 16, 4], F32)
        cb = alloc(nc, st, "a1_cb", [128, 16], F32)
        bdqT = alloc(nc, st, "a1_bdqT", [128, 16, 128], BF16)
        bdkT = alloc(nc, st, "a1_bdkT", [128, 16, 128], BF16)
        bdvT = alloc(nc, st, "a1_bdvT", [128, 16, 128], BF16)
        wg = alloc(nc, st, "a1_wg", [128, 48, 8], BF16)
        wgc = alloc(nc, st, "a1_wgc", [128, 16, 8], BF16)
        wgm = alloc(nc, st, "a1_wgm", [128, 16, 8], BF16)
        bg = alloc(nc, st, "a1_bg", [128, 8], F32)
        xmb = [alloc(nc, st, "a1_xmb%d" % i, [128, 16, 515], BF16) for i in range(2)]
        acc = [alloc(nc, st, "a1_acc%d" % i, [128, 512], F32) for i in range(2)]
        xm_st = [alloc(nc, st, "a1_xmst%d" % i, [128, 4, 16, 128], BF16) for i in range(2)]
        xc_st = [alloc(nc, st, "a1_xcst%d" % i, [128, 4, 16, 128], BF16) for i in range(2)]
        g_st = [alloc(nc, st, "a1_gst%d" % i, [128, 4, 8], F32) for i in range(2)]
        pm = [palloc(nc, st, "a1_pm%d" % i, [128, 512], F32) for i in range(3)]
        pgf = palloc(nc, st, "a1_pg", [128, 512], F32)
        pg = pgf[:, 0:32].rearrange("p (a g) -> p a g", a=4)

        DMA(S, "pool", W[:], lw["w_in"].rearrange("(k p) c -> p k c", p=128)[:, :, 0:AI], writes=["W"])
        DMA(S, "sp", gf[:], lw["gfold"], writes=["gf"])
        DMA(S, "sp", cw[:], lw["conv_w"], writes=["cw"])
        DMA(S, "sp", cb[:], lw["conv_b"], writes=["cb"])
        DMA(S, "pool", bdqT[:], lw["bdqT"], writes=["bdqT"])
        DMA(S, "pool", bdkT[:], lw["bdkT"], writes=["bdkT"])
        DMA(S, "pool", bdvT[:], lw["bdvT"], writes=["bdvT"])
        DMA(S, "pool", wg[:], lw["w_gate"], writes=["wg"])
        DMA(S, "sp", bg[:], lw["b_gate"], writes=["bg"])
        for k in range(8):
            TS(S, "dve", W[:, k, :], W[:, k, :], gf[:, k:k + 1], None, ALU.mult, None, ["W", "gf"], ["W"])
        for j in range(16):
            MM(S, pg[:, 0, :], bdqT[:, j, :], wg[:, j, :], True, False, ["bdqT", "wg"], ["PS:pg"])
            MM(S, pg[:, 0, :], bdkT[:, j, :], wg[:, 16 + j, :], False, True, ["bdkT", "wg"], ["PS:pg"])
            CP(S, "dve", wgc[:, j, :], pg[:, 0, :], ["PS:pg"], ["wgc"])
            MM(S, pg[:, 1, :], bdvT[:, j, :], wg[:, 32 + j, :], True, True, ["bdvT", "wg"], ["PS:pg"])
            CP(S, "dve", wgm[:, j, :], pg[:, 1, :], ["PS:pg"], ["wgm"])
        S.op("pool", lambda e: e.memset(xmb[0][:, :, 0:3], 0.0), [], ["xmb0h"])

        load_x(S, R, xsrc, 0)
        for s_ in range(NST):
            b = s_ % 2
            if s_ + 1 < NST:
                load_x(S, R, xsrc, s_ + 1)
            hT, hk = norm_T(S, R, s_)
            xb = xmb[b]
            xbk = "xmb%d" % b
            for j in range(16):
                p = pm[j % 3]
                pk = "PS:pm%d" % (j % 3)
                for k in range(8):
                    MM(S, p[:], W[:, k, j * 128:(j + 1) * 128], hT[:, k, :], k == 0, k == 7, ["W", hk[k]], [pk])
                CP(S, "act", xb[:, j, 3:515], p[:], [pk], [xbk + "_%d" % j])
                CP(S, "pool", xm_st[b][:, :, j, :], xb[:, j, 3:515].rearrange("p (a t) -> p a t", a=4),
                   [xbk + "_%d" % j], ["xmst%d" % b])
            if s_ + 1 < NST:
                CP(S, "pool", xmb[1 - b][:, :, 0:3], xb[:, :, 512:515], [xbk + "_%d" % j for j in range(16)],
                   ["xmb%dh" % (1 - b)])
            for j in range(16):
                a_ = acc[j % 2]
                ak = "acc%d" % (j % 2)
                rk = [xbk + "_%d" % j, xbk + "h", "cw", "cb"]
                TS(S, "dve", a_[:], xb[:, j, 0:512], cw[:, j, 0:1], cb[:, j:j + 1], ALU.mult, ALU.add, rk, [ak])
                for tap in range(1, 4):
                    STT(S, "dve", a_[:], xb[:, j, tap:tap + 512], cw[:, j, tap:tap + 1], a_[:], ALU.mult, ALU.add,
                        rk + [ak], [ak])
                ACT(S, xc_st[b][:, :, j, :], a_[:].rearrange("p (a t) -> p a t", a=4), AF.Silu, [ak], ["xcst%d" % b])
            for a in range(4):
                for j in range(16):
                    MM(S, pg[:, a, :], xc_st[b][:, a, j, :], wgc[:, j, :], j == 0, False, ["xcst%d" % b, "wgc"], ["PS:pg"])
                for j in range(16):
                    MM(S, pg[:, a, :], xm_st[b][:, a, j, :], wgm[:, j, :], False, j == 15, ["xmst%d" % b, "wgm"], ["PS:pg"])
            TT(S, "dve", g_st[b][:], pg[:], bg[:].unsqueeze(1).to_broadcast([128, 4, 8]), ALU.add, ["PS:pg", "bg"],
               ["gst%d" % b])
            DMA(S, "sp", XM[s_ * 4:(s_ + 1) * 4].rearrange("a p j t -> p a (j t)"),
                xm_st[b][:].rearrange("p a j t -> p a (j t)"), reads=["xmst%d" % b])
            DMA(S, "sp", XC[s_ * 4:(s_ + 1) * 4].rearrange("a p j t -> p a (j t)"),
                xc_st[b][:].rearrange("p a j t -> p a (j t)"), reads=["xcst%d" % b])
            DMA(S, "sp", G[s_ * 512:(s_ + 1) * 512, :].rearrange("(a p) g -> p a g", p=128), g_st[b][:],
                reads=["gst%d" % b])
        S.barrier()
        S.emit()


def pass_A2(nc, S, C, T, xsrc, lw, SZ, SO):
    NST = T // 512
    with ExitStack() as st:
        R = alloc_normT(nc, st, C)
        W = alloc(nc, st, "a2_w", [128, 8, 2 * AI], BF16)
        gf = alloc(nc, st, "a2_gf", [128, 8], F32)
        sz_st = [alloc(nc, st, "a2_szst%d" % i, [128, 4, 16, 128], BF16) for i in range(2)]
        so_st = [alloc(nc, st, "a2_sost%d" % i, [128, 4, AI], BF16) for i in range(2)]
        pm = [palloc(nc, st, "a2_pm%d" % i, [128, 512], F32) for i in range(4)]
        DMA(S, "pool", W[:], lw["w_in"].rearrange("(k p) c -> p k c", p=128)[:, :, AI:3 * AI], writes=["W"])
        DMA(S, "sp", gf[:], lw["gfold"], writes=["gf"])
        for k in range(8):
            TS(S, "dve", W[:, k, :], W[:, k, :], gf[:, k:k + 1], None, ALU.mult, None, ["W", "gf"], ["W"])
        load_x(S, R, xsrc, 0)
        n = 0
        for s_ in range(NST):
            b = s_ % 2
            if s_ + 1 < NST:
                load_x(S, R, xsrc, s_ + 1)
            hT, hk = norm_T(S, R, s_)
            for a in range(4):
                for cg in range(4):
                    p = pm[n % 4]
                    pk = "PS:pm%d" % (n % 4)
                    n += 1
                    for k in range(8):
                        MM(S, p[:], hT[:, k, a * 128:(a + 1) * 128], W[:, k, cg * 512:(cg + 1) * 512], k == 0, k == 7,
                           ["W", hk[k]], [pk])
                    ACT(S, so_st[b][:, a, cg * 512:(cg + 1) * 512], p[:], AF.Sigmoid, [pk], ["sost%d" % b])
            for j in range(16):
                p = pm[n % 4]
                pk = "PS:pm%d" % (n % 4)
                n += 1
                for k in range(8):
                    MM(S, p[:], W[:, k, AI + j * 128:AI + (j + 1) * 128], hT[:, k, :], k == 0, k == 7, ["W", hk[k]], [pk])
                ACT(S, sz_st[b][:, :, j, :], p[:].rearrange("p (a t) -> p a t", a=4), AF.Silu, [pk], ["szst%d" % b])
            DMA(S, "sp", SZ[s_ * 4:(s_ + 1) * 4].rearrange("a p j t -> p a (j t)"),
                sz_st[b][:].rearrange("p a j t -> p a (j t)"), reads=["szst%d" % b])
            DMA(S, "sp", SO[s_ * 512:(s_ + 1) * 512, :].rearrange("(a p) c -> p a c", p=128), so_st[b][:],
                reads=["sost%d" % b])
        S.barrier()
        S.emit()


def post_residual(S, P, py, pyk, xin, xink, gpost, xout, xoutk):
    for h in range(2):
        sl = slice(h * 512, (h + 1) * 512)
        CP(S, "act", P.pr_y[:, sl], py[h][:], [pyk[h]], ["pr_y%d" % h])
        STT(S, "dve", P.pr_junk[:], P.pr_y[:, sl], 1.0, P.pr_y[:, sl], ALU.mult, ALU.mult, ["pr_y%d" % h],
            ["pr_junk", "pr_ss"], accum_out=P.pr_ss[:, h:h + 1])
    TT(S, "dve", P.pr_r[:], P.pr_ss[:, 0:1], P.pr_ss[:, 1:2], ALU.add, ["pr_ss"], ["pr_r"])
    TS(S, "dve", P.pr_r[:], P.pr_r[:], 1.0 / D, EPS, ALU.mult, ALU.add, ["pr_r"], ["pr_r"])
    ACT(S, P.pr_r[:], P.pr_r[:], AF.Sqrt, ["pr_r"], ["pr_r"])
    S.op("dve", lambda e: e.reciprocal(out=P.pr_r[:], in_=P.pr_r[:]), ["pr_r"], ["pr_r"])
    for h in range(2):
        sl = slice(h * 512, (h + 1) * 512)
        STT(S, "dve", P.pr_t[:, sl], P.pr_y[:, sl], P.pr_r[:, 0:1], gpost[:, sl], ALU.mult, ALU.mult,
            ["pr_y%d" % h, "pr_r", "gpost"], ["pr_t%d" % h])
        TT(S, "pool", xout[:, sl], P.pr_t[:, sl], xin[:, sl], ALU.add, ["pr_t%d" % h, xink], [xoutk])


def alloc_post(nc, st):
    P = Ctx()
    P.pr_junk = alloc(nc, st, "pr_junk", [128, 512], BF16)
    P.pr_ss = alloc(nc, st, "pr_ss", [128, 2], F32)
    P.pr_r = alloc(nc, st, "pr_r", [128, 1], F32)
    P.pr_t = alloc(nc, st, "pr_t", [128, D], F32)
    P.pr_y = alloc(nc, st, "pr_y", [128, D], F32)
    return P


def pass_B(nc, S, C, T, xsrc, xdst, lw, XM, XC, SZ, SO, G):
    NCH = T // 128
    with ExitStack() as st:
        P = alloc_post(nc, st)
        bdq = alloc(nc, st, "b_bdq", [128, 16, 128], BF16)
        bdk = alloc(nc, st, "b_bdk", [128, 16, 128], BF16)
        bdv = alloc(nc, st, "b_bdv", [128, 16, 128], BF16)
        wout = alloc(nc, st, "b_wout", [128, 16, D], BF16)
        hnw = alloc(nc, st, "b_hnw", [128, AI], F32)
        skp = alloc(nc, st, "b_skip", [128, 16], F32)
        gpost = alloc(nc, st, "b_gpost", [128, D], F32)
        C32 = alloc(nc, st, "b_C32", [128, 16, 512], F32)
        Cbf = alloc(nc, st, "b_Cbf", [128, 16, 512], BF16)
        n32 = alloc(nc, st, "b_n32", [128, 16], F32)
        nbf = alloc(nc, st, "b_nbf", [128, 16], BF16)
        xc = [alloc(nc, st, "b_xc%d" % i, [128, 16, 128], BF16) for i in range(2)]
        xm = [alloc(nc, st, "b_xm%d" % i, [128, 16, 128], BF16) for i in range(2)]
        sz = [alloc(nc, st, "b_sz%d" % i, [128, 16, 128], BF16) for i in range(2)]
        so = [alloc(nc, st, "b_so%d" % i, [128, AI], BF16) for i in range(2)]
        gt = [alloc(nc, st, "b_gt%d" % i, [128, 8], F32) for i in range(2)]
        xin = [alloc(nc, st, "b_xin0", [128, D], F32)] * 2
        xout = [alloc(nc, st, "b_xout0", [128, D], F32)] * 2
        qT = alloc(nc, st, "b_qT", [128, 16, 128], BF16)
        qsT = alloc(nc, st, "b_qsT", [128, 16, 128], BF16)
        kT = alloc(nc, st, "b_kT", [128, 16, 128], BF16)
        ktok = alloc(nc, st, "b_ktok", [128, AI], BF16)
        kw = alloc(nc, st, "b_kw", [128, AI], BF16)
        vtok = alloc(nc, st, "b_vtok", [128, AI], BF16)
        lf = alloc(nc, st, "b_lf", [128, 4], F32)
        lfrep = alloc(nc, st, "b_lfrep", [128, 4, 128], F32)
        av = alloc(nc, st, "b_a", [128, 4], F32)
        wv = alloc(nc, st, "b_w", [128, 4], F32)
        ebt = alloc(nc, st, "b_ebt", [128, 4, 128], F32)
        logd = alloc(nc, st, "b_logd", [128, 4, 128], F32)
        dT = alloc(nc, st, "b_dT", [128, 4, 128], F32)
        pT = alloc(nc, st, "b_pT", [128, 4, 128], BF16)
        den = alloc(nc, st, "b_den", [128, 4], F32)
        hc = alloc(nc, st, "b_hc", [128, 512], F32)
        bst = alloc(nc, st, "b_bst", [128, 6], F32)
        mv = alloc(nc, st, "b_mv", [128, 2], F32)
        hn = alloc(nc, st, "b_hn", [128, AI], BF16)
        xs = alloc(nc, st, "b_xs", [128, 16, 128], BF16)
        yT = alloc(nc, st, "b_yT", [128, 16, 128], BF16)
        tmpf = alloc(nc, st, "b_tmpf", [128, 4, 128], F32)
        onesb = alloc(nc, st, "b_ones", [128, 1], BF16)
        pA = [palloc(nc, st, "b_pA%d" % i, [128, 512], F32) for i in range(4)]
        pS = palloc(nc, st, "b_pS", [128, 512], F32)
        pB = palloc(nc, st, "b_pB", [128, 512], F32)
        pD = palloc(nc, st, "b_pD", [128, 512], F32)
        pTb = palloc(nc, st, "b_pTb", [128, 1024], BF16)

        DMA(S, "pool", bdq[:], lw["bdq"], writes=["bdq"])
        DMA(S, "pool", bdk[:], lw["bdk"], writes=["bdk"])
        DMA(S, "pool", bdv[:], lw["bdv"], writes=["bdv"])
        DMA(S, "pool", wout[:], lw["w_out"].rearrange("(j p) c -> p j c", p=128), writes=["wout"])
        DMA(S, "sp", hnw[:], lw["head_norm"], writes=["hnw"])
        DMA(S, "sp", skp[:], lw["skip"], writes=["skp"])
        DMA(S, "sp", gpost[:], lw["gpost"], writes=["gpost"])
        S.op("pool", lambda e: e.memset(C32[:], 0.0), [], ["C32"])
        S.op("pool", lambda e: e.memset(Cbf[:], 0.0), [], ["Cbf"])
        S.op("pool", lambda e: e.memset(n32[:], 0.0), [], ["n32"])
        S.op("pool", lambda e: e.memset(nbf[:], 0.0), [], ["nbf"])
        S.op("pool", lambda e: e.memset(onesb[:], 1.0), [], ["onesb"])

        def loads(c):
            b = c % 2
            DMA(S, "sp", xc[b][:].rearrange("p j t -> p (j t)"), XC[c], writes=["xc%d" % b])
            DMA(S, "sp", xm[b][:].rearrange("p j t -> p (j t)"), XM[c], writes=["xm%d" % b])
            DMA(S, "sp", sz[b][:].rearrange("p j t -> p (j t)"), SZ[c], writes=["sz%d" % b])
            DMA(S, "sp", so[b][:], SO[c * 128:(c + 1) * 128, :], writes=["so%d" % b])
            DMA(S, "sp", gt[b][:], G[c * 128:(c + 1) * 128, :], writes=["gt%d" % b])

        loads(0)
        DMA(S, "sp", xin[0][:], xsrc[0:128, :], writes=["xin0"])
        npa = 0
        for c in range(NCH):
            b = c % 2
            if c + 1 < NCH:
                loads(c + 1)
            xck, xmk, szk, sok, gtk = "xc%d" % b, "xm%d" % b, "sz%d" % b, "so%d" % b, "gt%d" % b
            for h in range(4):
                for (dst, bd, src, srck, dk_, scale) in ((qT, bdq, xc[b], xck, "qT", None),
                                                         (kT, bdk, xc[b], xck, "kT", 512.0 ** -0.5)):
                    p = pA[npa % 4]
                    pk = "PS:pA%d" % (npa % 4)
                    npa += 1
                    for jj in range(4):
                        j = h * 4 + jj
                        MM(S, p[:, jj * 128:(jj + 1) * 128], bd[:, j, :], src[:, j, :], True, True,
                           ["bdq", "bdk", srck], [pk], inc=(jj == 3))
                    if scale is None:
                        CP(S, "act", dst[:, h * 4:(h + 1) * 4, :], p[:].rearrange("p (a t) -> p a t", a=4), [pk],
                           ["%s%d" % (dk_, h)])
                    else:
                        S.op("act", lambda e, dst=dst, p=p, h=h, scale=scale: e.mul(
                            out=dst[:, h * 4:(h + 1) * 4, :], in_=p[:].rearrange("p (a t) -> p a t", a=4), mul=scale),
                            [pk], ["%s%d" % (dk_, h)])
                for (dst, bd, src, srck, dk_, scale) in ((ktok, bdk, xc[b], xck, "ktok", 512.0 ** -0.5),
                                                         (vtok, bdv, xm[b], xmk, "vtok", None)):
                    p = pA[npa % 4]
                    pk = "PS:pA%d" % (npa % 4)
                    npa += 1
                    for jj in range(4):
                        j = h * 4 + jj
                        MM(S, p[:, jj * 128:(jj + 1) * 128], src[:, j, :], bd[:, j, :], True, True,
                           ["bdk", "bdv", srck], [pk], inc=(jj == 3))
                    if scale is None:
                        CP(S, "dve", dst[:, h * 512:(h + 1) * 512], p[:], [pk], ["%s%d" % (dk_, h)])
                    else:
                        TS(S, "dve", dst[:, h * 512:(h + 1) * 512], p[:], scale, None, ALU.mult, None, [pk],
                           ["%s%d" % (dk_, h)])
            ACT(S, lf[:], gt[b][:, 4:8], AF.Exp, [gtk], ["lf"], scale=-1.0)
            ACT(S, lf[:], lf[:], AF.Ln, ["lf"], ["lf"], bias=1.0)
            TS(S, "dve", lf[:], lf[:], -1.0, None, ALU.mult, None, ["lf"], ["lf"])
            CP(S, "dve", lfrep[:], lf[:].unsqueeze(2).to_broadcast([128, 4, 128]), ["lf"], ["lfrep"])
            MM(S, pD[:, 8:12], C.tri[:], lf[:], True, True, ["lf"], ["PS:pD"])
            TT(S, "dve", av[:], gt[b][:, 0:4], pD[:, 8:12], ALU.subtract, [gtk, "PS:pD"], ["av"])
            for h in range(4):
                MM(S, pB[:, h * 128:(h + 1) * 128], lfrep[:, h, :], C.tri[:], True, True, ["lfrep"], ["PS:pB"], inc=(h == 3))
            pB3 = pB[:].rearrange("p (h t) -> p h t", h=4)
            ACT(S, ebt[:], pB3, AF.Exp, ["PS:pB"], ["ebt"])
            TT(S, "dve", logd[:], pB3, C.maskb[:].unsqueeze(1).to_broadcast([128, 4, 128]), ALU.add, ["PS:pB"], ["logd"])
            TT(S, "dve", wv[:], av[:], pB3[:, :, 127], ALU.add, ["av", "PS:pB"], ["wv"])
            ACT(S, wv[:], wv[:], AF.Exp, ["wv"], ["wv"])
            for h in range(4):
                ACT(S, dT[:, h, :], logd[:, h, :], AF.Exp, ["logd", "av"], ["dT"], bias=av[:, h:h + 1])
            for h in range(4):
                for jj in range(4):
                    j = h * 4 + jj
                    MM(S, pS[:, h * 128:(h + 1) * 128], kT[:, j, :], qT[:, j, :], jj == 0, jj == 3,
                       ["kT%d" % h, "qT%d" % h], ["PS:pS"], inc=(h == 3 and jj == 3))
            TT(S, "dve", pT[:], pS[:].rearrange("p (h t) -> p h t", h=4), dT[:], ALU.mult, ["PS:pS", "dT"], ["pT"])
            for h in range(4):
                TT(S, "pool", qsT[:, h * 4:(h + 1) * 4, :], qT[:, h * 4:(h + 1) * 4, :],
                   ebt[:, h, :].unsqueeze(1).to_broadcast([128, 4, 128]), ALU.mult, ["qT%d" % h, "ebt"], ["qsT%d" % h])
            for h in range(4):
                MM(S, pD[:, h:h + 1], pT[:, h, :], onesb[:], True, False, ["pT", "onesb"], ["PS:pD"], inc=False)
                for jj in range(4):
                    j = h * 4 + jj
                    MM(S, pD[:, h:h + 1], qsT[:, j, :], nbf[:, j:j + 1], False, jj == 3, ["qsT%d" % h, "nbf"], ["PS:pD"],
                       inc=(h == 3 and jj == 3))
            ACT(S, den[:], pD[:, 0:4], AF.Abs, ["PS:pD"], ["den"])
            TS(S, "dve", den[:], den[:], 1.0, None, ALU.max, None, ["den"], ["den"])
            S.op("dve", lambda e: e.reciprocal(out=den[:], in_=den[:]), ["den"], ["den"])
            for h in range(4):
                p = pA[npa % 4]
                pk = "PS:pA%d" % (npa % 4)
                npa += 1
                MM(S, p[:], pT[:, h, :], vtok[:, h * 512:(h + 1) * 512], True, False, ["pT", "vtok%d" % h], [pk], inc=False)
                for jj in range(4):
                    j = h * 4 + jj
                    MM(S, p[:], qsT[:, j, :], Cbf[:, j, :], False, jj == 3, ["qsT%d" % h, "Cbf%d" % h], [pk])
                STT(S, "dve", hc[:], p[:], den[:, h:h + 1], so[b][:, h * 512:(h + 1) * 512], ALU.mult, ALU.mult,
                    [pk, "den", sok], ["hc"])
                S.op("dve", lambda e: e.bn_stats(out=bst[:], in_=hc[:]), ["hc"], ["bst"])
                S.op("dve", lambda e: e.bn_aggr(out=mv[:], in_=bst[:]), ["bst"], ["mv"])
                TS(S, "dve", mv[:, 1:2], mv[:, 1:2], EPS, None, ALU.add, None, ["mv"], ["mv"])
                ACT(S, mv[:, 1:2], mv[:, 1:2], AF.Sqrt, ["mv"], ["mv"])
                S.op("dve", lambda e: e.reciprocal(out=mv[:, 1:2], in_=mv[:, 1:2]), ["mv"], ["mv"])
                TS(S, "dve", hc[:], hc[:], mv[:, 0:1], mv[:, 1:2], ALU.subtract, ALU.mult, ["hc", "mv"], ["hc"])
                TT(S, "dve", hn[:, h * 512:(h + 1) * 512], hc[:], hnw[:, h * 512:(h + 1) * 512], ALU.mult,
                   ["hc", "hnw"], ["hn%d" % h])
                for jj in range(4):
                    TR(S, pTb[:, (h % 2) * 512 + jj * 128:(h % 2) * 512 + (jj + 1) * 128],
                       hn[:, h * 512 + jj * 128:h * 512 + (jj + 1) * 128], C.ident[:], ["hn%d" % h],
                       ["PS:pTb"], inc=(jj == 3))
                TT(S, "pool", xs[:, h * 4:(h + 1) * 4, :], xc[b][:, h * 4:(h + 1) * 4, :],
                   skp[:, h * 4:(h + 1) * 4].unsqueeze(2).to_broadcast([128, 4, 128]), ALU.mult, [xck, "skp"],
                   ["xs%d" % h])
                TT(S, "dve", tmpf[:], pTb[:, (h % 2) * 512:(h % 2 + 1) * 512].rearrange("p (a t) -> p a t", a=4),
                   xs[:, h * 4:(h + 1) * 4, :], ALU.add, ["PS:pTb", "xs%d" % h], ["tmpf"])
                TT(S, "dve", yT[:, h * 4:(h + 1) * 4, :], tmpf[:], sz[b][:, h * 4:(h + 1) * 4, :], ALU.mult,
                   ["tmpf", szk], ["yT%d" % h])
            py = [pA[npa % 4], pA[(npa + 1) % 4]]
            pyk = ["PS:pA%d" % (npa % 4), "PS:pA%d" % ((npa + 1) % 4)]
            npa += 2
            for hh in range(2):
                for j in range(16):
                    MM(S, py[hh][:], yT[:, j, :], wout[:, j, hh * 512:(hh + 1) * 512], j == 0, j == 15,
                       ["yT%d" % (j // 4), "wout"], [pyk[hh]])
            post_residual(S, P, py, pyk, xin[b], "xin0", gpost, xout[b], "xout0")
            DMA(S, "sp", xdst[c * 128:(c + 1) * 128, :], xout[b][:], reads=["xout0"])
            if c + 1 < NCH:
                DMA(S, "sp", xin[0][:], xsrc[(c + 1) * 128:(c + 2) * 128, :], writes=["xin0"])
            for h in range(4):
                TS(S, "pool", kw[:, h * 512:(h + 1) * 512], ktok[:, h * 512:(h + 1) * 512], wv[:, h:h + 1], None,
                   ALU.mult, None, ["ktok%d" % h, "wv"], ["kw%d" % h])
            for h in range(4):
                for jj in range(4):
                    j = h * 4 + jj
                    MM(S, pD[:, 16 + j:17 + j], kw[:, j * 128:(j + 1) * 128], onesb[:], True, True, ["kw%d" % h, "onesb"],
                       ["PS:pD"], inc=(h == 3 and jj == 3))
            for h in range(4):
                STT(S, "dve", n32[:, h * 4:(h + 1) * 4], n32[:, h * 4:(h + 1) * 4], ebt[:, h, 127:128],
                    pD[:, 16 + h * 4:16 + (h + 1) * 4], ALU.mult, ALU.add, ["n32", "ebt", "PS:pD"], ["n32"])
            CP(S, "dve", nbf[:], n32[:], ["n32"], ["nbf"])
            for h in range(4):
                for jj in range(4):
                    j = h * 4 + jj
                    p = pA[npa % 4]
                    pk = "PS:pA%d" % (npa % 4)
                    npa += 1
                    MM(S, p[:], kw[:, j * 128:(j + 1) * 128], vtok[:, h * 512:(h + 1) * 512], True, True,
                       ["kw%d" % h, "vtok%d" % h], [pk])
                    STT(S, "dve", C32[:, j, :], C32[:, j, :], ebt[:, h, 127:128], p[:], ALU.mult, ALU.add,
                        ["C32_%d" % j, "ebt", pk], ["C32_%d" % j])
                    CP(S, "act", Cbf[:, j, :], C32[:, j, :], ["C32_%d" % j], ["Cbf%d" % h])
        S.barrier()
        S.emit()


def host_consts():
    c = {}
    c["ident"] = np.eye(128, dtype=np.float32)
    s = np.arange(128)
    c["tri"] = (s[:, None] <= s[None, :]).astype(np.float32)
    c["maskb"] = np.where(s[:, None] <= s[None, :], 0.0, NEG).astype(np.float32)
    return c


def blockdiag_layout(w):
    bd = np.zeros((16, 128, 128), np.float32)
    wj = w.reshape(16, 32, 4, 4)
    for g in range(32):
        bd[:, g * 4:(g + 1) * 4, g * 4:(g + 1) * 4] = wj[:, g]
    return (np.ascontiguousarray(bd.transpose(1, 0, 2)), np.ascontiguousarray(bd.transpose(2, 0, 1)))


def layer_a_layout(inp, l):
    o = {}
    o["w_in"] = np.ascontiguousarray(inp["a_w_in"][l])
    o["gfold"] = np.ascontiguousarray(inp["norm_pre"][l].reshape(8, 128).T)
    o["conv_w"] = np.ascontiguousarray(inp["a_conv_w"][l].reshape(4, 16, 128).transpose(2, 1, 0))
    o["conv_b"] = np.ascontiguousarray(inp["a_conv_b"][l].reshape(16, 128).T)
    for n, k in (("q", "a_w_q"), ("k", "a_w_k"), ("v", "a_w_v")):
        bd, bdT = blockdiag_layout(inp[k][l])
        o["bd" + n] = bd
        o["bd" + n + "T"] = bdT
    o["w_gate"] = np.ascontiguousarray(inp["a_w_gate"][l].reshape(48, 128, 8).transpose(1, 0, 2))
    o["b_gate"] = np.ascontiguousarray(np.broadcast_to(inp["a_b_gate"][l][None, :], (128, 8)))
    o["head_norm"] = np.ascontiguousarray(np.broadcast_to(inp["a_head_norm"][l][None, :], (128, AI)))
    o["skip"] = np.ascontiguousarray(inp["a_skip"][l].reshape(16, 128).T)
    o["gpost"] = np.ascontiguousarray(np.broadcast_to(inp["norm_post"][l][None, :], (128, D)))
    o["w_out"] = np.ascontiguousarray(inp["a_w_out"][l])
    return o


def load_consts(nc, S, st, cdr):
    C = Ctx()
    C.ident = alloc(nc, st, "k_ident", [128, 128], BF16)
    C.tri = alloc(nc, st, "k_tri", [128, 128], F32)
    C.maskb = alloc(nc, st, "k_maskb", [128, 128], F32)
    DMA(S, "pool", C.ident[:], cdr["ident"], writes=["c_ident"])
    DMA(S, "sp", C.tri[:], cdr["tri"], writes=["c_tri"])
    DMA(S, "sp", C.maskb[:], cdr["maskb"], writes=["c_maskb"])
    S.barrier()
    S.emit()
    return C


def build_program(T, host_arrays, n_a_layers=2, n_b_layers=2, out_name="out", debug=False, skip=()):
    nc = bass.Bass("TRN2", target_bir_lowering=False)
    dr = {}
    for name, arr in host_arrays.items():
        dr[name] = nc.dram_tensor(name, list(arr.shape), F32, kind="ExternalInput").ap()
    out = nc.dram_tensor(out_name, [T, D], F32, kind="ExternalOutput").ap()
    NCH = T // 128

    def scratch(name, shape, dt):
        if debug:
            return nc.dram_tensor(name, list(shape), dt, kind="ExternalOutput").ap()
        return nc.dram_tensor(name, list(shape), dt).ap()

    XM = scratch("s_xm", [NCH, 128, 16 * 128], BF16)
    XC = scratch("s_xc", [NCH, 128, 16 * 128], BF16)
    SZ = scratch("s_sz", [NCH, 128, 16 * 128], BF16)
    XM4 = XM.rearrange("c p (j t) -> c p j t", j=16)
    XC4 = XC.rearrange("c p (j t) -> c p j t", j=16)
    SZ4 = SZ.rearrange("c p (j t) -> c p j t", j=16)
    SO = scratch("s_so", [T, AI], BF16)
    G = scratch("s_g", [T, 8], F32)
    nlay = n_a_layers + n_b_layers
    XS = [scratch("s_x%d" % i, [T, D], F32) for i in range(max(nlay - 1, 0))]
    with ExitStack() as gst:
        S = Sched(nc, gst)
        cdr = {k[2:]: v for k, v in dr.items() if k.startswith("c_")}
        C = load_consts(nc, S, gst, cdr)
        xs = dr["x"]
        li = 0
        for l in range(n_a_layers):
            lw = {k[3:]: v for k, v in dr.items() if k.startswith("a%d_" % l)}
            xd = out if li == nlay - 1 else XS[li]
            pass_A1(nc, S, C, T, xs, lw, XM4, XC4, G)
            pass_A2(nc, S, C, T, xs, lw, SZ4, SO)
            pass_B(nc, S, C, T, xs, xd, lw, XM, XC, SZ, SO, G)
            xs = xd
            li += 1
        if n_b_layers > 0:
            N = {k[2:]: v for k, v in dr.items() if k.startswith("n_")}
            kw_ = {k[3:]: v for k, v in dr.items() if k.startswith("kv_")}
            KST = [scratch("s_kst%d" % g, [128, T], BF16) for g in range(2)]
            KWT = [scratch("s_kwt%d" % g, [128, T], BF16) for g in range(2)]
            VS = [scratch("s_vs%d" % g, [T, 128], BF16) for g in range(2)]
            VW = [scratch("s_vw%d" % g, [T, 128], BF16) for g in range(2)]
            KCT = [scratch("s_kct%d" % g, [128, 512], BF16) for g in range(2)]
            VC = [scratch("s_vc%d" % g, [512, 128], BF16) for g in range(2)]
            QT = scratch("s_qt", [NCH, 128, NH * 128], BF16)
            GT = scratch("s_gt", [T, 48], F32)
            SZG = scratch("s_szg", [T, ZW], BF16)
            if "kv" not in skip:
                pass_KV(nc, S, C, T, xs, kw_, KST, KWT, VS, VW, KCT, VC)
            for lb in range(n_b_layers):
                lw = {k[3:]: v for k, v in dr.items() if k.startswith("b%d_" % lb)}
                xd = out if li == nlay - 1 else XS[li]
                if "q" not in skip:
                    pass_Q(nc, S, C, T, xs, lw, QT, GT)
                if "z" not in skip:
                    pass_Z(nc, S, C, T, xs, lw, SZG, 0)
                    pass_Z(nc, S, C, T, xs, lw, SZG, 1)
                if "att" not in skip:
                    pass_ATT(nc, S, C, T, xs, xd, lw, N, QT, GT, SZG, KST, KWT, VS, VW, KCT, VC)
                xs = xd
                li += 1
        print("instructions:", S.ninst)
    return nc


def make_host_arrays(inp, b, T, n_a_layers=2, n_b_layers=2, x_override=None):
    ha = {"x": np.ascontiguousarray(inp["x"][b, :T]) if x_override is None else x_override}
    for k, v in host_consts().items():
        ha["c_" + k] = v
    for l in range(n_a_layers):
        for k, v in layer_a_layout(inp, l).items():
            ha["a%d_%s" % (l, k)] = v
    if n_b_layers > 0:
        for k, v in nsa_consts(T).items():
            ha["n_" + k] = v
        for k, v in nsa_layout(inp).items():
            ha["kv_" + k] = v
        for lb in range(n_b_layers):
            for k, v in layer_b_layout(inp, lb, 2 + lb).items():
                ha["b%d_%s" % (lb, k)] = v
    return ha


_SHARED = ("c_", "a0_", "a1_", "n_", "kv_", "b0_", "b1_")


def kernel(**inputs):
    inp = {k: np.asarray(v) for k, v in inputs.items()}
    T = inp["x"].shape[1]
    base = make_host_arrays(inp, 0, T)
    maps = []
    for b in range(8):
        m = dict(base)
        m["x"] = np.ascontiguousarray(inp["x"][b, :T])
        maps.append(m)
    nc = build_program(T, base)
    res = run_bass_kernel_spmd(nc, maps, core_ids=list(range(8)))
    return np.stack([r["out"] for r in res.results], axis=0).astype(np.float32)


NH = 16
ZW = 6144
BIG = 1.0e30


def nsa_consts(T):
    c = {}
    i = np.arange(128)
    kk = np.arange(128)
    c["cdiag"] = (kk[:, None] <= i[None, :]).astype(np.float32)
    c["cwin"] = (kk[:, None] > i[None, :]).astype(np.float32)
    h = np.arange(1, NH + 1, dtype=np.float64)
    slopes = 2.0 ** (-8.0 * h / NH)
    rel = np.arange(-63, 1)
    ab = slopes[None, None, :] * (128.0 * rel[None, :, None] + kk[:, None, None] - 63.5)
    c["ab"] = ab.astype(np.float32)
    abc = slopes[None, None, :] * (128.0 * rel[None, :, None] + 16.0 * kk[:, None, None] - 48.0)
    c["abc"] = np.minimum(abc, 40.0).astype(np.float32)
    cm = np.zeros((128, 17, 128), np.float32)
    for r in range(-16, 1):
        cm[:, r + 16, :] = ((16 * kk[:, None] + 31 - i[None, :]) <= (-128 * r)).astype(np.float32)
    c["cm"] = cm
    wov = np.zeros((128, 33), np.float32)
    for n in range(128):
        for j in range(33):
            ov = min(16 * n + 32, 64 * j + 64) - max(16 * n, 64 * j)
            wov[n, j] = max(ov, 0) / 16.0
    c["wov"] = wov
    vm = np.zeros((128, 256), np.float32)
    fb = np.zeros((128, 256), np.float32)
    for ii in range(128):
        cur = 1 if ii >= 64 else 0
        for col in range(256):
            jr = col - 127
            if jr == cur or jr == cur - 1:
                fb[ii, col] = BIG
            elif jr < cur:
                vm[ii, col] = 1.0
            else:
                fb[ii, col] = -BIG
    c["vm"] = vm
    c["fb"] = fb
    return c


def nsa_layout(inp):
    o = {}
    o["kv_w"] = np.ascontiguousarray(inp["kv_w"])
    o["kv_gf"] = np.ascontiguousarray(inp["kv_norm"].reshape(8, 128).T)
    for s_ in range(2):
        o["w1_%d" % s_] = np.ascontiguousarray(inp["cmp_w1"][s_])
        o["posT_%d" % s_] = np.ascontiguousarray(inp["cmp_pos"][s_].T)
        o["b1_%d" % s_] = np.ascontiguousarray(inp["cmp_b1"][s_].reshape(2, 128).T)
        o["w2_%d" % s_] = np.ascontiguousarray(inp["cmp_w2"][s_])
    o["b2k"] = np.ascontiguousarray(inp["cmp_b2"][0].reshape(128, 1))
    o["b2v"] = np.ascontiguousarray(np.broadcast_to(inp["cmp_b2"][1][None, :], (128, 128)))
    return o


def layer_b_layout(inp, lb, layer):
    o = {}
    o["w_in"] = np.ascontiguousarray(inp["b_w_in"][lb])
    o["gfold"] = np.ascontiguousarray(inp["norm_pre"][layer].reshape(8, 128).T)
    o["w_out"] = np.ascontiguousarray(inp["b_w_out"][lb])
    o["gpost"] = np.ascontiguousarray(np.broadcast_to(inp["norm_post"][layer][None, :], (128, D)))
    return o


def load_w(S, W, gf, wsrc, gsrc, c0, c1):
    DMA(S, "pool", W[:], wsrc.rearrange("(k p) c -> p k c", p=128)[:, :, c0:c1], writes=["W"])
    DMA(S, "sp", gf[:], gsrc, writes=["gf"])
    for k in range(8):
        TS(S, "dve", W[:, k, :], W[:, k, :], gf[:, k:k + 1], None, ALU.mult, None, ["W", "gf"], ["W"])


def pass_KV(nc, S, C, T, xsrc, kw_, KST, KWT, VS, VW, KCT, VC):
    NST = T // 512
    NC = T // 16 - 1
    NNT = (NC + 127) // 128
    with ExitStack() as st:
        R = alloc_normT(nc, st, C)
        W = alloc(nc, st, "kv_w", [128, 8, 1536], BF16)
        gf = alloc(nc, st, "kv_gf", [128, 8], F32)
        aT = [alloc(nc, st, "kv_aT%d" % i, [128, T], BF16) for i in range(4)]
        kst = [alloc(nc, st, "kv_kst%d" % i, [128, 4, 512], BF16) for i in range(2)]
        vst = [alloc(nc, st, "kv_vst%d" % i, [128, 4, 4, 128], BF16) for i in range(2)]
        pm = [palloc(nc, st, "kv_pm%d" % i, [128, 512], F32) for i in range(4)]
        load_w(S, W, gf, kw_["kv_w"], kw_["kv_gf"], 0, 1536)
        load_x(S, R, xsrc, 0)
        n = 0
        for s_ in range(NST):
            b = s_ % 2
            if s_ + 1 < NST:
                load_x(S, R, xsrc, s_ + 1)
            hT, hk = norm_T(S, R, s_)
            fi = 0
            for cb in (0, 1, 2, 3, 4, 5, 8, 9):
                p = pm[n % 4]
                pk = "PS:pm%d" % (n % 4)
                n += 1
                for k in range(8):
                    MM(S, p[:], W[:, k, cb * 128:(cb + 1) * 128], hT[:, k, :], k == 0, k == 7, ["W", hk[k]], [pk])
                if cb < 4:
                    CP(S, "act" if cb % 2 == 0 else "dve", aT[cb][:, s_ * 512:(s_ + 1) * 512], p[:], [pk], ["aT%d" % cb])
                else:
                    CP(S, "act" if cb % 2 == 0 else "dve", kst[b][:, fi, :], p[:], [pk], ["kst%d" % b])
                    fi += 1
            for a in range(4):
                p = pm[n % 4]
                pk = "PS:pm%d" % (n % 4)
                n += 1
                for bi, cb in enumerate((6, 7, 10, 11)):
                    for k in range(8):
                        MM(S, p[:, bi * 128:(bi + 1) * 128], hT[:, k, a * 128:(a + 1) * 128], W[:, k, cb * 128:(cb + 1) * 128],
                           k == 0, k == 7, ["W", hk[k]], [pk], inc=(k == 7 and bi == 3))
                CP(S, "act" if a % 2 == 0 else "dve", vst[b][:, a, :, :], p[:].rearrange("p (b d) -> p b d", b=4), [pk],
                   ["vst%d" % b])
            sl = slice(s_ * 512, (s_ + 1) * 512)
            for g in range(2):
                DMA(S, "sp", KST[g][:, sl], kst[b][:, g, :], reads=["kst%d" % b])
                DMA(S, "sp", KWT[g][:, sl], kst[b][:, 2 + g, :], reads=["kst%d" % b])
                DMA(S, "sp", VS[g][sl, :].rearrange("(a p) d -> p a d", p=128), vst[b][:, :, g, :], reads=["vst%d" % b])
                DMA(S, "sp", VW[g][sl, :].rearrange("(a p) d -> p a d", p=128), vst[b][:, :, 2 + g, :], reads=["vst%d" % b])
        w1 = alloc(nc, st, "kv_w1", [128, 32, 256], BF16)
        w2 = alloc(nc, st, "kv_w2", [128, 2, 128], BF16)
        posT = alloc(nc, st, "kv_posT", [128, 32], BF16)
        b1 = alloc(nc, st, "kv_b1", [128, 2], F32)
        c1 = alloc(nc, st, "kv_c1", [128, 2], F32)
        b2k = alloc(nc, st, "kv_b2k", [128, 1], F32)
        b2v = alloc(nc, st, "kv_b2v", [128, 128], F32)
        hid = alloc(nc, st, "kv_hid", [128, 2, 512], BF16)
        kco = alloc(nc, st, "kv_kco", [128, 512], BF16)
        vco = alloc(nc, st, "kv_vco", [128, 4, 128], BF16)
        DMA(S, "sp", b2k[:], kw_["b2k"], writes=["b2k"])
        DMA(S, "sp", b2v[:], kw_["b2v"], writes=["b2v"])
        S.op("pool", lambda e: e.memset(hid[:], 0.0), [], ["hid"])
        S.op("pool", lambda e: e.memset(kco[:], 0.0), [], ["kco"])
        for slot in range(2):
            DMA(S, "pool", w1[:], kw_["w1_%d" % slot].rearrange("(i d) m -> d i m", d=128), writes=["w1"])
            DMA(S, "pool", w2[:], kw_["w2_%d" % slot].rearrange("(c m) d -> m c d", m=128), writes=["w2"])
            DMA(S, "pool", posT[:], kw_["posT_%d" % slot], writes=["posT"])
            DMA(S, "sp", b1[:], kw_["b1_%d" % slot], writes=["b1"])
            for mc in range(2):
                p = pm[n % 4]
                pk = "PS:pm%d" % (n % 4)
                n += 1
                for i in range(32):
                    MM(S, p[:, 0:1], w1[:, i, mc * 128:(mc + 1) * 128], posT[:, i:i + 1], i == 0, i == 31, ["w1", "posT"], [pk])
                TT(S, "dve", c1[:, mc:mc + 1], p[:, 0:1], b1[:, mc:mc + 1], ALU.add, [pk, "b1"], ["c1"])
            for g in range(2):
                src = aT[slot * 2 + g]
                for mc in range(2):
                    p = pm[n % 4]
                    pk = "PS:pm%d" % (n % 4)
                    n += 1
                    for i in range(32):
                        MM(S, p[:, 0:NC], w1[:, i, mc * 128:(mc + 1) * 128], src[:, i:i + 16 * (NC - 1) + 1:16], i == 0, i == 31,
                           ["w1", "aT%d" % (slot * 2 + g)], [pk])
                    ACT(S, hid[:, mc, 0:NC], p[:, 0:NC], AF.Silu, [pk, "c1"], ["hid"], bias=c1[:, mc:mc + 1])
                if slot == 0:
                    p = pm[n % 4]
                    pk = "PS:pm%d" % (n % 4)
                    n += 1
                    for mc in range(2):
                        MM(S, p[:, 0:NC], w2[:, mc, :], hid[:, mc, 0:NC], mc == 0, mc == 1, ["w2", "hid"], [pk])
                    TS(S, "dve", kco[:, 0:NC], p[:, 0:NC], b2k[:, 0:1], None, ALU.add, None, [pk, "b2k"], ["kco"])
                    DMA(S, "sp", KCT[g], kco[:], reads=["kco"])
                else:
                    p = pm[n % 4]
                    pk = "PS:pm%d" % (n % 4)
                    n += 1
                    for nt in range(NNT):
                        for mc in range(2):
                            MM(S, p[:, nt * 128:(nt + 1) * 128], hid[:, mc, nt * 128:(nt + 1) * 128], w2[:, mc, :], mc == 0, mc == 1,
                               ["w2", "hid"], [pk], inc=(mc == 1 and nt == NNT - 1))
                    TT(S, "dve", vco[:, 0:NNT, :], p[:, 0:NNT * 128].rearrange("p (t d) -> p t d", d=128),
                       b2v[:].unsqueeze(1).to_broadcast([128, NNT, 128]), ALU.add, [pk, "b2v"], ["vco"])
                    DMA(S, "sp", VC[g][0:NNT * 128, :].rearrange("(t p) d -> p t d", p=128), vco[:, 0:NNT, :], reads=["vco"])
        S.barrier()
        S.emit()


def pass_Q(nc, S, C, T, xsrc, lw, QT, GT):
    NST = T // 512
    with ExitStack() as st:
        R = alloc_normT(nc, st, C)
        W = alloc(nc, st, "q_w", [128, 8, 2096], BF16)
        gf = alloc(nc, st, "q_gf", [128, 8], F32)
        q_st = [alloc(nc, st, "q_st%d" % i, [128, 4, 16, 128], BF16) for i in range(2)]
        g_st = [alloc(nc, st, "q_gst%d" % i, [128, 4, 48], F32) for i in range(2)]
        pm = [palloc(nc, st, "q_pm%d" % i, [128, 512], F32) for i in range(4)]
        pgf = palloc(nc, st, "q_pg", [128, 512], F32)
        pg = pgf[:, 0:192].rearrange("p (a g) -> p a g", a=4)
        load_w(S, W, gf, lw["w_in"], lw["gfold"], 0, 2096)
        load_x(S, R, xsrc, 0)
        n = 0
        for s_ in range(NST):
            b = s_ % 2
            if s_ + 1 < NST:
                load_x(S, R, xsrc, s_ + 1)
            hT, hk = norm_T(S, R, s_)
            for hd in range(NH):
                p = pm[n % 4]
                pk = "PS:pm%d" % (n % 4)
                n += 1
                for k in range(8):
                    MM(S, p[:], W[:, k, hd * 128:(hd + 1) * 128], hT[:, k, :], k == 0, k == 7, ["W", hk[k]], [pk])
                S.op("act", lambda e, p=p, hd=hd, b=b: e.mul(out=q_st[b][:, :, hd, :],
                                                            in_=p[:].rearrange("p (a t) -> p a t", a=4), mul=128.0 ** -0.5),
                     [pk], ["qst%d" % b])
            for a in range(4):
                for k in range(8):
                    MM(S, pg[:, a, :], hT[:, k, a * 128:(a + 1) * 128], W[:, k, 2048:2096], k == 0, k == 7, ["W", hk[k]],
                       ["PS:pg"], inc=(k == 7 and a == 3))
            ACT(S, g_st[b][:], pg[:], AF.Sigmoid, ["PS:pg"], ["gst%d" % b])
            DMA(S, "sp", QT[s_ * 4:(s_ + 1) * 4].rearrange("a p (j t) -> p a j t", j=16), q_st[b][:], reads=["qst%d" % b])
            DMA(S, "sp", GT[s_ * 512:(s_ + 1) * 512, :].rearrange("(a p) g -> p a g", p=128), g_st[b][:], reads=["gst%d" % b])
        S.barrier()
        S.emit()


def pass_Z(nc, S, C, T, xsrc, lw, SZG, hz):
    NST = T // 512
    with ExitStack() as st:
        R = alloc_normT(nc, st, C)
        W = alloc(nc, st, "z_w", [128, 8, 3072], BF16)
        gf = alloc(nc, st, "z_gf", [128, 8], F32)
        z_st = [alloc(nc, st, "z_st%d" % i, [128, 4, 3072], BF16) for i in range(2)]
        pm = [palloc(nc, st, "z_pm%d" % i, [128, 512], F32) for i in range(4)]
        c0 = 2096 + hz * 3072
        load_w(S, W, gf, lw["w_in"], lw["gfold"], c0, c0 + 3072)
        load_x(S, R, xsrc, 0)
        n = 0
        for s_ in range(NST):
            b = s_ % 2
            if s_ + 1 < NST:
                load_x(S, R, xsrc, s_ + 1)
            hT, hk = norm_T(S, R, s_)
            for a in range(4):
                for cg in range(6):
                    p = pm[n % 4]
                    pk = "PS:pm%d" % (n % 4)
                    n += 1
                    for k in range(8):
                        MM(S, p[:], hT[:, k, a * 128:(a + 1) * 128], W[:, k, cg * 512:(cg + 1) * 512], k == 0, k == 7,
                           ["W", hk[k]], [pk])
                    ACT(S, z_st[b][:, a, cg * 512:(cg + 1) * 512], p[:], AF.Silu, [pk], ["zst%d" % b])
            DMA(S, "sp", SZG[s_ * 512:(s_ + 1) * 512, hz * 3072:(hz + 1) * 3072].rearrange("(a p) c -> p a c", p=128),
                z_st[b][:], reads=["zst%d" % b])
        S.barrier()
        S.emit()


def pass_ATT(nc, S, C, T, xsrc, xdst, lw, N, QT, GT, SZG, KST, KWT, VS, VW, KCT, VC):
    NQT = T // 128
    NC = T // 16 - 1
    with ExitStack() as st:
        P = alloc_post(nc, st)
        kst = [alloc(nc, st, "at_kst%d" % g, [128, T], BF16) for g in range(2)]
        vsa = [alloc(nc, st, "at_vsa%d" % g, [128, NQT, 129], BF16) for g in range(2)]
        wout = alloc(nc, st, "at_wout", [128, 16, D], BF16)
        gpost = alloc(nc, st, "at_gpost", [128, D], F32)
        kct = [alloc(nc, st, "at_kct%d" % g, [128, 512], BF16) for g in range(2)]
        rc = [alloc(nc, st, "at_rc%d" % g, [128, 4, 161], BF16) for g in range(2)]
        ab = alloc(nc, st, "at_ab", [128, 64, NH], F32)
        abc = alloc(nc, st, "at_abc", [128, 64, NH], F32)
        cm = alloc(nc, st, "at_cm", [128, 17, 128], BF16)
        cdiag = alloc(nc, st, "at_cdiag", [128, 128], BF16)
        cwin = alloc(nc, st, "at_cwin", [128, 128], BF16)
        vm = alloc(nc, st, "at_vm", [128, 256], F32)
        fb = alloc(nc, st, "at_fb", [128, 256], F32)
        mk_all = alloc(nc, st, "at_mkall", [128, NQT, 128], BF16)
        qt_sb = alloc(nc, st, "at_q", [128, NH, 128], BF16)
        szg = alloc(nc, st, "at_szg", [128, ZW], BF16)
        gts = alloc(nc, st, "at_gt", [128, 48], F32)
        xin = alloc(nc, st, "at_xin", [128, D], F32)
        xout = xin
        pT = [alloc(nc, st, "at_pT%d" % i, [128, 8, 128], BF16) for i in range(3)]
        kwin = [alloc(nc, st, "at_kwin%d" % i, [128, 640], BF16) for i in range(2)]
        vwin = [alloc(nc, st, "at_vwin%d" % i, [128, 5, 129], BF16) for i in range(2)]
        ocmp = alloc(nc, st, "at_ocmp", [128, 8, 128], F32)
        impu = alloc(nc, st, "at_impu", [128, 8, 4, 33], F32)
        impn = alloc(nc, st, "at_impn", [128, 4, 33], F32)
        imp = alloc(nc, st, "at_imp", [128, 160], F32)
        impm = alloc(nc, st, "at_impm", [128, 128], F32)
        imp2 = alloc(nc, st, "at_imp2", [128, 128], F32)
        m8 = alloc(nc, st, "at_m8", [128, 16], F32)
        thr = alloc(nc, st, "at_thr", [128, 1], F32)
        mask = alloc(nc, st, "at_mask", [128, 128], BF16)
        mrep = alloc(nc, st, "at_mrep", [128, 8, 64], BF16)
        zc = alloc(nc, st, "at_zc", [128, 8], F32)
        coef = alloc(nc, st, "at_coef", [128, 8], F32)
        osum = alloc(nc, st, "at_osum", [128, 8, 128], F32)
        otmp = alloc(nc, st, "at_otmp", [128, 8, 128], F32)
        oall = alloc(nc, st, "at_oall", [128, 2 * 1024], BF16)
        oT = alloc(nc, st, "at_oT", [128, 16, 128], BF16)
        sT = [palloc(nc, st, "at_sT%d" % i, [128, 1024], F32) for i in range(2)]
        oa = [palloc(nc, st, "at_oa%d" % i, [128, 512], F32) for i in range(3)]
        pmisc = palloc(nc, st, "at_misc", [128, 1024], BF16)

        def oav(hd, w):
            return oa[hd // 3][:, (hd % 3) * 161:(hd % 3) * 161 + w]

        def oak(hd):
            return "PS:oa%d" % (hd // 3)

        for g in range(2):
            DMA(S, "sp", kst[g][:], KST[g], writes=["kst%d" % g])
            S.op("pool", lambda e, g=g: e.memset(vsa[g][:, :, 128:129], 1.0), [], ["vsa1_%d" % g])
            for t4 in range(0, NQT, 16):
                t5 = min(t4 + 16, NQT)
                DMA(S, "sp", vsa[g][:, t4:t5, 0:128], VS[g][t4 * 128:t5 * 128, :].rearrange("(t p) d -> p t d", p=128),
                    writes=["vsa%d" % g])
            DMA(S, "sp", kct[g][:], KCT[g], writes=["kct%d" % g])
            DMA(S, "sp", rc[g][:, :, 0:128], VC[g].rearrange("(t p) d -> p t d", p=128), writes=["rc%d" % g])
            for nt in range(4):
                DMA(S, "pool", rc[g][:, nt, 128:161], N["wov"], writes=["rcw%d" % g])
        for i_ in range(2):
            S.op("pool", lambda e, i_=i_: e.memset(vwin[i_][:, :, 128:129], 1.0), [], ["vwin1_%d" % i_])
        DMA(S, "pool", wout[:], lw["w_out"].rearrange("(j p) c -> p j c", p=128), writes=["wout"])
        DMA(S, "sp", gpost[:], lw["gpost"], writes=["gpost"])
        DMA(S, "sp", ab[:], N["ab"], writes=["ab"])
        DMA(S, "sp", abc[:], N["abc"], writes=["abc"])
        DMA(S, "pool", cm[:], N["cm"], writes=["cm"])
        DMA(S, "pool", cdiag[:], N["cdiag"], writes=["cdiag"])
        DMA(S, "pool", cwin[:], N["cwin"], writes=["cwin"])
        DMA(S, "sp", vm[:], N["vm"], writes=["vm"])
        DMA(S, "sp", fb[:], N["fb"], writes=["fb"])
        consts_k = ["ab", "abc", "cm", "cdiag", "cwin"]
        st_ctr = [0]
        pt_ctr = [0]

        def att_step(g, lhsT_k, kkeys, bias_tab, mask_ap, mkeys, rhs_v, vkeys, ncol, first, last):
            si = st_ctr[0] % 2
            st_ctr[0] += 1
            pi = pt_ctr[0] % 3
            pt_ctr[0] += 1
            sk = "PS:sT%d" % si
            for hf in range(2):
                MM(S, sT[si][:, hf * 512:(hf + 1) * 512], lhsT_k,
                   qt_sb[:, g * 8 + hf * 4:g * 8 + hf * 4 + 4, :].rearrange("p h t -> p (h t)"), True, True,
                   kkeys + ["qt"], [sk], inc=(hf == 1))
            for hh in range(8):
                ACT(S, pT[pi][:, hh, :], sT[si][:, hh * 128:(hh + 1) * 128], AF.Exp, [sk] + consts_k, ["pT%d" % pi],
                    bias=bias_tab[:, g * 8 + hh:g * 8 + hh + 1])
            if mask_ap is not None:
                TT(S, "dve", pT[pi][:], pT[pi][:], mask_ap.unsqueeze(1).to_broadcast([128, 8, 128]), ALU.mult,
                   ["pT%d" % pi] + mkeys, ["pT%d" % pi])
            for hh in range(8):
                first_in_bank = (hh % 3 == 0)
                S.op("pe", lambda e, hh=hh, fib=first_in_bank: e.matmul(
                    oav(hh, ncol), lhsT=pT[pi][:, hh, :], rhs=rhs_v, start=(first and fib), stop=last,
                    skip_group_check=True), ["pT%d" % pi] + vkeys, [oak(hh)], inc=(hh == 7 or hh % 3 == 2))

        def combine(g, br, src_is_psum, accumulate):
            if src_is_psum:
                for bk in range(3):
                    nh_ = 3 if bk < 2 else 2
                    CP(S, "dve", zc[:, bk * 3:bk * 3 + nh_],
                       oa[bk][:, 0:nh_ * 161].rearrange("p (h c) -> p h c", c=161)[:, :, 128], ["PS:oa%d" % bk], ["zc"])
            TS(S, "dve", zc[:], zc[:], 1e-30, None, ALU.max, None, ["zc"], ["zc"])
            S.op("dve", lambda e: e.reciprocal(out=zc[:], in_=zc[:]), ["zc"], ["zc"])
            gsl = gts[:, (br * 2 + g) * 8:(br * 2 + g) * 8 + 8]
            TT(S, "dve", coef[:], zc[:], gsl, ALU.mult, ["zc", "gts"], ["coef"])
            dst = otmp if accumulate else osum
            dk = "otmp" if accumulate else "osum"
            for hh in range(8):
                src = oav(hh, 128) if src_is_psum else ocmp[:, hh, :]
                sk_ = oak(hh) if src_is_psum else "ocmp"
                zo = ((br * 2 + g) * 8 + hh) * 128
                STT(S, "dve", dst[:, hh, :], src, coef[:, hh:hh + 1], szg[:, zo:zo + 128], ALU.mult, ALU.mult,
                    [sk_, "coef", "szg"], [dk])
            if accumulate:
                TT(S, "pool", osum[:], osum[:], otmp[:], ALU.add, ["osum", "otmp"], ["osum"])

        for qt in range(NQT):
            t0 = qt * 128
            DMA(S, "sp", qt_sb[:].rearrange("p h t -> p (h t)"), QT[qt], writes=["qt"])
            DMA(S, "sp", szg[:], SZG[t0:t0 + 128, :], writes=["szg"])
            DMA(S, "sp", gts[:], GT[t0:t0 + 128, :], writes=["gts"])
            DMA(S, "sp", xin[:], xsrc[t0:t0 + 128, :], writes=["xin"])
            for g in range(2):
                wb = (qt * 2 + g) % 2
                k0 = max(qt - 4, 0)
                nwt = qt - k0 + 1
                DMA(S, "sp", kwin[wb][:, 0:nwt * 128], KWT[g][:, k0 * 128:(qt + 1) * 128], writes=["kwin%d" % wb])
                DMA(S, "sp", vwin[wb][:, 0:nwt, 0:128], VW[g][k0 * 128:(qt + 1) * 128, :].rearrange("(t p) d -> p t d", p=128),
                    writes=["vwin%d" % wb])
                nts = [nt for nt in range(4) if 16 * nt <= qt and nt * 128 < NC]
                S.op("pool", lambda e: e.memset(impu[:], 0.0), [], ["impu"])
                S.op("pool", lambda e: e.memset(ocmp[:], 0.0), [], ["ocmp"])
                for nt in nts:
                    relc = 16 * nt - qt
                    m_ap = cm[:, relc + 16, :] if relc >= -16 else None
                    att_step(g, kct[g][:, nt * 128:(nt + 1) * 128], ["kct%d" % g], abc[:, relc + 63, :], m_ap, ["cm"],
                             rc[g][:, nt, :], ["rc%d" % g, "rcw%d" % g], 161, True, True)
                    for bk in range(3):
                        nh_ = 3 if bk < 2 else 2
                        v3 = oa[bk][:, 0:nh_ * 161].rearrange("p (h c) -> p h c", c=161)
                        TT(S, "dve", ocmp[:, bk * 3:bk * 3 + nh_, :], ocmp[:, bk * 3:bk * 3 + nh_, :], v3[:, :, 0:128], ALU.add,
                           ["ocmp", "PS:oa%d" % bk], ["ocmp"])
                        CP(S, "dve", impu[:, bk * 3:bk * 3 + nh_, nt, :], v3[:, :, 128:161], ["PS:oa%d" % bk], ["impu"])
                S.op("dve", lambda e: e.reduce_sum(out=zc[:], in_=impu[:].rearrange("p h n j -> p h (n j)"),
                                                   axis=mybir.AxisListType.X), ["impu"], ["zc"])
                TS(S, "dve", zc[:], zc[:], 0.5, None, ALU.mult, None, ["zc"], ["zc"])
                combine(g, 0, False, False)
                S.op("pool", lambda e: e.memset(impn[:], 0.0), [], ["impn"])
                for hh in range(8):
                    STT(S, "dve", impn[:], impu[:, hh, :, :], zc[:, hh:hh + 1], impn[:], ALU.mult, ALU.add,
                        ["impu", "zc", "impn"], ["impn"])
                S.op("pool", lambda e: e.memset(imp[:], 0.0), [], ["imp"])
                for nt in nts:
                    TT(S, "dve", imp[:, 32 * nt:32 * nt + 33], imp[:, 32 * nt:32 * nt + 33], impn[:, nt, :], ALU.add,
                       ["imp", "impn"], ["imp"])
                c0 = 127 - 2 * qt
                TT(S, "dve", impm[:], imp[:, 0:128], vm[:, c0:c0 + 128], ALU.mult, ["imp", "vm"], ["impm"])
                TT(S, "dve", impm[:], impm[:], fb[:, c0:c0 + 128], ALU.add, ["impm", "fb"], ["impm"])
                S.op("dve", lambda e: e.memset(impm[:, 0:1], BIG), [], ["impm"])
                S.op("dve", lambda e: e.max(out=m8[:, 0:8], in_=impm[:]), ["impm"], ["m8"])
                S.op("dve", lambda e: e.match_replace(out=imp2[:], in_to_replace=m8[:, 0:8], in_values=impm[:],
                                                      imm_value=-3.0e38), ["impm", "m8"], ["imp2"])
                S.op("dve", lambda e: e.max(out=m8[:, 8:16], in_=imp2[:]), ["imp2"], ["m8"])
                S.op("dve", lambda e: e.tensor_reduce(out=thr[:], in_=m8[:, 8:16], axis=mybir.AxisListType.X, op=ALU.min),
                     ["m8"], ["thr"])
                TS(S, "dve", mask[:], impm[:], thr[:, 0:1], None, ALU.is_ge, None, ["impm", "thr"], ["mask"])
                for kb in range(0, qt, 4):
                    nk = min(4, qt - kb)
                    CP(S, "dve", mrep[:, 0:2 * nk, :], mask[:, 2 * kb:2 * kb + 2 * nk].unsqueeze(2).to_broadcast([128, 2 * nk, 64]),
                       ["mask"], ["mrep"])
                    si = st_ctr[0] % 2
                    st_ctr[0] += 1
                    for s4 in range(nk):
                        MM(S, sT[si][:, s4 * 128:(s4 + 1) * 128], mrep[:, 2 * s4:2 * s4 + 2, :].rearrange("p a b -> p (a b)"),
                           C.ident[:], True, True, ["mrep"], ["PS:sT%d" % si], inc=(s4 == nk - 1))
                    CP(S, "act", mk_all[:, kb:kb + nk, :], sT[si][:, 0:nk * 128].rearrange("p (k i) -> p k i", i=128),
                       ["PS:sT%d" % si], ["mk_all"])
                for kt in range(qt + 1):
                    m_ap = cdiag[:] if kt == qt else mk_all[:, kt, :]
                    att_step(g, kst[g][:, kt * 128:(kt + 1) * 128], ["kst%d" % g], ab[:, kt - qt + 63, :], m_ap,
                             ["cdiag", "mk_all"], vsa[g][:, kt, :], ["vsa%d" % g, "vsa1_%d" % g], 129, kt == 0, kt == qt)
                combine(g, 1, True, True)
                for wi in range(nwt):
                    kt = k0 + wi
                    if kt == qt:
                        m_ap = cdiag[:]
                    elif kt == qt - 4:
                        m_ap = cwin[:]
                    else:
                        m_ap = None
                    att_step(g, kwin[wb][:, wi * 128:(wi + 1) * 128], ["kwin%d" % wb], ab[:, kt - qt + 63, :], m_ap,
                             ["cdiag", "cwin"], vwin[wb][:, wi, :], ["vwin%d" % wb, "vwin1_%d" % wb], 129, wi == 0, wi == nwt - 1)
                combine(g, 2, True, True)
                CP(S, "act", oall[:, g * 1024:(g + 1) * 1024], osum[:].rearrange("p h d -> p (h d)"), ["osum"], ["oall%d" % g])
            for half in range(2):
                for jj in range(8):
                    j = half * 8 + jj
                    TR(S, pmisc[:, jj * 128:(jj + 1) * 128], oall[:, j * 128:(j + 1) * 128], C.ident[:], ["oall%d" % (j // 8)],
                       ["PS:misc"], inc=(jj == 7))
                CP(S, "dve", oT[:, half * 8:(half + 1) * 8, :], pmisc[:].rearrange("p (j t) -> p j t", t=128), ["PS:misc"],
                   ["oT%d" % half])
            si = st_ctr[0] % 2
            st_ctr[0] += 1
            py = [sT[si][:, 0:512], sT[si][:, 512:1024]]
            for hh in range(2):
                for j in range(16):
                    MM(S, py[hh], oT[:, j, :], wout[:, j, hh * 512:(hh + 1) * 512], j == 0, j == 15,
                       ["oT%d" % (j // 8), "wout"], ["PS:sT%d" % si])
            post_residual(S, P, py, ["PS:sT%d" % si] * 2, xin, "xin", gpost, xout, "xin")
            DMA(S, "sp", xdst[t0:t0 + 128, :], xout[:], reads=["xin"])
        S.barrier()
        S.emit()
```

```python
import numpy as np
from contextlib import ExitStack
import concourse.bass as bass
import concourse.mybir as mybir
from concourse.bass_utils import run_bass_kernel_spmd

F32 = mybir.dt.float32
BF16 = mybir.dt.bfloat16
AF = mybir.ActivationFunctionType
ALU = mybir.AluOpType
EPS = 1e-6
D = 1024
AI = 2048
NEG = -30000.0


class Sched:
    ENG = ("pe", "act", "dve", "pool", "sp")

    def __init__(self, nc, stack, ndsem=16):
        self.nc = nc
        self.sem = {e: stack.enter_context(nc.semaphore("sem_" + e)) for e in self.ENG}
        self.cnt = {e: 0 for e in self.ENG}
        self.waited = {e: {} for e in self.ENG}
        self.stream = {e: [] for e in self.ENG}
        self.lastw = {}
        self.reads = {}
        self.dsems = [stack.enter_context(nc.semaphore("semd%d" % i)) for i in range(ndsem)]
        self.dsem_cnt = [0] * ndsem
        self.dsem_rr = 0
        self.ninst = 0

    def _deps(self, eng, reads, writes):
        deps = []
        for r in reads:
            w = self.lastw.get(r)
            if w is not None:
                deps.append(w)
        for w_ in writes:
            w = self.lastw.get(w_)
            if w is not None and w[2] != eng:
                deps.append(w)
            for rd in self.reads.get(w_, {}).values():
                if rd[2] != eng:
                    deps.append(rd)
        out = {}
        for (s, v, tag) in deps:
            if tag == "pe" and eng == "pe":
                continue
            key = id(s)
            if v <= self.waited[eng].get(key, 0):
                continue
            if key not in out or out[key][1] < v:
                out[key] = (s, v)
        for key, (s, v) in out.items():
            self.waited[eng][key] = v
        return list(out.values())

    def _record(self, token, reads, writes):
        for r in reads:
            self.reads.setdefault(r, {})[token[2]] = token
        for w in writes:
            self.lastw[w] = token
            self.reads[w] = {}

    @staticmethod
    def _excl(reads, writes):
        ex = [k for k in reads if k.startswith("PS:")]
        if ex:
            writes = list(writes) + ex
            reads = [k for k in reads if not k.startswith("PS:")]
        return reads, writes

    def op(self, eng, fn, reads=(), writes=(), inc=True):
        reads, writes = self._excl(reads, writes)
        waits = self._deps(eng, reads, writes)
        for (s, v) in waits:
            if s is self.sem["pe"] and v > self.cnt["pe"]:
                raise RuntimeError("wait on un-signalled PE ticket")
        ticket = self.cnt[eng] + 1
        if inc:
            self.cnt[eng] = ticket
        else:
            assert eng == "pe"
        self.stream[eng].append((waits, fn, (self.sem[eng], 1) if inc else None))
        self._record((self.sem[eng], ticket, eng), reads, writes)
        self.ninst += 1

    def dma(self, eng, fn, reads=(), writes=()):
        i = self.dsem_rr
        self.dsem_rr = (self.dsem_rr + 1) % len(self.dsems)
        s = self.dsems[i]
        waits = self._deps(eng, reads, writes)
        prev = self.dsem_cnt[i]
        if prev > self.waited[eng].get(id(s), 0):
            waits = [w for w in waits if w[0] is not s] + [(s, prev)]
            self.waited[eng][id(s)] = prev
        self.dsem_cnt[i] = prev + 16
        self.stream[eng].append((waits, fn, (s, 16)))
        self._record((s, prev + 16, "d%d" % i), reads, writes)
        self.ninst += 1

    def barrier(self):
        assert all(it[2] is not None or it[1] is None for it in self.stream["pe"][-1:]), "last PE op must inc"
        for e in self.ENG:
            waits = []
            for e2 in self.ENG:
                s = self.sem[e2]
                if e2 != e and self.cnt[e2] > self.waited[e].get(id(s), 0):
                    waits.append((s, self.cnt[e2]))
                    self.waited[e][id(s)] = self.cnt[e2]
            for i, s in enumerate(self.dsems):
                if self.dsem_cnt[i] > self.waited[e].get(id(s), 0):
                    waits.append((s, self.dsem_cnt[i]))
                    self.waited[e][id(s)] = self.dsem_cnt[i]
            self.stream[e].append((waits, None, None))
        self.lastw.clear()
        self.reads.clear()

    def simulate(self, streams):
        if not hasattr(self, "simval"):
            self.simval = {}
        val = self.simval
        pos = {e: 0 for e in self.ENG}
        progress = True
        while progress:
            progress = False
            for e in self.ENG:
                items = streams[e]
                while pos[e] < len(items):
                    waits, fn, inc = items[pos[e]]
                    if any(val.get(id(s), 0) < v for (s, v) in waits):
                        break
                    if inc is not None:
                        val[id(inc[0])] = val.get(id(inc[0]), 0) + inc[1]
                    pos[e] += 1
                    progress = True
        for e in self.ENG:
            if pos[e] < len(streams[e]):
                waits, fn, inc = streams[e][pos[e]]
                bad = [(v, val.get(id(s), 0)) for (s, v) in waits if val.get(id(s), 0) < v]
                raise RuntimeError("DEADLOCK engine %s at item %d/%d waits(need,have)=%s" % (e, pos[e], len(streams[e]), bad))

    def emit(self):
        nc = self.nc
        streams = self.stream
        self.stream = {e: [] for e in self.ENG}
        self.simulate(streams)

        def run(engobj, items):
            for (waits, fn, inc) in items:
                for (s, v) in waits:
                    engobj.wait_ge(s, v)
                if fn is not None:
                    ins = fn(engobj)
                    if inc is not None:
                        ins.then_inc(inc[0], inc[1])

        with nc.Block() as block:
            @block.tensor
            def _(e):
                run(e, streams["pe"])

            @block.scalar
            def _(e):
                run(e, streams["act"])

            @block.vector
            def _(e):
                run(e, streams["dve"])

            @block.gpsimd
            def _(e):
                run(e, streams["pool"])

            @block.sync
            def _(e):
                run(e, streams["sp"])


def MM(S, out, lhsT, rhs, start, stop, reads, writes, inc=None):
    if inc is None:
        inc = stop
    S.op("pe", lambda e: e.matmul(out, lhsT=lhsT, rhs=rhs, start=start, stop=stop), reads, writes, inc)


def TR(S, out, in_, ident, reads, writes, inc=True):
    S.op("pe", lambda e: e.transpose(out, in_, ident), reads, writes, inc)


def ACT(S, out, in_, func, reads, writes, bias=None, scale=None):
    kw = {}
    if bias is not None:
        kw["bias"] = bias
    if scale is not None:
        kw["scale"] = scale
    S.op("act", lambda e: e.activation(out=out, in_=in_, func=func, **kw), reads, writes)


def TT(S, eng, out, in0, in1, op, reads, writes):
    S.op(eng, lambda e: e.tensor_tensor(out=out, in0=in0, in1=in1, op=op), reads, writes)


def TS(S, eng, out, in0, s1, s2, op0, op1, reads, writes):
    if s2 is None:
        S.op(eng, lambda e: e.tensor_scalar(out=out, in0=in0, scalar1=s1, scalar2=None, op0=op0), reads, writes)
    else:
        S.op(eng, lambda e: e.tensor_scalar(out=out, in0=in0, scalar1=s1, scalar2=s2, op0=op0, op1=op1), reads, writes)


def STT(S, eng, out, in0, scalar, in1, op0, op1, reads, writes, accum_out=None):
    if accum_out is None:
        S.op(eng, lambda e: e.scalar_tensor_tensor(out=out, in0=in0, scalar=scalar, in1=in1, op0=op0, op1=op1),
             reads, writes)
    else:
        S.op(eng, lambda e: e.scalar_tensor_tensor(out=out, in0=in0, scalar=scalar, in1=in1, op0=op0, op1=op1,
                                                   accum_out=accum_out), reads, writes)


def CP(S, eng, out, in_, reads, writes):
    if eng == "act":
        S.op("act", lambda e: e.copy(out=out, in_=in_), reads, writes)
    else:
        S.op(eng, lambda e: e.tensor_copy(out=out, in_=in_), reads, writes)


def DMA(S, eng, out, in_, reads=(), writes=()):
    S.dma(eng, lambda e: e.dma_start(out=out, in_=in_), reads, writes)


class Ctx:
    pass


_UID = [0]


def alloc(nc, st, name, shape, dt):
    _UID[0] += 1
    return st.enter_context(nc.sbuf_tensor("%s_u%d" % (name, _UID[0]), list(shape), dt))


def palloc(nc, st, name, shape, dt):
    _UID[0] += 1
    return st.enter_context(nc.psum_tensor("%s_u%d" % (name, _UID[0]), list(shape), dt))


def alloc_normT(nc, st, C):
    R = Ctx()
    R.xt = [alloc(nc, st, "xt%d" % i, [128, 4, D], F32) for i in range(2)]
    R.junk = alloc(nc, st, "nt_junk", [128, D], BF16)
    R.ss = [alloc(nc, st, "nt_ss%d" % i, [128, 4], F32) for i in range(2)]
    R.rstd = [alloc(nc, st, "nt_rstd%d" % i, [128, 4], F32) for i in range(2)]
    R.hn = alloc(nc, st, "nt_hn", [128, 4, D], BF16)
    R.hT = [alloc(nc, st, "hT%d" % i, [128, 8, 512], BF16) for i in range(2)]
    R.ptr = [palloc(nc, st, "nt_ptr%d" % i, [128, 1024], BF16) for i in range(2)]
    R.ident = C.ident
    return R


def load_x(S, R, xsrc, st):
    b = st % 2
    DMA(S, "sp", R.xt[b][:], xsrc[st * 512:(st + 1) * 512, :].rearrange("(a p) d -> p a d", p=128),
        writes=["xt%d" % b])


def norm_T(S, R, st):
    b = st % 2
    xt = R.xt[b]
    xk = "xt%d" % b
    for a in range(4):
        STT(S, "dve", R.junk[:], xt[:, a, :], 1.0, xt[:, a, :], ALU.mult, ALU.mult, [xk], ["nt_junk", "ss%d" % b],
            accum_out=R.ss[b][:, a:a + 1])
    TS(S, "dve", R.rstd[b][:], R.ss[b][:], 1.0 / D, EPS, ALU.mult, ALU.add, ["ss%d" % b], ["rstd%d" % b])
    ACT(S, R.rstd[b][:], R.rstd[b][:], AF.Sqrt, ["rstd%d" % b], ["rstd%d" % b])
    S.op("dve", lambda e: e.reciprocal(out=R.rstd[b][:], in_=R.rstd[b][:]), ["rstd%d" % b], ["rstd%d" % b])
    for a in range(4):
        TS(S, "dve" if a % 2 == 0 else "pool", R.hn[:, a, :], xt[:, a, :], R.rstd[b][:, a:a + 1], None, ALU.mult, None,
           [xk, "rstd%d" % b], ["hn%d" % a])
    for k in range(8):
        pt = R.ptr[k % 2]
        pk = "PS:ptr%d" % (k % 2)
        for a in range(4):
            TR(S, pt[:, a * 128:(a + 1) * 128], R.hn[:, a, k * 128:(k + 1) * 128], R.ident[:],
               ["hn%d" % a], [pk], inc=(a == 3))
        CP(S, "act" if k % 2 == 0 else "dve", R.hT[b][:, k, :], pt[:, 0:512], [pk], ["hT%d_%d" % (b, k)])
    return R.hT[b], ["hT%d_%d" % (b, k) for k in range(8)]


def pass_A1(nc, S, C, T, xsrc, lw, XM, XC, G):
    NST = T // 512
    with ExitStack() as st:
        R = alloc_normT(nc, st, C)
        W = alloc(nc, st, "a1_w", [128, 8, AI], BF16)
        gf = alloc(nc, st, "a1_gf", [128, 8], F32)
        cw = alloc(nc, st, "a1_cw", [128, 16, 4], F32)
        cb = alloc(nc, st, "a1_cb", [128, 16], F32)
        bdqT = alloc(nc, st, "a1_bdqT", [128, 16, 128], BF16)
        bdkT = alloc(nc, st, "a1_bdkT", [128, 16, 128], BF16)
        bdvT = alloc(nc, st, "a1_bdvT", [128, 16, 128], BF16)
        wg = alloc(nc, st, "a1_wg", [128, 48, 8], BF16)
        wgc = alloc(nc, st, "a1_wgc", [128, 16, 8], BF16)
        wgm = alloc(nc, st, "a1_wgm", [128, 16, 8], BF16)
        bg = alloc(nc, st, "a1_bg", [128, 8], F32)
        xmb = [alloc(nc, st, "a1_xmb%d" % i, [128, 16, 515], BF16) for i in range(2)]
        acc = [alloc(nc, st, "a1_acc%d" % i, [128, 512], F32) for i in range(2)]
        xm_st = [alloc(nc, st, "a1_xmst%d" % i, [128, 4, 16, 128], BF16) for i in range(2)]
        xc_st = [alloc(nc, st, "a1_xcst%d" % i, [128, 4, 16, 128], BF16) for i in range(2)]
        g_st = [alloc(nc, st, "a1_gst%d" % i, [128, 4, 8], F32) for i in range(2)]
        pm = [palloc(nc, st, "a1_pm%d" % i, [128, 512], F32) for i in range(3)]
        pgf = palloc(nc, st, "a1_pg", [128, 512], F32)
        pg = pgf[:, 0:32].rearrange("p (a g) -> p a g", a=4)

        DMA(S, "pool", W[:], lw["w_in"].rearrange("(k p) c -> p k c", p=128)[:, :, 0:AI], writes=["W"])
        DMA(S, "sp", gf[:], lw["gfold"], writes=["gf"])
        DMA(S, "sp", cw[:], lw["conv_w"], writes=["cw"])
        DMA(S, "sp", cb[:], lw["conv_b"], writes=["cb"])
        DMA(S, "pool", bdqT[:], lw["bdqT"], writes=["bdqT"])
        DMA(S, "pool", bdkT[:], lw["bdkT"], writes=["bdkT"])
        DMA(S, "pool", bdvT[:], lw["bdvT"], writes=["bdvT"])
        DMA(S, "pool", wg[:], lw["w_gate"], writes=["wg"])
        DMA(S, "sp", bg[:], lw["b_gate"], writes=["bg"])
        for k in range(8):
            TS(S, "dve", W[:, k, :], W[:, k, :], gf[:, k:k + 1], None, ALU.mult, None, ["W", "gf"], ["W"])
        for j in range(16):
            MM(S, pg[:, 0, :], bdqT[:, j, :], wg[:, j, :], True, False, ["bdqT", "wg"], ["PS:pg"])
            MM(S, pg[:, 0, :], bdkT[:, j, :], wg[:, 16 + j, :], False, True, ["bdkT", "wg"], ["PS:pg"])
            CP(S, "dve", wgc[:, j, :], pg[:, 0, :], ["PS:pg"], ["wgc"])
            MM(S, pg[:, 1, :], bdvT[:, j, :], wg[:, 32 + j, :], True, True, ["bdvT", "wg"], ["PS:pg"])
            CP(S, "dve", wgm[:, j, :], pg[:, 1, :], ["PS:pg"], ["wgm"])
        S.op("pool", lambda e: e.memset(xmb[0][:, :, 0:3], 0.0), [], ["xmb0h"])

        load_x(S, R, xsrc, 0)
        for s_ in range(NST):
            b = s_ % 2
            if s_ + 1 < NST:
                load_x(S, R, xsrc, s_ + 1)
            hT, hk = norm_T(S, R, s_)
            xb = xmb[b]
            xbk = "xmb%d" % b
            for j in range(16):
                p = pm[j % 3]
                pk = "PS:pm%d" % (j % 3)
                for k in range(8):
                    MM(S, p[:], W[:, k, j * 128:(j + 1) * 128], hT[:, k, :], k == 0, k == 7, ["W", hk[k]], [pk])
                CP(S, "act", xb[:, j, 3:515], p[:], [pk], [xbk + "_%d" % j])
                CP(S, "pool", xm_st[b][:, :, j, :], xb[:, j, 3:515].rearrange("p (a t) -> p a t", a=4),
                   [xbk + "_%d" % j], ["xmst%d" % b])
            if s_ + 1 < NST:
                CP(S, "pool", xmb[1 - b][:, :, 0:3], xb[:, :, 512:515], [xbk + "_%d" % j for j in range(16)],
                   ["xmb%dh" % (1 - b)])
            for j in range(16):
                a_ = acc[j % 2]
                ak = "acc%d" % (j % 2)
                rk = [xbk + "_%d" % j, xbk + "h", "cw", "cb"]
                TS(S, "dve", a_[:], xb[:, j, 0:512], cw[:, j, 0:1], cb[:, j:j + 1], ALU.mult, ALU.add, rk, [ak])
                for tap in range(1, 4):
                    STT(S, "dve", a_[:], xb[:, j, tap:tap + 512], cw[:, j, tap:tap + 1], a_[:], ALU.mult, ALU.add,
                        rk + [ak], [ak])
                ACT(S, xc_st[b][:, :, j, :], a_[:].rearrange("p (a t) -> p a t", a=4), AF.Silu, [ak], ["xcst%d" % b])
            for a in range(4):
                for j in range(16):
                    MM(S, pg[:, a, :], xc_st[b][:, a, j, :], wgc[:, j, :], j == 0, False, ["xcst%d" % b, "wgc"], ["PS:pg"])
                for j in range(16):
                    MM(S, pg[:, a, :], xm_st[b][:, a, j, :], wgm[:, j, :], False, j == 15, ["xmst%d" % b, "wgm"], ["PS:pg"])
            TT(S, "dve", g_st[b][:], pg[:], bg[:].unsqueeze(1).to_broadcast([128, 4, 8]), ALU.add, ["PS:pg", "bg"],
               ["gst%d" % b])
            DMA(S, "sp", XM[s_ * 4:(s_ + 1) * 4].rearrange("a p j t -> p a (j t)"),
                xm_st[b][:].rearrange("p a j t -> p a (j t)"), reads=["xmst%d" % b])
            DMA(S, "sp", XC[s_ * 4:(s_ + 1) * 4].rearrange("a p j t -> p a (j t)"),
                xc_st[b][:].rearrange("p a j t -> p a (j t)"), reads=["xcst%d" % b])
            DMA(S, "sp", G[s_ * 512:(s_ + 1) * 512, :].rearrange("(a p) g -> p a g", p=128), g_st[b][:],
                reads=["gst%d" % b])
        S.barrier()
        S.emit()


def pass_A2(nc, S, C, T, xsrc, lw, SZ, SO):
    NST = T // 512
    with ExitStack() as st:
        R = alloc_normT(nc, st, C)
        W = alloc(nc, st, "a2_w", [128, 8, 2 * AI], BF16)
        gf = alloc(nc, st, "a2_gf", [128, 8], F32)
        sz_st = [alloc(nc, st, "a2_szst%d" % i, [128, 4, 16, 128], BF16) for i in range(2)]
        so_st = [alloc(nc, st, "a2_sost%d" % i, [128, 4, AI], BF16) for i in range(2)]
        pm = [palloc(nc, st, "a2_pm%d" % i, [128, 512], F32) for i in range(4)]
        DMA(S, "pool", W[:], lw["w_in"].rearrange("(k p) c -> p k c", p=128)[:, :, AI:3 * AI], writes=["W"])
        DMA(S, "sp", gf[:], lw["gfold"], writes=["gf"])
        for k in range(8):
            TS(S, "dve", W[:, k, :], W[:, k, :], gf[:, k:k + 1], None, ALU.mult, None, ["W", "gf"], ["W"])
        load_x(S, R, xsrc, 0)
        n = 0
        for s_ in range(NST):
            b = s_ % 2
            if s_ + 1 < NST:
                load_x(S, R, xsrc, s_ + 1)
            hT, hk = norm_T(S, R, s_)
            for a in range(4):
                for cg in range(4):
                    p = pm[n % 4]
                    pk = "PS:pm%d" % (n % 4)
                    n += 1
                    for k in range(8):
                        MM(S, p[:], hT[:, k, a * 128:(a + 1) * 128], W[:, k, cg * 512:(cg + 1) * 512], k == 0, k == 7,
                           ["W", hk[k]], [pk])
                    ACT(S, so_st[b][:, a, cg * 512:(cg + 1) * 512], p[:], AF.Sigmoid, [pk], ["sost%d" % b])
            for j in range(16):
                p = pm[n % 4]
                pk = "PS:pm%d" % (n % 4)
                n += 1
                for k in range(8):
                    MM(S, p[:], W[:, k, AI + j * 128:AI + (j + 1) * 128], hT[:, k, :], k == 0, k == 7, ["W", hk[k]], [pk])
                ACT(S, sz_st[b][:, :, j, :], p[:].rearrange("p (a t) -> p a t", a=4), AF.Silu, [pk], ["szst%d" % b])
            DMA(S, "sp", SZ[s_ * 4:(s_ + 1) * 4].rearrange("a p j t -> p a (j t)"),
                sz_st[b][:].rearrange("p a j t -> p a (j t)"), reads=["szst%d" % b])
            DMA(S, "sp", SO[s_ * 512:(s_ + 1) * 512, :].rearrange("(a p) c -> p a c", p=128), so_st[b][:],
                reads=["sost%d" % b])
        S.barrier()
        S.emit()


def post_residual(S, P, py, pyk, xin, xink, gpost, xout, xoutk):
    for h in range(2):
        sl = slice(h * 512, (h + 1) * 512)
        CP(S, "act", P.pr_y[:, sl], py[h][:], [pyk[h]], ["pr_y%d" % h])
        STT(S, "dve", P.pr_junk[:], P.pr_y[:, sl], 1.0, P.pr_y[:, sl], ALU.mult, ALU.mult, ["pr_y%d" % h],
            ["pr_junk", "pr_ss"], accum_out=P.pr_ss[:, h:h + 1])
    TT(S, "dve", P.pr_r[:], P.pr_ss[:, 0:1], P.pr_ss[:, 1:2], ALU.add, ["pr_ss"], ["pr_r"])
    TS(S, "dve", P.pr_r[:], P.pr_r[:], 1.0 / D, EPS, ALU.mult, ALU.add, ["pr_r"], ["pr_r"])
    ACT(S, P.pr_r[:], P.pr_r[:], AF.Sqrt, ["pr_r"], ["pr_r"])
    S.op("dve", lambda e: e.reciprocal(out=P.pr_r[:], in_=P.pr_r[:]), ["pr_r"], ["pr_r"])
    for h in range(2):
        sl = slice(h * 512, (h + 1) * 512)
        STT(S, "dve", P.pr_t[:, sl], P.pr_y[:, sl], P.pr_r[:, 0:1], gpost[:, sl], ALU.mult, ALU.mult,
            ["pr_y%d" % h, "pr_r", "gpost"], ["pr_t%d" % h])
        TT(S, "pool", xout[:, sl], P.pr_t[:, sl], xin[:, sl], ALU.add, ["pr_t%d" % h, xink], [xoutk])


def alloc_post(nc, st):
    P = Ctx()
    P.pr_junk = alloc(nc, st, "pr_junk", [128, 512], BF16)
    P.pr_ss = alloc(nc, st, "pr_ss", [128, 2], F32)
    P.pr_r = alloc(nc, st, "pr_r", [128, 1], F32)
    P.pr_t = alloc(nc, st, "pr_t", [128, D], F32)
    P.pr_y = alloc(nc, st, "pr_y", [128, D], F32)
    return P


def pass_B(nc, S, C, T, xsrc, xdst, lw, XM, XC, SZ, SO, G):
    NCH = T // 128
    with ExitStack() as st:
        P = alloc_post(nc, st)
        bdq = alloc(nc, st, "b_bdq", [128, 16, 128], BF16)
        bdk = alloc(nc, st, "b_bdk", [128, 16, 128], BF16)
        bdv = alloc(nc, st, "b_bdv", [128, 16, 128], BF16)
        wout = alloc(nc, st, "b_wout", [128, 16, D], BF16)
        hnw = alloc(nc, st, "b_hnw", [128, AI], F32)
        skp = alloc(nc, st, "b_skip", [128, 16], F32)
        gpost = alloc(nc, st, "b_gpost", [128, D], F32)
        C32 = alloc(nc, st, "b_C32", [128, 16, 512], F32)
        Cbf = alloc(nc, st, "b_Cbf", [128, 16, 512], BF16)
        n32 = alloc(nc, st, "b_n32", [128, 16], F32)
        nbf = alloc(nc, st, "b_nbf", [128, 16], BF16)
        xc = [alloc(nc, st, "b_xc%d" % i, [128, 16, 128], BF16) for i in range(2)]
        xm = [alloc(nc, st, "b_xm%d" % i, [128, 16, 128], BF16) for i in range(2)]
        sz = [alloc(nc, st, "b_sz%d" % i, [128, 16, 128], BF16) for i in range(2)]
        so = [alloc(nc, st, "b_so%d" % i, [128, AI], BF16) for i in range(2)]
        gt = [alloc(nc, st, "b_gt%d" % i, [128, 8], F32) for i in range(2)]
        xin = [alloc(nc, st, "b_xin0", [128, D], F32)] * 2
        xout = [alloc(nc, st, "b_xout0", [128, D], F32)] * 2
        qT = alloc(nc, st, "b_qT", [128, 16, 128], BF16)
        qsT = alloc(nc, st, "b_qsT", [128, 16, 128], BF16)
        kT = alloc(nc, st, "b_kT", [128, 16, 128], BF16)
        ktok = alloc(nc, st, "b_ktok", [128, AI], BF16)
        kw = alloc(nc, st, "b_kw", [128, AI], BF16)
        vtok = alloc(nc, st, "b_vtok", [128, AI], BF16)
        lf = alloc(nc, st, "b_lf", [128, 4], F32)
        lfrep = alloc(nc, st, "b_lfrep", [128, 4, 128], F32)
        av = alloc(nc, st, "b_a", [128, 4], F32)
        wv = alloc(nc, st, "b_w", [128, 4], F32)
        ebt = alloc(nc, st, "b_ebt", [128, 4, 128], F32)
        logd = alloc(nc, st, "b_logd", [128, 4, 128], F32)
        dT = alloc(nc, st, "b_dT", [128, 4, 128], F32)
        pT = alloc(nc, st, "b_pT", [128, 4, 128], BF16)
        den = alloc(nc, st, "b_den", [128, 4], F32)
        hc = alloc(nc, st, "b_hc", [128, 512], F32)
        bst = alloc(nc, st, "b_bst", [128, 6], F32)
        mv = alloc(nc, st, "b_mv", [128, 2], F32)
        hn = alloc(nc, st, "b_hn", [128, AI], BF16)
        xs = alloc(nc, st, "b_xs", [128, 16, 128], BF16)
        yT = alloc(nc, st, "b_yT", [128, 16, 128], BF16)
        tmpf = alloc(nc, st, "b_tmpf", [128, 4, 128], F32)
        onesb = alloc(nc, st, "b_ones", [128, 1], BF16)
        pA = [palloc(nc, st, "b_pA%d" % i, [128, 512], F32) for i in range(4)]
        pS = palloc(nc, st, "b_pS", [128, 512], F32)
        pB = palloc(nc, st, "b_pB", [128, 512], F32)
        pD = palloc(nc, st, "b_pD", [128, 512], F32)
        pTb = palloc(nc, st, "b_pTb", [128, 1024], BF16)

        DMA(S, "pool", bdq[:], lw["bdq"], writes=["bdq"])
        DMA(S, "pool", bdk[:], lw["bdk"], writes=["bdk"])
        DMA(S, "pool", bdv[:], lw["bdv"], writes=["bdv"])
        DMA(S, "pool", wout[:], lw["w_out"].rearrange("(j p) c -> p j c", p=128), writes=["wout"])
        DMA(S, "sp", hnw[:], lw["head_norm"], writes=["hnw"])
        DMA(S, "sp", skp[:], lw["skip"], writes=["skp"])
        DMA(S, "sp", gpost[:], lw["gpost"], writes=["gpost"])
        S.op("pool", lambda e: e.memset(C32[:], 0.0), [], ["C32"])
        S.op("pool", lambda e: e.memset(Cbf[:], 0.0), [], ["Cbf"])
        S.op("pool", lambda e: e.memset(n32[:], 0.0), [], ["n32"])
        S.op("pool", lambda e: e.memset(nbf[:], 0.0), [], ["nbf"])
        S.op("pool", lambda e: e.memset(onesb[:], 1.0), [], ["onesb"])

        def loads(c):
            b = c % 2
            DMA(S, "sp", xc[b][:].rearrange("p j t -> p (j t)"), XC[c], writes=["xc%d" % b])
            DMA(S, "sp", xm[b][:].rearrange("p j t -> p (j t)"), XM[c], writes=["xm%d" % b])
            DMA(S, "sp", sz[b][:].rearrange("p j t -> p (j t)"), SZ[c], writes=["sz%d" % b])
            DMA(S, "sp", so[b][:], SO[c * 128:(c + 1) * 128, :], writes=["so%d" % b])
            DMA(S, "sp", gt[b][:], G[c * 128:(c + 1) * 128, :], writes=["gt%d" % b])

        loads(0)
        DMA(S, "sp", xin[0][:], xsrc[0:128, :], writes=["xin0"])
        npa = 0
        for c in range(NCH):
            b = c % 2
            if c + 1 < NCH:
                loads(c + 1)
            xck, xmk, szk, sok, gtk = "xc%d" % b, "xm%d" % b, "sz%d" % b, "so%d" % b, "gt%d" % b
            for h in range(4):
                for (dst, bd, src, srck, dk_, scale) in ((qT, bdq, xc[b], xck, "qT", None),
                                                         (kT, bdk, xc[b], xck, "kT", 512.0 ** -0.5)):
                    p = pA[npa % 4]
                    pk = "PS:pA%d" % (npa % 4)
                    npa += 1
                    for jj in range(4):
                        j = h * 4 + jj
                        MM(S, p[:, jj * 128:(jj + 1) * 128], bd[:, j, :], src[:, j, :], True, True,
                           ["bdq", "bdk", srck], [pk], inc=(jj == 3))
                    if scale is None:
                        CP(S, "act", dst[:, h * 4:(h + 1) * 4, :], p[:].rearrange("p (a t) -> p a t", a=4), [pk],
                           ["%s%d" % (dk_, h)])
                    else:
                        S.op("act", lambda e, dst=dst, p=p, h=h, scale=scale: e.mul(
                            out=dst[:, h * 4:(h + 1) * 4, :], in_=p[:].rearrange("p (a t) -> p a t", a=4), mul=scale),
                            [pk], ["%s%d" % (dk_, h)])
                for (dst, bd, src, srck, dk_, scale) in ((ktok, bdk, xc[b], xck, "ktok", 512.0 ** -0.5),
                                                         (vtok, bdv, xm[b], xmk, "vtok", None)):
                    p = pA[npa % 4]
                    pk = "PS:pA%d" % (npa % 4)
                    npa += 1
                    for jj in range(4):
                        j = h * 4 + jj
                        MM(S, p[:, jj * 128:(jj + 1) * 128], src[:, j, :], bd[:, j, :], True, True,
                           ["bdk", "bdv", srck], [pk], inc=(jj == 3))
                    if scale is None:
                        CP(S, "dve", dst[:, h * 512:(h + 1) * 512], p[:], [pk], ["%s%d" % (dk_, h)])
                    else:
                        TS(S, "dve", dst[:, h * 512:(h + 1) * 512], p[:], scale, None, ALU.mult, None, [pk],
                           ["%s%d" % (dk_, h)])
            ACT(S, lf[:], gt[b][:, 4:8], AF.Exp, [gtk], ["lf"], scale=-1.0)
            ACT(S, lf[:], lf[:], AF.Ln, ["lf"], ["lf"], bias=1.0)
            TS(S, "dve", lf[:], lf[:], -1.0, None, ALU.mult, None, ["lf"], ["lf"])
            CP(S, "dve", lfrep[:], lf[:].unsqueeze(2).to_broadcast([128, 4, 128]), ["lf"], ["lfrep"])
            MM(S, pD[:, 8:12], C.tri[:], lf[:], True, True, ["lf"], ["PS:pD"])
            TT(S, "dve", av[:], gt[b][:, 0:4], pD[:, 8:12], ALU.subtract, [gtk, "PS:pD"], ["av"])
            for h in range(4):
                MM(S, pB[:, h * 128:(h + 1) * 128], lfrep[:, h, :], C.tri[:], True, True, ["lfrep"], ["PS:pB"], inc=(h == 3))
            pB3 = pB[:].rearrange("p (h t) -> p h t", h=4)
            ACT(S, ebt[:], pB3, AF.Exp, ["PS:pB"], ["ebt"])
            TT(S, "dve", logd[:], pB3, C.maskb[:].unsqueeze(1).to_broadcast([128, 4, 128]), ALU.add, ["PS:pB"], ["logd"])
            TT(S, "dve", wv[:], av[:], pB3[:, :, 127], ALU.add, ["av", "PS:pB"], ["wv"])
            ACT(S, wv[:], wv[:], AF.Exp, ["wv"], ["wv"])
            for h in range(4):
                ACT(S, dT[:, h, :], logd[:, h, :], AF.Exp, ["logd", "av"], ["dT"], bias=av[:, h:h + 1])
            for h in range(4):
                for jj in range(4):
                    j = h * 4 + jj
                    MM(S, pS[:, h * 128:(h + 1) * 128], kT[:, j, :], qT[:, j, :], jj == 0, jj == 3,
                       ["kT%d" % h, "qT%d" % h], ["PS:pS"], inc=(h == 3 and jj == 3))
            TT(S, "dve", pT[:], pS[:].rearrange("p (h t) -> p h t", h=4), dT[:], ALU.mult, ["PS:pS", "dT"], ["pT"])
            for h in range(4):
                TT(S, "pool", qsT[:, h * 4:(h + 1) * 4, :], qT[:, h * 4:(h + 1) * 4, :],
                   ebt[:, h, :].unsqueeze(1).to_broadcast([128, 4, 128]), ALU.mult, ["qT%d" % h, "ebt"], ["qsT%d" % h])
            for h in range(4):
                MM(S, pD[:, h:h + 1], pT[:, h, :], onesb[:], True, False, ["pT", "onesb"], ["PS:pD"], inc=False)
                for jj in range(4):
                    j = h * 4 + jj
                    MM(S, pD[:, h:h + 1], qsT[:, j, :], nbf[:, j:j + 1], False, jj == 3, ["qsT%d" % h, "nbf"], ["PS:pD"],
                       inc=(h == 3 and jj == 3))
            ACT(S, den[:], pD[:, 0:4], AF.Abs, ["PS:pD"], ["den"])
            TS(S, "dve", den[:], den[:], 1.0, None, ALU.max, None, ["den"], ["den"])
            S.op("dve", lambda e: e.reciprocal(out=den[:], in_=den[:]), ["den"], ["den"])
            for h in range(4):
                p = pA[npa % 4]
                pk = "PS:pA%d" % (npa % 4)
                npa += 1
                MM(S, p[:], pT[:, h, :], vtok[:, h * 512:(h + 1) * 512], True, False, ["pT", "vtok%d" % h], [pk], inc=False)
                for jj in range(4):
                    j = h * 4 + jj
                    MM(S, p[:], qsT[:, j, :], Cbf[:, j, :], False, jj == 3, ["qsT%d" % h, "Cbf%d" % h], [pk])
                STT(S, "dve", hc[:], p[:], den[:, h:h + 1], so[b][:, h * 512:(h + 1) * 512], ALU.mult, ALU.mult,
                    [pk, "den", sok], ["hc"])
                S.op("dve", lambda e: e.bn_stats(out=bst[:], in_=hc[:]), ["hc"], ["bst"])
                S.op("dve", lambda e: e.bn_aggr(out=mv[:], in_=bst[:]), ["bst"], ["mv"])
                TS(S, "dve", mv[:, 1:2], mv[:, 1:2], EPS, None, ALU.add, None, ["mv"], ["mv"])
                ACT(S, mv[:, 1:2], mv[:, 1:2], AF.Sqrt, ["mv"], ["mv"])
                S.op("dve", lambda e: e.reciprocal(out=mv[:, 1:2], in_=mv[:, 1:2]), ["mv"], ["mv"])
                TS(S, "dve", hc[:], hc[:], mv[:, 0:1], mv[:, 1:2], ALU.subtract, ALU.mult, ["hc", "mv"], ["hc"])
                TT(S, "dve", hn[:, h * 512:(h + 1) * 512], hc[:], hnw[:, h * 512:(h + 1) * 512], ALU.mult,
                   ["hc", "hnw"], ["hn%d" % h])
                for jj in range(4):
                    TR(S, pTb[:, (h % 2) * 512 + jj * 128:(h % 2) * 512 + (jj + 1) * 128],
                       hn[:, h * 512 + jj * 128:h * 512 + (jj + 1) * 128], C.ident[:], ["hn%d" % h],
                       ["PS:pTb"], inc=(jj == 3))
                TT(S, "pool", xs[:, h * 4:(h + 1) * 4, :], xc[b][:, h * 4:(h + 1) * 4, :],
                   skp[:, h * 4:(h + 1) * 4].unsqueeze(2).to_broadcast([128, 4, 128]), ALU.mult, [xck, "skp"],
                   ["xs%d" % h])
                TT(S, "dve", tmpf[:], pTb[:, (h % 2) * 512:(h % 2 + 1) * 512].rearrange("p (a t) -> p a t", a=4),
                   xs[:, h * 4:(h + 1) * 4, :], ALU.add, ["PS:pTb", "xs%d" % h], ["tmpf"])
                TT(S, "dve", yT[:, h * 4:(h + 1) * 4, :], tmpf[:], sz[b][:, h * 4:(h + 1) * 4, :], ALU.mult,
                   ["tmpf", szk], ["yT%d" % h])
            py = [pA[npa % 4], pA[(npa + 1) % 4]]
            pyk = ["PS:pA%d" % (npa % 4), "PS:pA%d" % ((npa + 1) % 4)]
            npa += 2
            for hh in range(2):
                for j in range(16):
                    MM(S, py[hh][:], yT[:, j, :], wout[:, j, hh * 512:(hh + 1) * 512], j == 0, j == 15,
                       ["yT%d" % (j // 4), "wout"], [pyk[hh]])
            post_residual(S, P, py, pyk, xin[b], "xin0", gpost, xout[b], "xout0")
            DMA(S, "sp", xdst[c * 128:(c + 1) * 128, :], xout[b][:], reads=["xout0"])
            if c + 1 < NCH:
                DMA(S, "sp", xin[0][:], xsrc[(c + 1) * 128:(c + 2) * 128, :], writes=["xin0"])
            for h in range(4):
                TS(S, "pool", kw[:, h * 512:(h + 1) * 512], ktok[:, h * 512:(h + 1) * 512], wv[:, h:h + 1], None,
                   ALU.mult, None, ["ktok%d" % h, "wv"], ["kw%d" % h])
            for h in range(4):
                for jj in range(4):
                    j = h * 4 + jj
                    MM(S, pD[:, 16 + j:17 + j], kw[:, j * 128:(j + 1) * 128], onesb[:], True, True, ["kw%d" % h, "onesb"],
                       ["PS:pD"], inc=(h == 3 and jj == 3))
            for h in range(4):
                STT(S, "dve", n32[:, h * 4:(h + 1) * 4], n32[:, h * 4:(h + 1) * 4], ebt[:, h, 127:128],
                    pD[:, 16 + h * 4:16 + (h + 1) * 4], ALU.mult, ALU.add, ["n32", "ebt", "PS:pD"], ["n32"])
            CP(S, "dve", nbf[:], n32[:], ["n32"], ["nbf"])
            for h in range(4):
                for jj in range(4):
                    j = h * 4 + jj
                    p = pA[npa % 4]
                    pk = "PS:pA%d" % (npa % 4)
                    npa += 1
                    MM(S, p[:], kw[:, j * 128:(j + 1) * 128], vtok[:, h * 512:(h + 1) * 512], True, True,
                       ["kw%d" % h, "vtok%d" % h], [pk])
                    STT(S, "dve", C32[:, j, :], C32[:, j, :], ebt[:, h, 127:128], p[:], ALU.mult, ALU.add,
                        ["C32_%d" % j, "ebt", pk], ["C32_%d" % j])
                    CP(S, "act", Cbf[:, j, :], C32[:, j, :], ["C32_%d" % j], ["Cbf%d" % h])
        S.barrier()
        S.emit()


def host_consts():
    c = {}
    c["ident"] = np.eye(128, dtype=np.float32)
    s = np.arange(128)
    c["tri"] = (s[:, None] <= s[None, :]).astype(np.float32)
    c["maskb"] = np.where(s[:, None] <= s[None, :], 0.0, NEG).astype(np.float32)
    return c


def blockdiag_layout(w):
    bd = np.zeros((16, 128, 128), np.float32)
    wj = w.reshape(16, 32, 4, 4)
    for g in range(32):
        bd[:, g * 4:(g + 1) * 4, g * 4:(g + 1) * 4] = wj[:, g]
    return (np.ascontiguousarray(bd.transpose(1, 0, 2)), np.ascontiguousarray(bd.transpose(2, 0, 1)))


def layer_a_layout(inp, l):
    o = {}
    o["w_in"] = np.ascontiguousarray(inp["a_w_in"][l])
    o["gfold"] = np.ascontiguousarray(inp["norm_pre"][l].reshape(8, 128).T)
    o["conv_w"] = np.ascontiguousarray(inp["a_conv_w"][l].reshape(4, 16, 128).transpose(2, 1, 0))
    o["conv_b"] = np.ascontiguousarray(inp["a_conv_b"][l].reshape(16, 128).T)
    for n, k in (("q", "a_w_q"), ("k", "a_w_k"), ("v", "a_w_v")):
        bd, bdT = blockdiag_layout(inp[k][l])
        o["bd" + n] = bd
        o["bd" + n + "T"] = bdT
    o["w_gate"] = np.ascontiguousarray(inp["a_w_gate"][l].reshape(48, 128, 8).transpose(1, 0, 2))
    o["b_gate"] = np.ascontiguousarray(np.broadcast_to(inp["a_b_gate"][l][None, :], (128, 8)))
    o["head_norm"] = np.ascontiguousarray(np.broadcast_to(inp["a_head_norm"][l][None, :], (128, AI)))
    o["skip"] = np.ascontiguousarray(inp["a_skip"][l].reshape(16, 128).T)
    o["gpost"] = np.ascontiguousarray(np.broadcast_to(inp["norm_post"][l][None, :], (128, D)))
    o["w_out"] = np.ascontiguousarray(inp["a_w_out"][l])
    return o


def load_consts(nc, S, st, cdr):
    C = Ctx()
    C.ident = alloc(nc, st, "k_ident", [128, 128], BF16)
    C.tri = alloc(nc, st, "k_tri", [128, 128], F32)
    C.maskb = alloc(nc, st, "k_maskb", [128, 128], F32)
    DMA(S, "pool", C.ident[:], cdr["ident"], writes=["c_ident"])
    DMA(S, "sp", C.tri[:], cdr["tri"], writes=["c_tri"])
    DMA(S, "sp", C.maskb[:], cdr["maskb"], writes=["c_maskb"])
    S.barrier()
    S.emit()
    return C


def build_program(T, host_arrays, n_a_layers=2, n_b_layers=2, out_name="out", debug=False, skip=()):
    nc = bass.Bass("TRN2", target_bir_lowering=False)
    dr = {}
    for name, arr in host_arrays.items():
        dr[name] = nc.dram_tensor(name, list(arr.shape), F32, kind="ExternalInput").ap()
    out = nc.dram_tensor(out_name, [T, D], F32, kind="ExternalOutput").ap()
    NCH = T // 128

    def scratch(name, shape, dt):
        if debug:
            return nc.dram_tensor(name, list(shape), dt, kind="ExternalOutput").ap()
        return nc.dram_tensor(name, list(shape), dt).ap()

    XM = scratch("s_xm", [NCH, 128, 16 * 128], BF16)
    XC = scratch("s_xc", [NCH, 128, 16 * 128], BF16)
    SZ = scratch("s_sz", [NCH, 128, 16 * 128], BF16)
    XM4 = XM.rearrange("c p (j t) -> c p j t", j=16)
    XC4 = XC.rearrange("c p (j t) -> c p j t", j=16)
    SZ4 = SZ.rearrange("c p (j t) -> c p j t", j=16)
    SO = scratch("s_so", [T, AI], BF16)
    G = scratch("s_g", [T, 8], F32)
    nlay = n_a_layers + n_b_layers
    XS = [scratch("s_x%d" % i, [T, D], F32) for i in range(max(nlay - 1, 0))]
    with ExitStack() as gst:
        S = Sched(nc, gst)
        cdr = {k[2:]: v for k, v in dr.items() if k.startswith("c_")}
        C = load_consts(nc, S, gst, cdr)
        xs = dr["x"]
        li = 0
        for l in range(n_a_layers):
            lw = {k[3:]: v for k, v in dr.items() if k.startswith("a%d_" % l)}
            xd = out if li == nlay - 1 else XS[li]
            pass_A1(nc, S, C, T, xs, lw, XM4, XC4, G)
            pass_A2(nc, S, C, T, xs, lw, SZ4, SO)
            pass_B(nc, S, C, T, xs, xd, lw, XM, XC, SZ, SO, G)
            xs = xd
            li += 1
        if n_b_layers > 0:
            N = {k[2:]: v for k, v in dr.items() if k.startswith("n_")}
            kw_ = {k[3:]: v for k, v in dr.items() if k.startswith("kv_")}
            KST = [scratch("s_kst%d" % g, [128, T], BF16) for g in range(2)]
            KWT = [scratch("s_kwt%d" % g, [128, T], BF16) for g in range(2)]
            VS = [scratch("s_vs%d" % g, [T, 128], BF16) for g in range(2)]
            VW = [scratch("s_vw%d" % g, [T, 128], BF16) for g in range(2)]
            KCT = [scratch("s_kct%d" % g, [128, 512], BF16) for g in range(2)]
            VC = [scratch("s_vc%d" % g, [512, 128], BF16) for g in range(2)]
            QT = scratch("s_qt", [NCH, 128, NH * 128], BF16)
            GT = scratch("s_gt", [T, 48], F32)
            SZG = scratch("s_szg", [T, ZW], BF16)
            if "kv" not in skip:
                pass_KV(nc, S, C, T, xs, kw_, KST, KWT, VS, VW, KCT, VC)
            for lb in range(n_b_layers):
                lw = {k[3:]: v for k, v in dr.items() if k.startswith("b%d_" % lb)}
                xd = out if li == nlay - 1 else XS[li]
                if "q" not in skip:
                    pass_Q(nc, S, C, T, xs, lw, QT, GT)
                if "z" not in skip:
                    pass_Z(nc, S, C, T, xs, lw, SZG, 0)
                    pass_Z(nc, S, C, T, xs, lw, SZG, 1)
                if "att" not in skip:
                    pass_ATT(nc, S, C, T, xs, xd, lw, N, QT, GT, SZG, KST, KWT, VS, VW, KCT, VC)
                xs = xd
                li += 1
        print("instructions:", S.ninst)
    return nc


def make_host_arrays(inp, b, T, n_a_layers=2, n_b_layers=2, x_override=None):
    ha = {"x": np.ascontiguousarray(inp["x"][b, :T]) if x_override is None else x_override}
    for k, v in host_consts().items():
        ha["c_" + k] = v
    for l in range(n_a_layers):
        for k, v in layer_a_layout(inp, l).items():
            ha["a%d_%s" % (l, k)] = v
    if n_b_layers > 0:
        for k, v in nsa_consts(T).items():
            ha["n_" + k] = v
        for k, v in nsa_layout(inp).items():
            ha["kv_" + k] = v
        for lb in range(n_b_layers):
            for k, v in layer_b_layout(inp, lb, 2 + lb).items():
                ha["b%d_%s" % (lb, k)] = v
    return ha


_SHARED = ("c_", "a0_", "a1_", "n_", "kv_", "b0_", "b1_")


def kernel(**inputs):
    inp = {k: np.asarray(v) for k, v in inputs.items()}
    T = inp["x"].shape[1]
    base = make_host_arrays(inp, 0, T)
    maps = []
    for b in range(8):
        m = dict(base)
        m["x"] = np.ascontiguousarray(inp["x"][b, :T])
        maps.append(m)
    nc = build_program(T, base)
    res = run_bass_kernel_spmd(nc, maps, core_ids=list(range(8)))
    return np.stack([r["out"] for r in res.results], axis=0).astype(np.float32)


NH = 16
ZW = 6144
BIG = 1.0e30


def nsa_consts(T):
    c = {}
    i = np.arange(128)
    kk = np.arange(128)
    c["cdiag"] = (kk[:, None] <= i[None, :]).astype(np.float32)
    c["cwin"] = (kk[:, None] > i[None, :]).astype(np.float32)
    h = np.arange(1, NH + 1, dtype=np.float64)
    slopes = 2.0 ** (-8.0 * h / NH)
    rel = np.arange(-63, 1)
    ab = slopes[None, None, :] * (128.0 * rel[None, :, None] + kk[:, None, None] - 63.5)
    c["ab"] = ab.astype(np.float32)
    abc = slopes[None, None, :] * (128.0 * rel[None, :, None] + 16.0 * kk[:, None, None] - 48.0)
    c["abc"] = np.minimum(abc, 40.0).astype(np.float32)
    cm = np.zeros((128, 17, 128), np.float32)
    for r in range(-16, 1):
        cm[:, r + 16, :] = ((16 * kk[:, None] + 31 - i[None, :]) <= (-128 * r)).astype(np.float32)
    c["cm"] = cm
    wov = np.zeros((128, 33), np.float32)
    for n in range(128):
        for j in range(33):
            ov = min(16 * n + 32, 64 * j + 64) - max(16 * n, 64 * j)
            wov[n, j] = max(ov, 0) / 16.0
    c["wov"] = wov
    vm = np.zeros((128, 256), np.float32)
    fb = np.zeros((128, 256), np.float32)
    for ii in range(128):
        cur = 1 if ii >= 64 else 0
        for col in range(256):
            jr = col - 127
            if jr == cur or jr == cur - 1:
                fb[ii, col] = BIG
            elif jr < cur:
                vm[ii, col] = 1.0
            else:
                fb[ii, col] = -BIG
    c["vm"] = vm
    c["fb"] = fb
    return c


def nsa_layout(inp):
    o = {}
    o["kv_w"] = np.ascontiguousarray(inp["kv_w"])
    o["kv_gf"] = np.ascontiguousarray(inp["kv_norm"].reshape(8, 128).T)
    for s_ in range(2):
        o["w1_%d" % s_] = np.ascontiguousarray(inp["cmp_w1"][s_])
        o["posT_%d" % s_] = np.ascontiguousarray(inp["cmp_pos"][s_].T)
        o["b1_%d" % s_] = np.ascontiguousarray(inp["cmp_b1"][s_].reshape(2, 128).T)
        o["w2_%d" % s_] = np.ascontiguousarray(inp["cmp_w2"][s_])
    o["b2k"] = np.ascontiguousarray(inp["cmp_b2"][0].reshape(128, 1))
    o["b2v"] = np.ascontiguousarray(np.broadcast_to(inp["cmp_b2"][1][None, :], (128, 128)))
    return o


def layer_b_layout(inp, lb, layer):
    o = {}
    o["w_in"] = np.ascontiguousarray(inp["b_w_in"][lb])
    o["gfold"] = np.ascontiguousarray(inp["norm_pre"][layer].reshape(8, 128).T)
    o["w_out"] = np.ascontiguousarray(inp["b_w_out"][lb])
    o["gpost"] = np.ascontiguousarray(np.broadcast_to(inp["norm_post"][layer][None, :], (128, D)))
    return o


def load_w(S, W, gf, wsrc, gsrc, c0, c1):
    DMA(S, "pool", W[:], wsrc.rearrange("(k p) c -> p k c", p=128)[:, :, c0:c1], writes=["W"])
    DMA(S, "sp", gf[:], gsrc, writes=["gf"])
    for k in range(8):
        TS(S, "dve", W[:, k, :], W[:, k, :], gf[:, k:k + 1], None, ALU.mult, None, ["W", "gf"], ["W"])


def pass_KV(nc, S, C, T, xsrc, kw_, KST, KWT, VS, VW, KCT, VC):
    NST = T // 512
    NC = T // 16 - 1
    NNT = (NC + 127) // 128
    with ExitStack() as st:
        R = alloc_normT(nc, st, C)
        W = alloc(nc, st, "kv_w", [128, 8, 1536], BF16)
        gf = alloc(nc, st, "kv_gf", [128, 8], F32)
        aT = [alloc(nc, st, "kv_aT%d" % i, [128, T], BF16) for i in range(4)]
        kst = [alloc(nc, st, "kv_kst%d" % i, [128, 4, 512], BF16) for i in range(2)]
        vst = [alloc(nc, st, "kv_vst%d" % i, [128, 4, 4, 128], BF16) for i in range(2)]
        pm = [palloc(nc, st, "kv_pm%d" % i, [128, 512], F32) for i in range(4)]
        load_w(S, W, gf, kw_["kv_w"], kw_["kv_gf"], 0, 1536)
        load_x(S, R, xsrc, 0)
        n = 0
        for s_ in range(NST):
            b = s_ % 2
            if s_ + 1 < NST:
                load_x(S, R, xsrc, s_ + 1)
            hT, hk = norm_T(S, R, s_)
            fi = 0
            for cb in (0, 1, 2, 3, 4, 5, 8, 9):
                p = pm[n % 4]
                pk = "PS:pm%d" % (n % 4)
                n += 1
                for k in range(8):
                    MM(S, p[:], W[:, k, cb * 128:(cb + 1) * 128], hT[:, k, :], k == 0, k == 7, ["W", hk[k]], [pk])
                if cb < 4:
                    CP(S, "act" if cb % 2 == 0 else "dve", aT[cb][:, s_ * 512:(s_ + 1) * 512], p[:], [pk], ["aT%d" % cb])
                else:
                    CP(S, "act" if cb % 2 == 0 else "dve", kst[b][:, fi, :], p[:], [pk], ["kst%d" % b])
                    fi += 1
            for a in range(4):
                p = pm[n % 4]
                pk = "PS:pm%d" % (n % 4)
                n += 1
                for bi, cb in enumerate((6, 7, 10, 11)):
                    for k in range(8):
                        MM(S, p[:, bi * 128:(bi + 1) * 128], hT[:, k, a * 128:(a + 1) * 128], W[:, k, cb * 128:(cb + 1) * 128],
                           k == 0, k == 7, ["W", hk[k]], [pk], inc=(k == 7 and bi == 3))
                CP(S, "act" if a % 2 == 0 else "dve", vst[b][:, a, :, :], p[:].rearrange("p (b d) -> p b d", b=4), [pk],
                   ["vst%d" % b])
            sl = slice(s_ * 512, (s_ + 1) * 512)
            for g in range(2):
                DMA(S, "sp", KST[g][:, sl], kst[b][:, g, :], reads=["kst%d" % b])
                DMA(S, "sp", KWT[g][:, sl], kst[b][:, 2 + g, :], reads=["kst%d" % b])
                DMA(S, "sp", VS[g][sl, :].rearrange("(a p) d -> p a d", p=128), vst[b][:, :, g, :], reads=["vst%d" % b])
                DMA(S, "sp", VW[g][sl, :].rearrange("(a p) d -> p a d", p=128), vst[b][:, :, 2 + g, :], reads=["vst%d" % b])
        w1 = alloc(nc, st, "kv_w1", [128, 32, 256], BF16)
        w2 = alloc(nc, st, "kv_w2", [128, 2, 128], BF16)
        posT = alloc(nc, st, "kv_posT", [128, 32], BF16)
        b1 = alloc(nc, st, "kv_b1", [128, 2], F32)
        c1 = alloc(nc, st, "kv_c1", [128, 2], F32)
        b2k = alloc(nc, st, "kv_b2k", [128, 1], F32)
        b2v = alloc(nc, st, "kv_b2v", [128, 128], F32)
        hid = alloc(nc, st, "kv_hid", [128, 2, 512], BF16)
        kco = alloc(nc, st, "kv_kco", [128, 512], BF16)
        vco = alloc(nc, st, "kv_vco", [128, 4, 128], BF16)
        DMA(S, "sp", b2k[:], kw_["b2k"], writes=["b2k"])
        DMA(S, "sp", b2v[:], kw_["b2v"], writes=["b2v"])
        S.op("pool", lambda e: e.memset(hid[:], 0.0), [], ["hid"])
        S.op("pool", lambda e: e.memset(kco[:], 0.0), [], ["kco"])
        for slot in range(2):
            DMA(S, "pool", w1[:], kw_["w1_%d" % slot].rearrange("(i d) m -> d i m", d=128), writes=["w1"])
            DMA(S, "pool", w2[:], kw_["w2_%d" % slot].rearrange("(c m) d -> m c d", m=128), writes=["w2"])
            DMA(S, "pool", posT[:], kw_["posT_%d" % slot], writes=["posT"])
            DMA(S, "sp", b1[:], kw_["b1_%d" % slot], writes=["b1"])
            for mc in range(2):
                p = pm[n % 4]
                pk = "PS:pm%d" % (n % 4)
                n += 1
                for i in range(32):
                    MM(S, p[:, 0:1], w1[:, i, mc * 128:(mc + 1) * 128], posT[:, i:i + 1], i == 0, i == 31, ["w1", "posT"], [pk])
                TT(S, "dve", c1[:, mc:mc + 1], p[:, 0:1], b1[:, mc:mc + 1], ALU.add, [pk, "b1"], ["c1"])
            for g in range(2):
                src = aT[slot * 2 + g]
                for mc in range(2):
                    p = pm[n % 4]
                    pk = "PS:pm%d" % (n % 4)
                    n += 1
                    for i in range(32):
                        MM(S, p[:, 0:NC], w1[:, i, mc * 128:(mc + 1) * 128], src[:, i:i + 16 * (NC - 1) + 1:16], i == 0, i == 31,
                           ["w1", "aT%d" % (slot * 2 + g)], [pk])
                    ACT(S, hid[:, mc, 0:NC], p[:, 0:NC], AF.Silu, [pk, "c1"], ["hid"], bias=c1[:, mc:mc + 1])
                if slot == 0:
                    p = pm[n % 4]
                    pk = "PS:pm%d" % (n % 4)
                    n += 1
                    for mc in range(2):
                        MM(S, p[:, 0:NC], w2[:, mc, :], hid[:, mc, 0:NC], mc == 0, mc == 1, ["w2", "hid"], [pk])
                    TS(S, "dve", kco[:, 0:NC], p[:, 0:NC], b2k[:, 0:1], None, ALU.add, None, [pk, "b2k"], ["kco"])
                    DMA(S, "sp", KCT[g], kco[:], reads=["kco"])
                else:
                    p = pm[n % 4]
                    pk = "PS:pm%d" % (n % 4)
                    n += 1
                    for nt in range(NNT):
                        for mc in range(2):
                            MM(S, p[:, nt * 128:(nt + 1) * 128], hid[:, mc, nt * 128:(nt + 1) * 128], w2[:, mc, :], mc == 0, mc == 1,
                               ["w2", "hid"], [pk], inc=(mc == 1 and nt == NNT - 1))
                    TT(S, "dve", vco[:, 0:NNT, :], p[:, 0:NNT * 128].rearrange("p (t d) -> p t d", d=128),
                       b2v[:].unsqueeze(1).to_broadcast([128, NNT, 128]), ALU.add, [pk, "b2v"], ["vco"])
                    DMA(S, "sp", VC[g][0:NNT * 128, :].rearrange("(t p) d -> p t d", p=128), vco[:, 0:NNT, :], reads=["vco"])
        S.barrier()
        S.emit()


def pass_Q(nc, S, C, T, xsrc, lw, QT, GT):
    NST = T // 512
    with ExitStack() as st:
        R = alloc_normT(nc, st, C)
        W = alloc(nc, st, "q_w", [128, 8, 2096], BF16)
        gf = alloc(nc, st, "q_gf", [128, 8], F32)
        q_st = [alloc(nc, st, "q_st%d" % i, [128, 4, 16, 128], BF16) for i in range(2)]
        g_st = [alloc(nc, st, "q_gst%d" % i, [128, 4, 48], F32) for i in range(2)]
        pm = [palloc(nc, st, "q_pm%d" % i, [128, 512], F32) for i in range(4)]
        pgf = palloc(nc, st, "q_pg", [128, 512], F32)
        pg = pgf[:, 0:192].rearrange("p (a g) -> p a g", a=4)
        load_w(S, W, gf, lw["w_in"], lw["gfold"], 0, 2096)
        load_x(S, R, xsrc, 0)
        n = 0
        for s_ in range(NST):
            b = s_ % 2
            if s_ + 1 < NST:
                load_x(S, R, xsrc, s_ + 1)
            hT, hk = norm_T(S, R, s_)
            for hd in range(NH):
                p = pm[n % 4]
                pk = "PS:pm%d" % (n % 4)
                n += 1
                for k in range(8):
                    MM(S, p[:], W[:, k, hd * 128:(hd + 1) * 128], hT[:, k, :], k == 0, k == 7, ["W", hk[k]], [pk])
                S.op("act", lambda e, p=p, hd=hd, b=b: e.mul(out=q_st[b][:, :, hd, :],
                                                            in_=p[:].rearrange("p (a t) -> p a t", a=4), mul=128.0 ** -0.5),
                     [pk], ["qst%d" % b])
            for a in range(4):
                for k in range(8):
                    MM(S, pg[:, a, :], hT[:, k, a * 128:(a + 1) * 128], W[:, k, 2048:2096], k == 0, k == 7, ["W", hk[k]],
                       ["PS:pg"], inc=(k == 7 and a == 3))
            ACT(S, g_st[b][:], pg[:], AF.Sigmoid, ["PS:pg"], ["gst%d" % b])
            DMA(S, "sp", QT[s_ * 4:(s_ + 1) * 4].rearrange("a p (j t) -> p a j t", j=16), q_st[b][:], reads=["qst%d" % b])
            DMA(S, "sp", GT[s_ * 512:(s_ + 1) * 512, :].rearrange("(a p) g -> p a g", p=128), g_st[b][:], reads=["gst%d" % b])
        S.barrier()
        S.emit()


def pass_Z(nc, S, C, T, xsrc, lw, SZG, hz):
    NST = T // 512
    with ExitStack() as st:
        R = alloc_normT(nc, st, C)
        W = alloc(nc, st, "z_w", [128, 8, 3072], BF16)
        gf = alloc(nc, st, "z_gf", [128, 8], F32)
        z_st = [alloc(nc, st, "z_st%d" % i, [128, 4, 3072], BF16) for i in range(2)]
        pm = [palloc(nc, st, "z_pm%d" % i, [128, 512], F32) for i in range(4)]
        c0 = 2096 + hz * 3072
        load_w(S, W, gf, lw["w_in"], lw["gfold"], c0, c0 + 3072)
        load_x(S, R, xsrc, 0)
        n = 0
        for s_ in range(NST):
            b = s_ % 2
            if s_ + 1 < NST:
                load_x(S, R, xsrc, s_ + 1)
            hT, hk = norm_T(S, R, s_)
            for a in range(4):
                for cg in range(6):
                    p = pm[n % 4]
                    pk = "PS:pm%d" % (n % 4)
                    n += 1
                    for k in range(8):
                        MM(S, p[:], hT[:, k, a * 128:(a + 1) * 128], W[:, k, cg * 512:(cg + 1) * 512], k == 0, k == 7,
                           ["W", hk[k]], [pk])
                    ACT(S, z_st[b][:, a, cg * 512:(cg + 1) * 512], p[:], AF.Silu, [pk], ["zst%d" % b])
            DMA(S, "sp", SZG[s_ * 512:(s_ + 1) * 512, hz * 3072:(hz + 1) * 3072].rearrange("(a p) c -> p a c", p=128),
                z_st[b][:], reads=["zst%d" % b])
        S.barrier()
        S.emit()


def pass_ATT(nc, S, C, T, xsrc, xdst, lw, N, QT, GT, SZG, KST, KWT, VS, VW, KCT, VC):
    NQT = T // 128
    NC = T // 16 - 1
    with ExitStack() as st:
        P = alloc_post(nc, st)
        kst = [alloc(nc, st, "at_kst%d" % g, [128, T], BF16) for g in range(2)]
        vsa = [alloc(nc, st, "at_vsa%d" % g, [128, NQT, 129], BF16) for g in range(2)]
        wout = alloc(nc, st, "at_wout", [128, 16, D], BF16)
        gpost = alloc(nc, st, "at_gpost", [128, D], F32)
        kct = [alloc(nc, st, "at_kct%d" % g, [128, 512], BF16) for g in range(2)]
        rc = [alloc(nc, st, "at_rc%d" % g, [128, 4, 161], BF16) for g in range(2)]
        ab = alloc(nc, st, "at_ab", [128, 64, NH], F32)
        abc = alloc(nc, st, "at_abc", [128, 64, NH], F32)
        cm = alloc(nc, st, "at_cm", [128, 17, 128], BF16)
        cdiag = alloc(nc, st, "at_cdiag", [128, 128], BF16)
        cwin = alloc(nc, st, "at_cwin", [128, 128], BF16)
        vm = alloc(nc, st, "at_vm", [128, 256], F32)
        fb = alloc(nc, st, "at_fb", [128, 256], F32)
        mk_all = alloc(nc, st, "at_mkall", [128, NQT, 128], BF16)
        qt_sb = alloc(nc, st, "at_q", [128, NH, 128], BF16)
        szg = alloc(nc, st, "at_szg", [128, ZW], BF16)
        gts = alloc(nc, st, "at_gt", [128, 48], F32)
        xin = alloc(nc, st, "at_xin", [128, D], F32)
        xout = xin
        pT = [alloc(nc, st, "at_pT%d" % i, [128, 8, 128], BF16) for i in range(3)]
        kwin = [alloc(nc, st, "at_kwin%d" % i, [128, 640], BF16) for i in range(2)]
        vwin = [alloc(nc, st, "at_vwin%d" % i, [128, 5, 129], BF16) for i in range(2)]
        ocmp = alloc(nc, st, "at_ocmp", [128, 8, 128], F32)
        impu = alloc(nc, st, "at_impu", [128, 8, 4, 33], F32)
        impn = alloc(nc, st, "at_impn", [128, 4, 33], F32)
        imp = alloc(nc, st, "at_imp", [128, 160], F32)
        impm = alloc(nc, st, "at_impm", [128, 128], F32)
        imp2 = alloc(nc, st, "at_imp2", [128, 128], F32)
        m8 = alloc(nc, st, "at_m8", [128, 16], F32)
        thr = alloc(nc, st, "at_thr", [128, 1], F32)
        mask = alloc(nc, st, "at_mask", [128, 128], BF16)
        mrep = alloc(nc, st, "at_mrep", [128, 8, 64], BF16)
        zc = alloc(nc, st, "at_zc", [128, 8], F32)
        coef = alloc(nc, st, "at_coef", [128, 8], F32)
        osum = alloc(nc, st, "at_osum", [128, 8, 128], F32)
        otmp = alloc(nc, st, "at_otmp", [128, 8, 128], F32)
        oall = alloc(nc, st, "at_oall", [128, 2 * 1024], BF16)
        oT = alloc(nc, st, "at_oT", [128, 16, 128], BF16)
        sT = [palloc(nc, st, "at_sT%d" % i, [128, 1024], F32) for i in range(2)]
        oa = [palloc(nc, st, "at_oa%d" % i, [128, 512], F32) for i in range(3)]
        pmisc = palloc(nc, st, "at_misc", [128, 1024], BF16)

        def oav(hd, w):
            return oa[hd // 3][:, (hd % 3) * 161:(hd % 3) * 161 + w]

        def oak(hd):
            return "PS:oa%d" % (hd // 3)

        for g in range(2):
            DMA(S, "sp", kst[g][:], KST[g], writes=["kst%d" % g])
            S.op("pool", lambda e, g=g: e.memset(vsa[g][:, :, 128:129], 1.0), [], ["vsa1_%d" % g])
            for t4 in range(0, NQT, 16):
                t5 = min(t4 + 16, NQT)
                DMA(S, "sp", vsa[g][:, t4:t5, 0:128], VS[g][t4 * 128:t5 * 128, :].rearrange("(t p) d -> p t d", p=128),
                    writes=["vsa%d" % g])
            DMA(S, "sp", kct[g][:], KCT[g], writes=["kct%d" % g])
            DMA(S, "sp", rc[g][:, :, 0:128], VC[g].rearrange("(t p) d -> p t d", p=128), writes=["rc%d" % g])
            for nt in range(4):
                DMA(S, "pool", rc[g][:, nt, 128:161], N["wov"], writes=["rcw%d" % g])
        for i_ in range(2):
            S.op("pool", lambda e, i_=i_: e.memset(vwin[i_][:, :, 128:129], 1.0), [], ["vwin1_%d" % i_])
        DMA(S, "pool", wout[:], lw["w_out"].rearrange("(j p) c -> p j c", p=128), writes=["wout"])
        DMA(S, "sp", gpost[:], lw["gpost"], writes=["gpost"])
        DMA(S, "sp", ab[:], N["ab"], writes=["ab"])
        DMA(S, "sp", abc[:], N["abc"], writes=["abc"])
        DMA(S, "pool", cm[:], N["cm"], writes=["cm"])
        DMA(S, "pool", cdiag[:], N["cdiag"], writes=["cdiag"])
        DMA(S, "pool", cwin[:], N["cwin"], writes=["cwin"])
        DMA(S, "sp", vm[:], N["vm"], writes=["vm"])
        DMA(S, "sp", fb[:], N["fb"], writes=["fb"])
        consts_k = ["ab", "abc", "cm", "cdiag", "cwin"]
        st_ctr = [0]
        pt_ctr = [0]

        def att_step(g, lhsT_k, kkeys, bias_tab, mask_ap, mkeys, rhs_v, vkeys, ncol, first, last):
            si = st_ctr[0] % 2
            st_ctr[0] += 1
            pi = pt_ctr[0] % 3
            pt_ctr[0] += 1
            sk = "PS:sT%d" % si
            for hf in range(2):
                MM(S, sT[si][:, hf * 512:(hf + 1) * 512], lhsT_k,
                   qt_sb[:, g * 8 + hf * 4:g * 8 + hf * 4 + 4, :].rearrange("p h t -> p (h t)"), True, True,
                   kkeys + ["qt"], [sk], inc=(hf == 1))
            for hh in range(8):
                ACT(S, pT[pi][:, hh, :], sT[si][:, hh * 128:(hh + 1) * 128], AF.Exp, [sk] + consts_k, ["pT%d" % pi],
                    bias=bias_tab[:, g * 8 + hh:g * 8 + hh + 1])
            if mask_ap is not None:
                TT(S, "dve", pT[pi][:], pT[pi][:], mask_ap.unsqueeze(1).to_broadcast([128, 8, 128]), ALU.mult,
                   ["pT%d" % pi] + mkeys, ["pT%d" % pi])
            for hh in range(8):
                first_in_bank = (hh % 3 == 0)
                S.op("pe", lambda e, hh=hh, fib=first_in_bank: e.matmul(
                    oav(hh, ncol), lhsT=pT[pi][:, hh, :], rhs=rhs_v, start=(first and fib), stop=last,
                    skip_group_check=True), ["pT%d" % pi] + vkeys, [oak(hh)], inc=(hh == 7 or hh % 3 == 2))

        def combine(g, br, src_is_psum, accumulate):
            if src_is_psum:
                for bk in range(3):
                    nh_ = 3 if bk < 2 else 2
                    CP(S, "dve", zc[:, bk * 3:bk * 3 + nh_],
                       oa[bk][:, 0:nh_ * 161].rearrange("p (h c) -> p h c", c=161)[:, :, 128], ["PS:oa%d" % bk], ["zc"])
            TS(S, "dve", zc[:], zc[:], 1e-30, None, ALU.max, None, ["zc"], ["zc"])
            S.op("dve", lambda e: e.reciprocal(out=zc[:], in_=zc[:]), ["zc"], ["zc"])
            gsl = gts[:, (br * 2 + g) * 8:(br * 2 + g) * 8 + 8]
            TT(S, "dve", coef[:], zc[:], gsl, ALU.mult, ["zc", "gts"], ["coef"])
            dst = otmp if accumulate else osum
            dk = "otmp" if accumulate else "osum"
            for hh in range(8):
                src = oav(hh, 128) if src_is_psum else ocmp[:, hh, :]
                sk_ = oak(hh) if src_is_psum else "ocmp"
                zo = ((br * 2 + g) * 8 + hh) * 128
                STT(S, "dve", dst[:, hh, :], src, coef[:, hh:hh + 1], szg[:, zo:zo + 128], ALU.mult, ALU.mult,
                    [sk_, "coef", "szg"], [dk])
            if accumulate:
                TT(S, "pool", osum[:], osum[:], otmp[:], ALU.add, ["osum", "otmp"], ["osum"])

        for qt in range(NQT):
            t0 = qt * 128
            DMA(S, "sp", qt_sb[:].rearrange("p h t -> p (h t)"), QT[qt], writes=["qt"])
            DMA(S, "sp", szg[:], SZG[t0:t0 + 128, :], writes=["szg"])
            DMA(S, "sp", gts[:], GT[t0:t0 + 128, :], writes=["gts"])
            DMA(S, "sp", xin[:], xsrc[t0:t0 + 128, :], writes=["xin"])
            for g in range(2):
                wb = (qt * 2 + g) % 2
                k0 = max(qt - 4, 0)
                nwt = qt - k0 + 1
                DMA(S, "sp", kwin[wb][:, 0:nwt * 128], KWT[g][:, k0 * 128:(qt + 1) * 128], writes=["kwin%d" % wb])
                DMA(S, "sp", vwin[wb][:, 0:nwt, 0:128], VW[g][k0 * 128:(qt + 1) * 128, :].rearrange("(t p) d -> p t d", p=128),
                    writes=["vwin%d" % wb])
                nts = [nt for nt in range(4) if 16 * nt <= qt and nt * 128 < NC]
                S.op("pool", lambda e: e.memset(impu[:], 0.0), [], ["impu"])
                S.op("pool", lambda e: e.memset(ocmp[:], 0.0), [], ["ocmp"])
                for nt in nts:
                    relc = 16 * nt - qt
                    m_ap = cm[:, relc + 16, :] if relc >= -16 else None
                    att_step(g, kct[g][:, nt * 128:(nt + 1) * 128], ["kct%d" % g], abc[:, relc + 63, :], m_ap, ["cm"],
                             rc[g][:, nt, :], ["rc%d" % g, "rcw%d" % g], 161, True, True)
                    for bk in range(3):
                        nh_ = 3 if bk < 2 else 2
                        v3 = oa[bk][:, 0:nh_ * 161].rearrange("p (h c) -> p h c", c=161)
                        TT(S, "dve", ocmp[:, bk * 3:bk * 3 + nh_, :], ocmp[:, bk * 3:bk * 3 + nh_, :], v3[:, :, 0:128], ALU.add,
                           ["ocmp", "PS:oa%d" % bk], ["ocmp"])
                        CP(S, "dve", impu[:, bk * 3:bk * 3 + nh_, nt, :], v3[:, :, 128:161], ["PS:oa%d" % bk], ["impu"])
                S.op("dve", lambda e: e.reduce_sum(out=zc[:], in_=impu[:].rearrange("p h n j -> p h (n j)"),
                                                   axis=mybir.AxisListType.X), ["impu"], ["zc"])
                TS(S, "dve", zc[:], zc[:], 0.5, None, ALU.mult, None, ["zc"], ["zc"])
                combine(g, 0, False, False)
                S.op("pool", lambda e: e.memset(impn[:], 0.0), [], ["impn"])
                for hh in range(8):
                    STT(S, "dve", impn[:], impu[:, hh, :, :], zc[:, hh:hh + 1], impn[:], ALU.mult, ALU.add,
                        ["impu", "zc", "impn"], ["impn"])
                S.op("pool", lambda e: e.memset(imp[:], 0.0), [], ["imp"])
                for nt in nts:
                    TT(S, "dve", imp[:, 32 * nt:32 * nt + 33], imp[:, 32 * nt:32 * nt + 33], impn[:, nt, :], ALU.add,
                       ["imp", "impn"], ["imp"])
                c0 = 127 - 2 * qt
                TT(S, "dve", impm[:], imp[:, 0:128], vm[:, c0:c0 + 128], ALU.mult, ["imp", "vm"], ["impm"])
                TT(S, "dve", impm[:], impm[:], fb[:, c0:c0 + 128], ALU.add, ["impm", "fb"], ["impm"])
                S.op("dve", lambda e: e.memset(impm[:, 0:1], BIG), [], ["impm"])
                S.op("dve", lambda e: e.max(out=m8[:, 0:8], in_=impm[:]), ["impm"], ["m8"])
                S.op("dve", lambda e: e.match_replace(out=imp2[:], in_to_replace=m8[:, 0:8], in_values=impm[:],
                                                      imm_value=-3.0e38), ["impm", "m8"], ["imp2"])
                S.op("dve", lambda e: e.max(out=m8[:, 8:16], in_=imp2[:]), ["imp2"], ["m8"])
                S.op("dve", lambda e: e.tensor_reduce(out=thr[:], in_=m8[:, 8:16], axis=mybir.AxisListType.X, op=ALU.min),
                     ["m8"], ["thr"])
                TS(S, "dve", mask[:], impm[:], thr[:, 0:1], None, ALU.is_ge, None, ["impm", "thr"], ["mask"])
                kb_lo = (max(0, qt - 16) // 4) * 4 if g == 0 else 0
                for kb in range(kb_lo, qt, 4):
                    nk = min(4, qt - kb)
                    CP(S, "dve", mrep[:, 0:2 * nk, :], mask[:, 2 * kb:2 * kb + 2 * nk].unsqueeze(2).to_broadcast([128, 2 * nk, 64]),
                       ["mask"], ["mrep"])
                    si = st_ctr[0] % 2
                    st_ctr[0] += 1
                    for s4 in range(nk):
                        MM(S, sT[si][:, s4 * 128:(s4 + 1) * 128], mrep[:, 2 * s4:2 * s4 + 2, :].rearrange("p a b -> p (a b)"),
                           C.ident[:], True, True, ["mrep"], ["PS:sT%d" % si], inc=(s4 == nk - 1))
                    CP(S, "act", mk_all[:, kb:kb + nk, :], sT[si][:, 0:nk * 128].rearrange("p (k i) -> p k i", i=128),
                       ["PS:sT%d" % si], ["mk_all"])
                kt_lo = max(0, qt - 16) if g == 0 else 0
                for kt in range(kt_lo, qt + 1):
                    m_ap = cdiag[:] if kt == qt else mk_all[:, kt, :]
                    att_step(g, kst[g][:, kt * 128:(kt + 1) * 128], ["kst%d" % g], ab[:, kt - qt + 63, :], m_ap,
                             ["cdiag", "mk_all"], vsa[g][:, kt, :], ["vsa%d" % g, "vsa1_%d" % g], 129, kt == kt_lo, kt == qt)
                combine(g, 1, True, True)
                for wi in range(nwt):
                    kt = k0 + wi
                    if kt == qt:
                        m_ap = cdiag[:]
                    elif kt == qt - 4:
                        m_ap = cwin[:]
                    else:
                        m_ap = None
                    att_step(g, kwin[wb][:, wi * 128:(wi + 1) * 128], ["kwin%d" % wb], ab[:, kt - qt + 63, :], m_ap,
                             ["cdiag", "cwin"], vwin[wb][:, wi, :], ["vwin%d" % wb, "vwin1_%d" % wb], 129, wi == 0, wi == nwt - 1)
                combine(g, 2, True, True)
                CP(S, "act", oall[:, g * 1024:(g + 1) * 1024], osum[:].rearrange("p h d -> p (h d)"), ["osum"], ["oall%d" % g])
            for half in range(2):
                for jj in range(8):
                    j = half * 8 + jj
                    TR(S, pmisc[:, jj * 128:(jj + 1) * 128], oall[:, j * 128:(j + 1) * 128], C.ident[:], ["oall%d" % (j // 8)],
                       ["PS:misc"], inc=(jj == 7))
                CP(S, "dve", oT[:, half * 8:(half + 1) * 8, :], pmisc[:].rearrange("p (j t) -> p j t", t=128), ["PS:misc"],
                   ["oT%d" % half])
            si = st_ctr[0] % 2
            st_ctr[0] += 1
            py = [sT[si][:, 0:512], sT[si][:, 512:1024]]
            for hh in range(2):
                for j in range(16):
                    MM(S, py[hh], oT[:, j, :], wout[:, j, hh * 512:(hh + 1) * 512], j == 0, j == 15,
                       ["oT%d" % (j // 8), "wout"], ["PS:sT%d" % si])
            post_residual(S, P, py, ["PS:sT%d" % si] * 2, xin, "xin", gpost, xout, "xin")
            DMA(S, "sp", xdst[t0:t0 + 128, :], xout[:], reads=["xin"])
        S.barrier()
        S.emit()
```

```python
import numpy as np
from contextlib import ExitStack
import concourse.bass as bass
import concourse.mybir as mybir
from concourse.bass_utils import run_bass_kernel_spmd

F32 = mybir.dt.float32
BF16 = mybir.dt.bfloat16
AF = mybir.ActivationFunctionType
ALU = mybir.AluOpType
EPS = 1e-6
D = 1024
AI = 2048
NEG = -30000.0


class Sched:
    ENG = ("pe", "act", "dve", "pool", "sp")

    def __init__(self, nc, stack, ndsem=16):
        self.nc = nc
        self.sem = {e: stack.enter_context(nc.semaphore("sem_" + e)) for e in self.ENG}
        self.cnt = {e: 0 for e in self.ENG}
        self.waited = {e: {} for e in self.ENG}
        self.stream = {e: [] for e in self.ENG}
        self.lastw = {}
        self.reads = {}
        self.dsems = [stack.enter_context(nc.semaphore("semd%d" % i)) for i in range(ndsem)]
        self.dsem_cnt = [0] * ndsem
        self.dsem_rr = 0
        self.ninst = 0

    def _deps(self, eng, reads, writes):
        deps = []
        for r in reads:
            w = self.lastw.get(r)
            if w is not None:
                deps.append(w)
        for w_ in writes:
            w = self.lastw.get(w_)
            if w is not None and w[2] != eng:
                deps.append(w)
            for rd in self.reads.get(w_, {}).values():
                if rd[2] != eng:
                    deps.append(rd)
        out = {}
        for (s, v, tag) in deps:
            if tag == "pe" and eng == "pe":
                continue
            key = id(s)
            if v <= self.waited[eng].get(key, 0):
                continue
            if key not in out or out[key][1] < v:
                out[key] = (s, v)
        for key, (s, v) in out.items():
            self.waited[eng][key] = v
        return list(out.values())

    def _record(self, token, reads, writes):
        for r in reads:
            self.reads.setdefault(r, {})[token[2]] = token
        for w in writes:
            self.lastw[w] = token
            self.reads[w] = {}

    @staticmethod
    def _excl(reads, writes):
        ex = [k for k in reads if k.startswith("PS:")]
        if ex:
            writes = list(writes) + ex
            reads = [k for k in reads if not k.startswith("PS:")]
        return reads, writes

    def op(self, eng, fn, reads=(), writes=(), inc=True):
        reads, writes = self._excl(reads, writes)
        waits = self._deps(eng, reads, writes)
        for (s, v) in waits:
            if s is self.sem["pe"] and v > self.cnt["pe"]:
                raise RuntimeError("wait on un-signalled PE ticket")
        ticket = self.cnt[eng] + 1
        if inc:
            self.cnt[eng] = ticket
        else:
            assert eng == "pe"
        self.stream[eng].append((waits, fn, (self.sem[eng], 1) if inc else None))
        self._record((self.sem[eng], ticket, eng), reads, writes)
        self.ninst += 1

    def dma(self, eng, fn, reads=(), writes=()):
        i = self.dsem_rr
        self.dsem_rr = (self.dsem_rr + 1) % len(self.dsems)
        s = self.dsems[i]
        waits = self._deps(eng, reads, writes)
        prev = self.dsem_cnt[i]
        if prev > self.waited[eng].get(id(s), 0):
            waits = [w for w in waits if w[0] is not s] + [(s, prev)]
            self.waited[eng][id(s)] = prev
        self.dsem_cnt[i] = prev + 16
        self.stream[eng].append((waits, fn, (s, 16)))
        self._record((s, prev + 16, "d%d" % i), reads, writes)
        self.ninst += 1

    def barrier(self):
        assert all(it[2] is not None or it[1] is None for it in self.stream["pe"][-1:]), "last PE op must inc"
        for e in self.ENG:
            waits = []
            for e2 in self.ENG:
                s = self.sem[e2]
                if e2 != e and self.cnt[e2] > self.waited[e].get(id(s), 0):
                    waits.append((s, self.cnt[e2]))
                    self.waited[e][id(s)] = self.cnt[e2]
            for i, s in enumerate(self.dsems):
                if self.dsem_cnt[i] > self.waited[e].get(id(s), 0):
                    waits.append((s, self.dsem_cnt[i]))
                    self.waited[e][id(s)] = self.dsem_cnt[i]
            self.stream[e].append((waits, None, None))
        self.lastw.clear()
        self.reads.clear()

    def simulate(self, streams):
        if not hasattr(self, "simval"):
            self.simval = {}
        val = self.simval
        pos = {e: 0 for e in self.ENG}
        progress = True
        while progress:
            progress = False
            for e in self.ENG:
                items = streams[e]
                while pos[e] < len(items):
                    waits, fn, inc = items[pos[e]]
                    if any(val.get(id(s), 0) < v for (s, v) in waits):
                        break
                    if inc is not None:
                        val[id(inc[0])] = val.get(id(inc[0]), 0) + inc[1]
                    pos[e] += 1
                    progress = True
        for e in self.ENG:
            if pos[e] < len(streams[e]):
                waits, fn, inc = streams[e][pos[e]]
                bad = [(v, val.get(id(s), 0)) for (s, v) in waits if val.get(id(s), 0) < v]
                raise RuntimeError("DEADLOCK engine %s at item %d/%d waits(need,have)=%s" % (e, pos[e], len(streams[e]), bad))

    def emit(self):
        nc = self.nc
        streams = self.stream
        self.stream = {e: [] for e in self.ENG}
        self.simulate(streams)

        def run(engobj, items):
            for (waits, fn, inc) in items:
                for (s, v) in waits:
                    engobj.wait_ge(s, v)
                if fn is not None:
                    ins = fn(engobj)
                    if inc is not None:
                        ins.then_inc(inc[0], inc[1])

        with nc.Block() as block:
            @block.tensor
            def _(e):
                run(e, streams["pe"])

            @block.scalar
            def _(e):
                run(e, streams["act"])

            @block.vector
            def _(e):
                run(e, streams["dve"])

            @block.gpsimd
            def _(e):
                run(e, streams["pool"])

            @block.sync
            def _(e):
                run(e, streams["sp"])


def MM(S, out, lhsT, rhs, start, stop, reads, writes, inc=None):
    if inc is None:
        inc = stop
    S.op("pe", lambda e: e.matmul(out, lhsT=lhsT, rhs=rhs, start=start, stop=stop), reads, writes, inc)


def TR(S, out, in_, ident, reads, writes, inc=True):
    S.op("pe", lambda e: e.transpose(out, in_, ident), reads, writes, inc)


def ACT(S, out, in_, func, reads, writes, bias=None, scale=None):
    kw = {}
    if bias is not None:
        kw["bias"] = bias
    if scale is not None:
        kw["scale"] = scale
    S.op("act", lambda e: e.activation(out=out, in_=in_, func=func, **kw), reads, writes)


def TT(S, eng, out, in0, in1, op, reads, writes):
    S.op(eng, lambda e: e.tensor_tensor(out=out, in0=in0, in1=in1, op=op), reads, writes)


def TS(S, eng, out, in0, s1, s2, op0, op1, reads, writes):
    if s2 is None:
        S.op(eng, lambda e: e.tensor_scalar(out=out, in0=in0, scalar1=s1, scalar2=None, op0=op0), reads, writes)
    else:
        S.op(eng, lambda e: e.tensor_scalar(out=out, in0=in0, scalar1=s1, scalar2=s2, op0=op0, op1=op1), reads, writes)


def STT(S, eng, out, in0, scalar, in1, op0, op1, reads, writes, accum_out=None):
    if accum_out is None:
        S.op(eng, lambda e: e.scalar_tensor_tensor(out=out, in0=in0, scalar=scalar, in1=in1, op0=op0, op1=op1),
             reads, writes)
    else:
        S.op(eng, lambda e: e.scalar_tensor_tensor(out=out, in0=in0, scalar=scalar, in1=in1, op0=op0, op1=op1,
                                                   accum_out=accum_out), reads, writes)


def CP(S, eng, out, in_, reads, writes):
    if eng == "act":
        S.op("act", lambda e: e.copy(out=out, in_=in_), reads, writes)
    else:
        S.op(eng, lambda e: e.tensor_copy(out=out, in_=in_), reads, writes)


def DMA(S, eng, out, in_, reads=(), writes=()):
    S.dma(eng, lambda e: e.dma_start(out=out, in_=in_), reads, writes)


class Ctx:
    pass


_UID = [0]


def alloc(nc, st, name, shape, dt):
    _UID[0] += 1
    return st.enter_context(nc.sbuf_tensor("%s_u%d" % (name, _UID[0]), list(shape), dt))


def palloc(nc, st, name, shape, dt):
    _UID[0] += 1
    return st.enter_context(nc.psum_tensor("%s_u%d" % (name, _UID[0]), list(shape), dt))


def alloc_normT(nc, st, C):
    R = Ctx()
    R.xt = [alloc(nc, st, "xt%d" % i, [128, 4, D], F32) for i in range(2)]
    R.junk = alloc(nc, st, "nt_junk", [128, D], BF16)
    R.ss = [alloc(nc, st, "nt_ss%d" % i, [128, 4], F32) for i in range(2)]
    R.rstd = [alloc(nc, st, "nt_rstd%d" % i, [128, 4], F32) for i in range(2)]
    R.hn = alloc(nc, st, "nt_hn", [128, 4, D], BF16)
    R.hT = [alloc(nc, st, "hT%d" % i, [128, 8, 512], BF16) for i in range(2)]
    R.ptr = [palloc(nc, st, "nt_ptr%d" % i, [128, 1024], BF16) for i in range(2)]
    R.ident = C.ident
    return R


def load_x(S, R, xsrc, st):
    b = st % 2
    DMA(S, "sp", R.xt[b][:], xsrc[st * 512:(st + 1) * 512, :].rearrange("(a p) d -> p a d", p=128),
        writes=["xt%d" % b])


def norm_T(S, R, st):
    b = st % 2
    xt = R.xt[b]
    xk = "xt%d" % b
    for a in range(4):
        STT(S, "dve", R.junk[:], xt[:, a, :], 1.0, xt[:, a, :], ALU.mult, ALU.mult, [xk], ["nt_junk", "ss%d" % b],
            accum_out=R.ss[b][:, a:a + 1])
    TS(S, "dve", R.rstd[b][:], R.ss[b][:], 1.0 / D, EPS, ALU.mult, ALU.add, ["ss%d" % b], ["rstd%d" % b])
    ACT(S, R.rstd[b][:], R.rstd[b][:], AF.Sqrt, ["rstd%d" % b], ["rstd%d" % b])
    S.op("dve", lambda e: e.reciprocal(out=R.rstd[b][:], in_=R.rstd[b][:]), ["rstd%d" % b], ["rstd%d" % b])
    for a in range(4):
        TS(S, "dve" if a % 2 == 0 else "pool", R.hn[:, a, :], xt[:, a, :], R.rstd[b][:, a:a + 1], None, ALU.mult, None,
           [xk, "rstd%d" % b], ["hn%d" % a])
    for k in range(8):
        pt = R.ptr[k % 2]
        pk = "PS:ptr%d" % (k % 2)
        for a in range(4):
            TR(S, pt[:, a * 128:(a + 1) * 128], R.hn[:, a, k * 128:(k + 1) * 128], R.ident[:],
               ["hn%d" % a], [pk], inc=(a == 3))
        CP(S, "act" if k % 2 == 0 else "dve", R.hT[b][:, k, :], pt[:, 0:512], [pk], ["hT%d_%d" % (b, k)])
    return R.hT[b], ["hT%d_%d" % (b, k) for k in range(8)]


def pass_A1(nc, S, C, T, xsrc, lw, XM, XC, G):
    NST = T // 512
    with ExitStack() as st:
        R = alloc_normT(nc, st, C)
        W = alloc(nc, st, "a1_w", [128, 8, AI], BF16)
        gf = alloc(nc, st, "a1_gf", [128, 8], F32)
        cw = alloc(nc, st, "a1_cw", [128, 16, 4], F32)
        cb = alloc(nc, st, "a1_cb", [128, 16], F32)
        bdqT = alloc(nc, st, "a1_bdqT", [128, 16, 128], BF16)
        bdkT = alloc(nc, st, "a1_bdkT", [128, 16, 128], BF16)
        bdvT = alloc(nc, st, "a1_bdvT", [128, 16, 128], BF16)
        wg = alloc(nc, st, "a1_wg", [128, 48, 8], BF16)
        wgc = alloc(nc, st, "a1_wgc", [128, 16, 8], BF16)
        wgm = alloc(nc, st, "a1_wgm", [128, 16, 8], BF16)
        bg = alloc(nc, st, "a1_bg", [128, 8], F32)
        xmb = [alloc(nc, st, "a1_xmb%d" % i, [128, 16, 515], BF16) for i in range(2)]
        acc = [alloc(nc, st, "a1_acc%d" % i, [128, 512], F32) for i in range(2)]
        xm_st = [alloc(nc, st, "a1_xmst%d" % i, [128, 4, 16, 128], BF16) for i in range(2)]
        xc_st = [alloc(nc, st, "a1_xcst%d" % i, [128, 4, 16, 128], BF16) for i in range(2)]
        g_st = [alloc(nc, st, "a1_gst%d" % i, [128, 4, 8], F32) for i in range(2)]
        pm = [palloc(nc, st, "a1_pm%d" % i, [128, 512], F32) for i in range(3)]
        pgf = palloc(nc, st, "a1_pg", [128, 512], F32)
        pg = pgf[:, 0:32].rearrange("p (a g) -> p a g", a=4)

        DMA(S, "pool", W[:], lw["w_in"].rearrange("(k p) c -> p k c", p=128)[:, :, 0:AI], writes=["W"])
        DMA(S, "sp", gf[:], lw["gfold"], writes=["gf"])
        DMA(S, "sp", cw[:], lw["conv_w"], writes=["cw"])
        DMA(S, "sp", cb[:], lw["conv_b"], writes=["cb"])
        DMA(S, "pool", bdqT[:], lw["bdqT"], writes=["bdqT"])
        DMA(S, "pool", bdkT[:], lw["bdkT"], writes=["bdkT"])
        DMA(S, "pool", bdvT[:], lw["bdvT"], writes=["bdvT"])
        DMA(S, "pool", wg[:], lw["w_gate"], writes=["wg"])
        DMA(S, "sp", bg[:], lw["b_gate"], writes=["bg"])
        for k in range(8):
            TS(S, "dve", W[:, k, :], W[:, k, :], gf[:, k:k + 1], None, ALU.mult, None, ["W", "gf"], ["W"])
        for j in range(16):
            MM(S, pg[:, 0, :], bdqT[:, j, :], wg[:, j, :], True, False, ["bdqT", "wg"], ["PS:pg"])
            MM(S, pg[:, 0, :], bdkT[:, j, :], wg[:, 16 + j, :], False, True, ["bdkT", "wg"], ["PS:pg"])
            CP(S, "dve", wgc[:, j, :], pg[:, 0, :], ["PS:pg"], ["wgc"])
            MM(S, pg[:, 1, :], bdvT[:, j, :], wg[:, 32 + j, :], True, True, ["bdvT", "wg"], ["PS:pg"])
            CP(S, "dve", wgm[:, j, :], pg[:, 1, :], ["PS:pg"], ["wgm"])
        S.op("pool", lambda e: e.memset(xmb[0][:, :, 0:3], 0.0), [], ["xmb0h"])

        load_x(S, R, xsrc, 0)
        for s_ in range(NST):
            b = s_ % 2
            if s_ + 1 < NST:
                load_x(S, R, xsrc, s_ + 1)
            hT, hk = norm_T(S, R, s_)
            xb = xmb[b]
            xbk = "xmb%d" % b
            for j in range(16):
                p = pm[j % 3]
                pk = "PS:pm%d" % (j % 3)
                for k in range(8):
                    MM(S, p[:], W[:, k, j * 128:(j + 1) * 128], hT[:, k, :], k == 0, k == 7, ["W", hk[k]], [pk])
                CP(S, "act", xb[:, j, 3:515], p[:], [pk], [xbk + "_%d" % j])
                CP(S, "pool", xm_st[b][:, :, j, :], xb[:, j, 3:515].rearrange("p (a t) -> p a t", a=4),
                   [xbk + "_%d" % j], ["xmst%d" % b])
            if s_ + 1 < NST:
                CP(S, "pool", xmb[1 - b][:, :, 0:3], xb[:, :, 512:515], [xbk + "_%d" % j for j in range(16)],
                   ["xmb%dh" % (1 - b)])
            for j in range(16):
                a_ = acc[j % 2]
                ak = "acc%d" % (j % 2)
                rk = [xbk + "_%d" % j, xbk + "h", "cw", "cb"]
                TS(S, "dve", a_[:], xb[:, j, 0:512], cw[:, j, 0:1], cb[:, j:j + 1], ALU.mult, ALU.add, rk, [ak])
                for tap in range(1, 4):
                    STT(S, "dve", a_[:], xb[:, j, tap:tap + 512], cw[:, j, tap:tap + 1], a_[:], ALU.mult, ALU.add,
                        rk + [ak], [ak])
                ACT(S, xc_st[b][:, :, j, :], a_[:].rearrange("p (a t) -> p a t", a=4), AF.Silu, [ak], ["xcst%d" % b])
            for a in range(4):
                for j in range(16):
                    MM(S, pg[:, a, :], xc_st[b][:, a, j, :], wgc[:, j, :], j == 0, False, ["xcst%d" % b, "wgc"], ["PS:pg"])
                for j in range(16):
                    MM(S, pg[:, a, :], xm_st[b][:, a, j, :], wgm[:, j, :], False, j == 15, ["xmst%d" % b, "wgm"], ["PS:pg"])
            TT(S, "dve", g_st[b][:], pg[:], bg[:].unsqueeze(1).to_broadcast([128, 4, 8]), ALU.add, ["PS:pg", "bg"],
               ["gst%d" % b])
            DMA(S, "sp", XM[s_ * 4:(s_ + 1) * 4].rearrange("a p j t -> p a (j t)"),
                xm_st[b][:].rearrange("p a j t -> p a (j t)"), reads=["xmst%d" % b])
            DMA(S, "sp", XC[s_ * 4:(s_ + 1) * 4].rearrange("a p j t -> p a (j t)"),
                xc_st[b][:].rearrange("p a j t -> p a (j t)"), reads=["xcst%d" % b])
            DMA(S, "sp", G[s_ * 512:(s_ + 1) * 512, :].rearrange("(a p) g -> p a g", p=128), g_st[b][:],
                reads=["gst%d" % b])
        S.barrier()
        S.emit()


def pass_A2(nc, S, C, T, xsrc, lw, SZ, SO):
    NST = T // 512
    with ExitStack() as st:
        R = alloc_normT(nc, st, C)
        W = alloc(nc, st, "a2_w", [128, 8, 2 * AI], BF16)
        gf = alloc(nc, st, "a2_gf", [128, 8], F32)
        sz_st = [alloc(nc, st, "a2_szst%d" % i, [128, 4, 16, 128], BF16) for i in range(2)]
        so_st = [alloc(nc, st, "a2_sost%d" % i, [128, 4, AI], BF16) for i in range(2)]
        pm = [palloc(nc, st, "a2_pm%d" % i, [128, 512], F32) for i in range(4)]
        DMA(S, "pool", W[:], lw["w_in"].rearrange("(k p) c -> p k c", p=128)[:, :, AI:3 * AI], writes=["W"])
        DMA(S, "sp", gf[:], lw["gfold"], writes=["gf"])
        for k in range(8):
            TS(S, "dve", W[:, k, :], W[:, k, :], gf[:, k:k + 1], None, ALU.mult, None, ["W", "gf"], ["W"])
        load_x(S, R, xsrc, 0)
        n = 0
        for s_ in range(NST):
            b = s_ % 2
            if s_ + 1 < NST:
                load_x(S, R, xsrc, s_ + 1)
            hT, hk = norm_T(S, R, s_)
            for a in range(4):
                for cg in range(4):
                    p = pm[n % 4]
                    pk = "PS:pm%d" % (n % 4)
                    n += 1
                    for k in range(8):
                        MM(S, p[:], hT[:, k, a * 128:(a + 1) * 128], W[:, k, cg * 512:(cg + 1) * 512], k == 0, k == 7,
                           ["W", hk[k]], [pk])
                    ACT(S, so_st[b][:, a, cg * 512:(cg + 1) * 512], p[:], AF.Sigmoid, [pk], ["sost%d" % b])
            for j in range(16):
                p = pm[n % 4]
                pk = "PS:pm%d" % (n % 4)
                n += 1
                for k in range(8):
                    MM(S, p[:], W[:, k, AI + j * 128:AI + (j + 1) * 128], hT[:, k, :], k == 0, k == 7, ["W", hk[k]], [pk])
                ACT(S, sz_st[b][:, :, j, :], p[:].rearrange("p (a t) -> p a t", a=4), AF.Silu, [pk], ["szst%d" % b])
            DMA(S, "sp", SZ[s_ * 4:(s_ + 1) * 4].rearrange("a p j t -> p a (j t)"),
                sz_st[b][:].rearrange("p a j t -> p a (j t)"), reads=["szst%d" % b])
            DMA(S, "sp", SO[s_ * 512:(s_ + 1) * 512, :].rearrange("(a p) c -> p a c", p=128), so_st[b][:],
                reads=["sost%d" % b])
        S.barrier()
        S.emit()


def post_residual(S, P, py, pyk, xin, xink, gpost, xout, xoutk):
    for h in range(2):
        sl = slice(h * 512, (h + 1) * 512)
        CP(S, "act", P.pr_y[:, sl], py[h][:], [pyk[h]], ["pr_y%d" % h])
        STT(S, "dve", P.pr_junk[:], P.pr_y[:, sl], 1.0, P.pr_y[:, sl], ALU.mult, ALU.mult, ["pr_y%d" % h],
            ["pr_junk", "pr_ss"], accum_out=P.pr_ss[:, h:h + 1])
    TT(S, "dve", P.pr_r[:], P.pr_ss[:, 0:1], P.pr_ss[:, 1:2], ALU.add, ["pr_ss"], ["pr_r"])
    TS(S, "dve", P.pr_r[:], P.pr_r[:], 1.0 / D, EPS, ALU.mult, ALU.add, ["pr_r"], ["pr_r"])
    ACT(S, P.pr_r[:], P.pr_r[:], AF.Sqrt, ["pr_r"], ["pr_r"])
    S.op("dve", lambda e: e.reciprocal(out=P.pr_r[:], in_=P.pr_r[:]), ["pr_r"], ["pr_r"])
    for h in range(2):
        sl = slice(h * 512, (h + 1) * 512)
        STT(S, "dve", P.pr_t[:, sl], P.pr_y[:, sl], P.pr_r[:, 0:1], gpost[:, sl], ALU.mult, ALU.mult,
            ["pr_y%d" % h, "pr_r", "gpost"], ["pr_t%d" % h])
        TT(S, "pool", xout[:, sl], P.pr_t[:, sl], xin[:, sl], ALU.add, ["pr_t%d" % h, xink], [xoutk])


def alloc_post(nc, st):
    P = Ctx()
    P.pr_junk = alloc(nc, st, "pr_junk", [128, 512], BF16)
    P.pr_ss = alloc(nc, st, "pr_ss", [128, 2], F32)
    P.pr_r = alloc(nc, st, "pr_r", [128, 1], F32)
    P.pr_t = alloc(nc, st, "pr_t", [128, D], F32)
    P.pr_y = alloc(nc, st, "pr_y", [128, D], F32)
    return P


def pass_B(nc, S, C, T, xsrc, xdst, lw, XM, XC, SZ, SO, G):
    NCH = T // 128
    with ExitStack() as st:
        P = alloc_post(nc, st)
        bdq = alloc(nc, st, "b_bdq", [128, 16, 128], BF16)
        bdk = alloc(nc, st, "b_bdk", [128, 16, 128], BF16)
        bdv = alloc(nc, st, "b_bdv", [128, 16, 128], BF16)
        wout = alloc(nc, st, "b_wout", [128, 16, D], BF16)
        hnw = alloc(nc, st, "b_hnw", [128, AI], F32)
        skp = alloc(nc, st, "b_skip", [128, 16], F32)
        gpost = alloc(nc, st, "b_gpost", [128, D], F32)
        C32 = alloc(nc, st, "b_C32", [128, 16, 512], F32)
        Cbf = alloc(nc, st, "b_Cbf", [128, 16, 512], BF16)
        n32 = alloc(nc, st, "b_n32", [128, 16], F32)
        nbf = alloc(nc, st, "b_nbf", [128, 16], BF16)
        xc = [alloc(nc, st, "b_xc%d" % i, [128, 16, 128], BF16) for i in range(2)]
        xm = [alloc(nc, st, "b_xm%d" % i, [128, 16, 128], BF16) for i in range(2)]
        sz = [alloc(nc, st, "b_sz%d" % i, [128, 16, 128], BF16) for i in range(2)]
        so = [alloc(nc, st, "b_so%d" % i, [128, AI], BF16) for i in range(2)]
        gt = [alloc(nc, st, "b_gt%d" % i, [128, 8], F32) for i in range(2)]
        xin = [alloc(nc, st, "b_xin0", [128, D], F32)] * 2
        xout = [alloc(nc, st, "b_xout0", [128, D], F32)] * 2
        qT = alloc(nc, st, "b_qT", [128, 16, 128], BF16)
        qsT = alloc(nc, st, "b_qsT", [128, 16, 128], BF16)
        kT = alloc(nc, st, "b_kT", [128, 16, 128], BF16)
        ktok = alloc(nc, st, "b_ktok", [128, AI], BF16)
        kw = alloc(nc, st, "b_kw", [128, AI], BF16)
        vtok = alloc(nc, st, "b_vtok", [128, AI], BF16)
        lf = alloc(nc, st, "b_lf", [128, 4], F32)
        lfrep = alloc(nc, st, "b_lfrep", [128, 4, 128], F32)
        av = alloc(nc, st, "b_a", [128, 4], F32)
        wv = alloc(nc, st, "b_w", [128, 4], F32)
        ebt = alloc(nc, st, "b_ebt", [128, 4, 128], F32)
        logd = alloc(nc, st, "b_logd", [128, 4, 128], F32)
        dT = alloc(nc, st, "b_dT", [128, 4, 128], F32)
        pT = alloc(nc, st, "b_pT", [128, 4, 128], BF16)
        den = alloc(nc, st, "b_den", [128, 4], F32)
        hc = alloc(nc, st, "b_hc", [128, 512], F32)
        bst = alloc(nc, st, "b_bst", [128, 6], F32)
        mv = alloc(nc, st, "b_mv", [128, 2], F32)
        hn = alloc(nc, st, "b_hn", [128, AI], BF16)
        xs = alloc(nc, st, "b_xs", [128, 16, 128], BF16)
        yT = alloc(nc, st, "b_yT", [128, 16, 128], BF16)
        tmpf = alloc(nc, st, "b_tmpf", [128, 4, 128], F32)
        onesb = alloc(nc, st, "b_ones", [128, 1], BF16)
        pA = [palloc(nc, st, "b_pA%d" % i, [128, 512], F32) for i in range(4)]
        pS = palloc(nc, st, "b_pS", [128, 512], F32)
        pB = palloc(nc, st, "b_pB", [128, 512], F32)
        pD = palloc(nc, st, "b_pD", [128, 512], F32)
        pTb = palloc(nc, st, "b_pTb", [128, 1024], BF16)

        DMA(S, "pool", bdq[:], lw["bdq"], writes=["bdq"])
        DMA(S, "pool", bdk[:], lw["bdk"], writes=["bdk"])
        DMA(S, "pool", bdv[:], lw["bdv"], writes=["bdv"])
        DMA(S, "pool", wout[:], lw["w_out"].rearrange("(j p) c -> p j c", p=128), writes=["wout"])
        DMA(S, "sp", hnw[:], lw["head_norm"], writes=["hnw"])
        DMA(S, "sp", skp[:], lw["skip"], writes=["skp"])
        DMA(S, "sp", gpost[:], lw["gpost"], writes=["gpost"])
        S.op("pool", lambda e: e.memset(C32[:], 0.0), [], ["C32"])
        S.op("pool", lambda e: e.memset(Cbf[:], 0.0), [], ["Cbf"])
        S.op("pool", lambda e: e.memset(n32[:], 0.0), [], ["n32"])
        S.op("pool", lambda e: e.memset(nbf[:], 0.0), [], ["nbf"])
        S.op("pool", lambda e: e.memset(onesb[:], 1.0), [], ["onesb"])

        def loads(c):
            b = c % 2
            DMA(S, "sp", xc[b][:].rearrange("p j t -> p (j t)"), XC[c], writes=["xc%d" % b])
            DMA(S, "sp", xm[b][:].rearrange("p j t -> p (j t)"), XM[c], writes=["xm%d" % b])
            DMA(S, "sp", sz[b][:].rearrange("p j t -> p (j t)"), SZ[c], writes=["sz%d" % b])
            DMA(S, "sp", so[b][:], SO[c * 128:(c + 1) * 128, :], writes=["so%d" % b])
            DMA(S, "sp", gt[b][:], G[c * 128:(c + 1) * 128, :], writes=["gt%d" % b])

        loads(0)
        DMA(S, "sp", xin[0][:], xsrc[0:128, :], writes=["xin0"])
        npa = 0
        for c in range(NCH):
            b = c % 2
            if c + 1 < NCH:
                loads(c + 1)
            xck, xmk, szk, sok, gtk = "xc%d" % b, "xm%d" % b, "sz%d" % b, "so%d" % b, "gt%d" % b
            for h in range(4):
                for (dst, bd, src, srck, dk_, scale) in ((qT, bdq, xc[b], xck, "qT", None),
                                                         (kT, bdk, xc[b], xck, "kT", 512.0 ** -0.5)):
                    p = pA[npa % 4]
                    pk = "PS:pA%d" % (npa % 4)
                    npa += 1
                    for jj in range(4):
                        j = h * 4 + jj
                        MM(S, p[:, jj * 128:(jj + 1) * 128], bd[:, j, :], src[:, j, :], True, True,
                           ["bdq", "bdk", srck], [pk], inc=(jj == 3))
                    if scale is None:
                        CP(S, "act", dst[:, h * 4:(h + 1) * 4, :], p[:].rearrange("p (a t) -> p a t", a=4), [pk],
                           ["%s%d" % (dk_, h)])
                    else:
                        S.op("act", lambda e, dst=dst, p=p, h=h, scale=scale: e.mul(
                            out=dst[:, h * 4:(h + 1) * 4, :], in_=p[:].rearrange("p (a t) -> p a t", a=4), mul=scale),
                            [pk], ["%s%d" % (dk_, h)])
                for (dst, bd, src, srck, dk_, scale) in ((ktok, bdk, xc[b], xck, "ktok", 512.0 ** -0.5),
                                                         (vtok, bdv, xm[b], xmk, "vtok", None)):
                    p = pA[npa % 4]
                    pk = "PS:pA%d" % (npa % 4)
                    npa += 1
                    for jj in range(4):
                        j = h * 4 + jj
                        MM(S, p[:, jj * 128:(jj + 1) * 128], src[:, j, :], bd[:, j, :], True, True,
                           ["bdk", "bdv", srck], [pk], inc=(jj == 3))
                    if scale is None:
                        CP(S, "dve", dst[:, h * 512:(h + 1) * 512], p[:], [pk], ["%s%d" % (dk_, h)])
                    else:
                        TS(S, "dve", dst[:, h * 512:(h + 1) * 512], p[:], scale, None, ALU.mult, None, [pk],
                           ["%s%d" % (dk_, h)])
            ACT(S, lf[:], gt[b][:, 4:8], AF.Exp, [gtk], ["lf"], scale=-1.0)
            ACT(S, lf[:], lf[:], AF.Ln, ["lf"], ["lf"], bias=1.0)
            TS(S, "dve", lf[:], lf[:], -1.0, None, ALU.mult, None, ["lf"], ["lf"])
            CP(S, "dve", lfrep[:], lf[:].unsqueeze(2).to_broadcast([128, 4, 128]), ["lf"], ["lfrep"])
            MM(S, pD[:, 8:12], C.tri[:], lf[:], True, True, ["lf"], ["PS:pD"])
            TT(S, "dve", av[:], gt[b][:, 0:4], pD[:, 8:12], ALU.subtract, [gtk, "PS:pD"], ["av"])
            for h in range(4):
                MM(S, pB[:, h * 128:(h + 1) * 128], lfrep[:, h, :], C.tri[:], True, True, ["lfrep"], ["PS:pB"], inc=(h == 3))
            pB3 = pB[:].rearrange("p (h t) -> p h t", h=4)
            ACT(S, ebt[:], pB3, AF.Exp, ["PS:pB"], ["ebt"])
            TT(S, "dve", logd[:], pB3, C.maskb[:].unsqueeze(1).to_broadcast([128, 4, 128]), ALU.add, ["PS:pB"], ["logd"])
            TT(S, "dve", wv[:], av[:], pB3[:, :, 127], ALU.add, ["av", "PS:pB"], ["wv"])
            ACT(S, wv[:], wv[:], AF.Exp, ["wv"], ["wv"])
            for h in range(4):
                ACT(S, dT[:, h, :], logd[:, h, :], AF.Exp, ["logd", "av"], ["dT"], bias=av[:, h:h + 1])
            for h in range(4):
                for jj in range(4):
                    j = h * 4 + jj
                    MM(S, pS[:, h * 128:(h + 1) * 128], kT[:, j, :], qT[:, j, :], jj == 0, jj == 3,
                       ["kT%d" % h, "qT%d" % h], ["PS:pS"], inc=(h == 3 and jj == 3))
            TT(S, "dve", pT[:], pS[:].rearrange("p (h t) -> p h t", h=4), dT[:], ALU.mult, ["PS:pS", "dT"], ["pT"])
            for h in range(4):
                TT(S, "pool", qsT[:, h * 4:(h + 1) * 4, :], qT[:, h * 4:(h + 1) * 4, :],
                   ebt[:, h, :].unsqueeze(1).to_broadcast([128, 4, 128]), ALU.mult, ["qT%d" % h, "ebt"], ["qsT%d" % h])
            for h in range(4):
                MM(S, pD[:, h:h + 1], pT[:, h, :], onesb[:], True, False, ["pT", "onesb"], ["PS:pD"], inc=False)
                for jj in range(4):
                    j = h * 4 + jj
                    MM(S, pD[:, h:h + 1], qsT[:, j, :], nbf[:, j:j + 1], False, jj == 3, ["qsT%d" % h, "nbf"], ["PS:pD"],
                       inc=(h == 3 and jj == 3))
            ACT(S, den[:], pD[:, 0:4], AF.Abs, ["PS:pD"], ["den"])
            TS(S, "dve", den[:], den[:], 1.0, None, ALU.max, None, ["den"], ["den"])
            S.op("dve", lambda e: e.reciprocal(out=den[:], in_=den[:]), ["den"], ["den"])
            for h in range(4):
                p = pA[npa % 4]
                pk = "PS:pA%d" % (npa % 4)
                npa += 1
                MM(S, p[:], pT[:, h, :], vtok[:, h * 512:(h + 1) * 512], True, False, ["pT", "vtok%d" % h], [pk], inc=False)
                for jj in range(4):
                    j = h * 4 + jj
                    MM(S, p[:], qsT[:, j, :], Cbf[:, j, :], False, jj == 3, ["qsT%d" % h, "Cbf%d" % h], [pk])
                STT(S, "dve", hc[:], p[:], den[:, h:h + 1], so[b][:, h * 512:(h + 1) * 512], ALU.mult, ALU.mult,
                    [pk, "den", sok], ["hc"])
                S.op("dve", lambda e: e.bn_stats(out=bst[:], in_=hc[:]), ["hc"], ["bst"])
                S.op("dve", lambda e: e.bn_aggr(out=mv[:], in_=bst[:]), ["bst"], ["mv"])
                TS(S, "dve", mv[:, 1:2], mv[:, 1:2], EPS, None, ALU.add, None, ["mv"], ["mv"])
                ACT(S, mv[:, 1:2], mv[:, 1:2], AF.Sqrt, ["mv"], ["mv"])
                S.op("dve", lambda e: e.reciprocal(out=mv[:, 1:2], in_=mv[:, 1:2]), ["mv"], ["mv"])
                TS(S, "dve", hc[:], hc[:], mv[:, 0:1], mv[:, 1:2], ALU.subtract, ALU.mult, ["hc", "mv"], ["hc"])
                TT(S, "dve", hn[:, h * 512:(h + 1) * 512], hc[:], hnw[:, h * 512:(h + 1) * 512], ALU.mult,
                   ["hc", "hnw"], ["hn%d" % h])
                for jj in range(4):
                    TR(S, pTb[:, (h % 2) * 512 + jj * 128:(h % 2) * 512 + (jj + 1) * 128],
                       hn[:, h * 512 + jj * 128:h * 512 + (jj + 1) * 128], C.ident[:], ["hn%d" % h],
                       ["PS:pTb"], inc=(jj == 3))
                TT(S, "pool", xs[:, h * 4:(h + 1) * 4, :], xc[b][:, h * 4:(h + 1) * 4, :],
                   skp[:, h * 4:(h + 1) * 4].unsqueeze(2).to_broadcast([128, 4, 128]), ALU.mult, [xck, "skp"],
                   ["xs%d" % h])
                TT(S, "dve", tmpf[:], pTb[:, (h % 2) * 512:(h % 2 + 1) * 512].rearrange("p (a t) -> p a t", a=4),
                   xs[:, h * 4:(h + 1) * 4, :], ALU.add, ["PS:pTb", "xs%d" % h], ["tmpf"])
                TT(S, "dve", yT[:, h * 4:(h + 1) * 4, :], tmpf[:], sz[b][:, h * 4:(h + 1) * 4, :], ALU.mult,
                   ["tmpf", szk], ["yT%d" % h])
            py = [pA[npa % 4], pA[(npa + 1) % 4]]
            pyk = ["PS:pA%d" % (npa % 4), "PS:pA%d" % ((npa + 1) % 4)]
            npa += 2
            for hh in range(2):
                for j in range(16):
                    MM(S, py[hh][:], yT[:, j, :], wout[:, j, hh * 512:(hh + 1) * 512], j == 0, j == 15,
                       ["yT%d" % (j // 4), "wout"], [pyk[hh]])
            post_residual(S, P, py, pyk, xin[b], "xin0", gpost, xout[b], "xout0")
            DMA(S, "sp", xdst[c * 128:(c + 1) * 128, :], xout[b][:], reads=["xout0"])
            if c + 1 < NCH:
                DMA(S, "sp", xin[0][:], xsrc[(c + 1) * 128:(c + 2) * 128, :], writes=["xin0"])
            for h in range(4):
                TS(S, "pool", kw[:, h * 512:(h + 1) * 512], ktok[:, h * 512:(h + 1) * 512], wv[:, h:h + 1], None,
                   ALU.mult, None, ["ktok%d" % h, "wv"], ["kw%d" % h])
            for h in range(4):
                for jj in range(4):
                    j = h * 4 + jj
                    MM(S, pD[:, 16 + j:17 + j], kw[:, j * 128:(j + 1) * 128], onesb[:], True, True, ["kw%d" % h, "onesb"],
                       ["PS:pD"], inc=(h == 3 and jj == 3))
            for h in range(4):
                STT(S, "dve", n32[:, h * 4:(h + 1) * 4], n32[:, h * 4:(h + 1) * 4], ebt[:, h, 127:128],
                    pD[:, 16 + h * 4:16 + (h + 1) * 4], ALU.mult, ALU.add, ["n32", "ebt", "PS:pD"], ["n32"])
            CP(S, "dve", nbf[:], n32[:], ["n32"], ["nbf"])
            for h in range(4):
                for jj in range(4):
                    j = h * 4 + jj
                    p = pA[npa % 4]
                    pk = "PS:pA%d" % (npa % 4)
                    npa += 1
                    MM(S, p[:], kw[:, j * 128:(j + 1) * 128], vtok[:, h * 512:(h + 1) * 512], True, True,
                       ["kw%d" % h, "vtok%d" % h], [pk])
                    STT(S, "dve", C32[:, j, :], C32[:, j, :], ebt[:, h, 127:128], p[:], ALU.mult, ALU.add,
                        ["C32_%d" % j, "ebt", pk], ["C32_%d" % j])
                    CP(S, "act", Cbf[:, j, :], C32[:, j, :], ["C32_%d" % j], ["Cbf%d" % h])
        S.barrier()
        S.emit()


def host_consts():
    c = {}
    c["ident"] = np.eye(128, dtype=np.float32)
    s = np.arange(128)
    c["tri"] = (s[:, None] <= s[None, :]).astype(np.float32)
    c["maskb"] = np.where(s[:, None] <= s[None, :], 0.0, NEG).astype(np.float32)
    return c


def blockdiag_layout(w):
    bd = np.zeros((16, 128, 128), np.float32)
    wj = w.reshape(16, 32, 4, 4)
    for g in range(32):
        bd[:, g * 4:(g + 1) * 4, g * 4:(g + 1) * 4] = wj[:, g]
    return (np.ascontiguousarray(bd.transpose(1, 0, 2)), np.ascontiguousarray(bd.transpose(2, 0, 1)))


def layer_a_layout(inp, l):
    o = {}
    o["w_in"] = np.ascontiguousarray(inp["a_w_in"][l])
    o["gfold"] = np.ascontiguousarray(inp["norm_pre"][l].reshape(8, 128).T)
    o["conv_w"] = np.ascontiguousarray(inp["a_conv_w"][l].reshape(4, 16, 128).transpose(2, 1, 0))
    o["conv_b"] = np.ascontiguousarray(inp["a_conv_b"][l].reshape(16, 128).T)
    for n, k in (("q", "a_w_q"), ("k", "a_w_k"), ("v", "a_w_v")):
        bd, bdT = blockdiag_layout(inp[k][l])
        o["bd" + n] = bd
        o["bd" + n + "T"] = bdT
    o["w_gate"] = np.ascontiguousarray(inp["a_w_gate"][l].reshape(48, 128, 8).transpose(1, 0, 2))
    o["b_gate"] = np.ascontiguousarray(np.broadcast_to(inp["a_b_gate"][l][None, :], (128, 8)))
    o["head_norm"] = np.ascontiguousarray(np.broadcast_to(inp["a_head_norm"][l][None, :], (128, AI)))
    o["skip"] = np.ascontiguousarray(inp["a_skip"][l].reshape(16, 128).T)
    o["gpost"] = np.ascontiguousarray(np.broadcast_to(inp["norm_post"][l][None, :], (128, D)))
    o["w_out"] = np.ascontiguousarray(inp["a_w_out"][l])
    return o


def load_consts(nc, S, st, cdr):
    C = Ctx()
    C.ident = alloc(nc, st, "k_ident", [128, 128], BF16)
    C.tri = alloc(nc, st, "k_tri", [128, 128], F32)
    C.maskb = alloc(nc, st, "k_maskb", [128, 128], F32)
    DMA(S, "pool", C.ident[:], cdr["ident"], writes=["c_ident"])
    DMA(S, "sp", C.tri[:], cdr["tri"], writes=["c_tri"])
    DMA(S, "sp", C.maskb[:], cdr["maskb"], writes=["c_maskb"])
    S.barrier()
    S.emit()
    return C


def build_program(T, host_arrays, n_a_layers=2, n_b_layers=2, out_name="out", debug=False, skip=()):
    nc = bass.Bass("TRN2", target_bir_lowering=False)
    dr = {}
    for name, arr in host_arrays.items():
        dr[name] = nc.dram_tensor(name, list(arr.shape), F32, kind="ExternalInput").ap()
    out = nc.dram_tensor(out_name, [T, D], F32, kind="ExternalOutput").ap()
    NCH = T // 128

    def scratch(name, shape, dt):
        if debug:
            return nc.dram_tensor(name, list(shape), dt, kind="ExternalOutput").ap()
        return nc.dram_tensor(name, list(shape), dt).ap()

    XM = scratch("s_xm", [NCH, 128, 16 * 128], BF16)
    XC = scratch("s_xc", [NCH, 128, 16 * 128], BF16)
    SZ = scratch("s_sz", [NCH, 128, 16 * 128], BF16)
    XM4 = XM.rearrange("c p (j t) -> c p j t", j=16)
    XC4 = XC.rearrange("c p (j t) -> c p j t", j=16)
    SZ4 = SZ.rearrange("c p (j t) -> c p j t", j=16)
    SO = scratch("s_so", [T, AI], BF16)
    G = scratch("s_g", [T, 8], F32)
    nlay = n_a_layers + n_b_layers
    XS = [scratch("s_x%d" % i, [T, D], F32) for i in range(max(nlay - 1, 0))]
    with ExitStack() as gst:
        S = Sched(nc, gst)
        cdr = {k[2:]: v for k, v in dr.items() if k.startswith("c_")}
        C = load_consts(nc, S, gst, cdr)
        xs = dr["x"]
        li = 0
        for l in range(n_a_layers):
            lw = {k[3:]: v for k, v in dr.items() if k.startswith("a%d_" % l)}
            xd = out if li == nlay - 1 else XS[li]
            pass_A1(nc, S, C, T, xs, lw, XM4, XC4, G)
            pass_A2(nc, S, C, T, xs, lw, SZ4, SO)
            pass_B(nc, S, C, T, xs, xd, lw, XM, XC, SZ, SO, G)
            xs = xd
            li += 1
        if n_b_layers > 0:
            N = {k[2:]: v for k, v in dr.items() if k.startswith("n_")}
            kw_ = {k[3:]: v for k, v in dr.items() if k.startswith("kv_")}
            KST = [scratch("s_kst%d" % g, [128, T], BF16) for g in range(2)]
            KWT = [scratch("s_kwt%d" % g, [128, T], BF16) for g in range(2)]
            VS = [scratch("s_vs%d" % g, [T, 128], BF16) for g in range(2)]
            VW = [scratch("s_vw%d" % g, [T, 128], BF16) for g in range(2)]
            KCT = [scratch("s_kct%d" % g, [128, 512], BF16) for g in range(2)]
            VC = [scratch("s_vc%d" % g, [512, 128], BF16) for g in range(2)]
            QT = scratch("s_qt", [NCH, 128, NH * 128], BF16)
            GT = scratch("s_gt", [T, 48], F32)
            SZG = scratch("s_szg", [T, ZW], BF16)
            if "kv" not in skip:
                pass_KV(nc, S, C, T, xs, kw_, KST, KWT, VS, VW, KCT, VC)
            for lb in range(n_b_layers):
                lw = {k[3:]: v for k, v in dr.items() if k.startswith("b%d_" % lb)}
                xd = out if li == nlay - 1 else XS[li]
                if "q" not in skip:
                    pass_Q(nc, S, C, T, xs, lw, QT, GT)
                if "z" not in skip:
                    pass_Z(nc, S, C, T, xs, lw, SZG, 0)
                    pass_Z(nc, S, C, T, xs, lw, SZG, 1)
                if "att" not in skip:
                    pass_ATT(nc, S, C, T, xs, xd, lw, N, QT, GT, SZG, KST, KWT, VS, VW, KCT, VC)
                xs = xd
                li += 1
        print("instructions:", S.ninst)
    return nc


def make_host_arrays(inp, b, T, n_a_layers=2, n_b_layers=2, x_override=None):
    ha = {"x": np.ascontiguousarray(inp["x"][b, :T]) if x_override is None else x_override}
    for k, v in host_consts().items():
        ha["c_" + k] = v
    for l in range(n_a_layers):
        for k, v in layer_a_layout(inp, l).items():
            ha["a%d_%s" % (l, k)] = v
    if n_b_layers > 0:
        for k, v in nsa_consts(T).items():
            ha["n_" + k] = v
        for k, v in nsa_layout(inp).items():
            ha["kv_" + k] = v
        for lb in range(n_b_layers):
            for k, v in layer_b_layout(inp, lb, 2 + lb).items():
                ha["b%d_%s" % (lb, k)] = v
    return ha


_SHARED = ("c_", "a0_", "a1_", "n_", "kv_", "b0_", "b1_")


def kernel(**inputs):
    inp = {k: np.asarray(v) for k, v in inputs.items()}
    T = inp["x"].shape[1]
    base = make_host_arrays(inp, 0, T)
    maps = []
    for b in range(8):
        m = dict(base)
        m["x"] = np.ascontiguousarray(inp["x"][b, :T])
        maps.append(m)
    nc = build_program(T, base)
    res = run_bass_kernel_spmd(nc, maps, core_ids=list(range(8)))
    return np.stack([r["out"] for r in res.results], axis=0).astype(np.float32)


NH = 16
ZW = 6144
BIG = 1.0e30


def nsa_consts(T):
    c = {}
    i = np.arange(128)
    kk = np.arange(128)
    c["cdiag"] = (kk[:, None] <= i[None, :]).astype(np.float32)
    c["cwin"] = (kk[:, None] > i[None, :]).astype(np.float32)
    h = np.arange(1, NH + 1, dtype=np.float64)
    slopes = 2.0 ** (-8.0 * h / NH)
    rel = np.arange(-63, 1)
    ab = slopes[None, None, :] * (128.0 * rel[None, :, None] + kk[:, None, None] - 63.5)
    c["ab"] = np.exp(ab).astype(np.float32)
    abc = slopes[None, None, :] * (128.0 * rel[None, :, None] + 16.0 * kk[:, None, None] - 48.0)
    c["abc"] = np.exp(np.minimum(abc, 40.0)).astype(np.float32)
    cm = np.zeros((128, 17, 128), np.float32)
    for r in range(-16, 1):
        cm[:, r + 16, :] = ((16 * kk[:, None] + 31 - i[None, :]) <= (-128 * r)).astype(np.float32)
    c["cm"] = cm
    wov = np.zeros((128, 33), np.float32)
    for n in range(128):
        for j in range(33):
            ov = min(16 * n + 32, 64 * j + 64) - max(16 * n, 64 * j)
            wov[n, j] = max(ov, 0) / 16.0
    c["wov"] = wov
    vm = np.zeros((128, 256), np.float32)
    fb = np.zeros((128, 256), np.float32)
    for ii in range(128):
        cur = 1 if ii >= 64 else 0
        for col in range(256):
            jr = col - 127
            if jr == cur or jr == cur - 1:
                fb[ii, col] = BIG
            elif jr < cur:
                vm[ii, col] = 1.0
            else:
                fb[ii, col] = -BIG
    c["vm"] = vm
    c["fb"] = fb
    return c


def nsa_layout(inp):
    o = {}
    o["kv_w"] = np.ascontiguousarray(inp["kv_w"])
    o["kv_gf"] = np.ascontiguousarray(inp["kv_norm"].reshape(8, 128).T)
    for s_ in range(2):
        o["w1_%d" % s_] = np.ascontiguousarray(inp["cmp_w1"][s_])
        o["posT_%d" % s_] = np.ascontiguousarray(inp["cmp_pos"][s_].T)
        o["b1_%d" % s_] = np.ascontiguousarray(inp["cmp_b1"][s_].reshape(2, 128).T)
        o["w2_%d" % s_] = np.ascontiguousarray(inp["cmp_w2"][s_])
    o["b2k"] = np.ascontiguousarray(inp["cmp_b2"][0].reshape(128, 1))
    o["b2v"] = np.ascontiguousarray(np.broadcast_to(inp["cmp_b2"][1][None, :], (128, 128)))
    return o


def layer_b_layout(inp, lb, layer):
    o = {}
    o["w_in"] = np.ascontiguousarray(inp["b_w_in"][lb])
    o["gfold"] = np.ascontiguousarray(inp["norm_pre"][layer].reshape(8, 128).T)
    o["w_out"] = np.ascontiguousarray(inp["b_w_out"][lb])
    o["gpost"] = np.ascontiguousarray(np.broadcast_to(inp["norm_post"][layer][None, :], (128, D)))
    return o


def load_w(S, W, gf, wsrc, gsrc, c0, c1):
    DMA(S, "pool", W[:], wsrc.rearrange("(k p) c -> p k c", p=128)[:, :, c0:c1], writes=["W"])
    DMA(S, "sp", gf[:], gsrc, writes=["gf"])
    for k in range(8):
        TS(S, "dve", W[:, k, :], W[:, k, :], gf[:, k:k + 1], None, ALU.mult, None, ["W", "gf"], ["W"])


def pass_KV(nc, S, C, T, xsrc, kw_, KST, KWT, VS, VW, KCT, VC):
    NST = T // 512
    NC = T // 16 - 1
    NNT = (NC + 127) // 128
    with ExitStack() as st:
        R = alloc_normT(nc, st, C)
        W = alloc(nc, st, "kv_w", [128, 8, 1536], BF16)
        gf = alloc(nc, st, "kv_gf", [128, 8], F32)
        aT = [alloc(nc, st, "kv_aT%d" % i, [128, T], BF16) for i in range(4)]
        kst = [alloc(nc, st, "kv_kst%d" % i, [128, 4, 512], BF16) for i in range(2)]
        vst = [alloc(nc, st, "kv_vst%d" % i, [128, 4, 4, 128], BF16) for i in range(2)]
        pm = [palloc(nc, st, "kv_pm%d" % i, [128, 512], F32) for i in range(4)]
        load_w(S, W, gf, kw_["kv_w"], kw_["kv_gf"], 0, 1536)
        load_x(S, R, xsrc, 0)
        n = 0
        for s_ in range(NST):
            b = s_ % 2
            if s_ + 1 < NST:
                load_x(S, R, xsrc, s_ + 1)
            hT, hk = norm_T(S, R, s_)
            fi = 0
            for cb in (0, 1, 2, 3, 4, 5, 8, 9):
                p = pm[n % 4]
                pk = "PS:pm%d" % (n % 4)
                n += 1
                for k in range(8):
                    MM(S, p[:], W[:, k, cb * 128:(cb + 1) * 128], hT[:, k, :], k == 0, k == 7, ["W", hk[k]], [pk])
                if cb < 4:
                    CP(S, "act" if cb % 2 == 0 else "dve", aT[cb][:, s_ * 512:(s_ + 1) * 512], p[:], [pk], ["aT%d" % cb])
                else:
                    CP(S, "act" if cb % 2 == 0 else "dve", kst[b][:, fi, :], p[:], [pk], ["kst%d" % b])
                    fi += 1
            for a in range(4):
                p = pm[n % 4]
                pk = "PS:pm%d" % (n % 4)
                n += 1
                for bi, cb in enumerate((6, 7, 10, 11)):
                    for k in range(8):
                        MM(S, p[:, bi * 128:(bi + 1) * 128], hT[:, k, a * 128:(a + 1) * 128], W[:, k, cb * 128:(cb + 1) * 128],
                           k == 0, k == 7, ["W", hk[k]], [pk], inc=(k == 7 and bi == 3))
                CP(S, "act" if a % 2 == 0 else "dve", vst[b][:, a, :, :], p[:].rearrange("p (b d) -> p b d", b=4), [pk],
                   ["vst%d" % b])
            sl = slice(s_ * 512, (s_ + 1) * 512)
            for g in range(2):
                DMA(S, "sp", KST[g][:, sl], kst[b][:, g, :], reads=["kst%d" % b])
                DMA(S, "sp", KWT[g][:, sl], kst[b][:, 2 + g, :], reads=["kst%d" % b])
                DMA(S, "sp", VS[g][sl, :].rearrange("(a p) d -> p a d", p=128), vst[b][:, :, g, :], reads=["vst%d" % b])
                DMA(S, "sp", VW[g][sl, :].rearrange("(a p) d -> p a d", p=128), vst[b][:, :, 2 + g, :], reads=["vst%d" % b])
        w1 = alloc(nc, st, "kv_w1", [128, 32, 256], BF16)
        w2 = alloc(nc, st, "kv_w2", [128, 2, 128], BF16)
        posT = alloc(nc, st, "kv_posT", [128, 32], BF16)
        b1 = alloc(nc, st, "kv_b1", [128, 2], F32)
        c1 = alloc(nc, st, "kv_c1", [128, 2], F32)
        b2k = alloc(nc, st, "kv_b2k", [128, 1], F32)
        b2v = alloc(nc, st, "kv_b2v", [128, 128], F32)
        hid = alloc(nc, st, "kv_hid", [128, 2, 512], BF16)
        kco = alloc(nc, st, "kv_kco", [128, 512], BF16)
        vco = alloc(nc, st, "kv_vco", [128, 4, 128], BF16)
        DMA(S, "sp", b2k[:], kw_["b2k"], writes=["b2k"])
        DMA(S, "sp", b2v[:], kw_["b2v"], writes=["b2v"])
        S.op("pool", lambda e: e.memset(hid[:], 0.0), [], ["hid"])
        S.op("pool", lambda e: e.memset(kco[:], 0.0), [], ["kco"])
        for slot in range(2):
            DMA(S, "pool", w1[:], kw_["w1_%d" % slot].rearrange("(i d) m -> d i m", d=128), writes=["w1"])
            DMA(S, "pool", w2[:], kw_["w2_%d" % slot].rearrange("(c m) d -> m c d", m=128), writes=["w2"])
            DMA(S, "pool", posT[:], kw_["posT_%d" % slot], writes=["posT"])
            DMA(S, "sp", b1[:], kw_["b1_%d" % slot], writes=["b1"])
            for mc in range(2):
                p = pm[n % 4]
                pk = "PS:pm%d" % (n % 4)
                n += 1
                for i in range(32):
                    MM(S, p[:, 0:1], w1[:, i, mc * 128:(mc + 1) * 128], posT[:, i:i + 1], i == 0, i == 31, ["w1", "posT"], [pk])
                TT(S, "dve", c1[:, mc:mc + 1], p[:, 0:1], b1[:, mc:mc + 1], ALU.add, [pk, "b1"], ["c1"])
            for g in range(2):
                src = aT[slot * 2 + g]
                for mc in range(2):
                    p = pm[n % 4]
                    pk = "PS:pm%d" % (n % 4)
                    n += 1
                    for i in range(32):
                        MM(S, p[:, 0:NC], w1[:, i, mc * 128:(mc + 1) * 128], src[:, i:i + 16 * (NC - 1) + 1:16], i == 0, i == 31,
                           ["w1", "aT%d" % (slot * 2 + g)], [pk])
                    ACT(S, hid[:, mc, 0:NC], p[:, 0:NC], AF.Silu, [pk, "c1"], ["hid"], bias=c1[:, mc:mc + 1])
                if slot == 0:
                    p = pm[n % 4]
                    pk = "PS:pm%d" % (n % 4)
                    n += 1
                    for mc in range(2):
                        MM(S, p[:, 0:NC], w2[:, mc, :], hid[:, mc, 0:NC], mc == 0, mc == 1, ["w2", "hid"], [pk])
                    TS(S, "dve", kco[:, 0:NC], p[:, 0:NC], b2k[:, 0:1], None, ALU.add, None, [pk, "b2k"], ["kco"])
                    DMA(S, "sp", KCT[g], kco[:], reads=["kco"])
                else:
                    p = pm[n % 4]
                    pk = "PS:pm%d" % (n % 4)
                    n += 1
                    for nt in range(NNT):
                        for mc in range(2):
                            MM(S, p[:, nt * 128:(nt + 1) * 128], hid[:, mc, nt * 128:(nt + 1) * 128], w2[:, mc, :], mc == 0, mc == 1,
                               ["w2", "hid"], [pk], inc=(mc == 1 and nt == NNT - 1))
                    TT(S, "dve", vco[:, 0:NNT, :], p[:, 0:NNT * 128].rearrange("p (t d) -> p t d", d=128),
                       b2v[:].unsqueeze(1).to_broadcast([128, NNT, 128]), ALU.add, [pk, "b2v"], ["vco"])
                    DMA(S, "sp", VC[g][0:NNT * 128, :].rearrange("(t p) d -> p t d", p=128), vco[:, 0:NNT, :], reads=["vco"])
        S.barrier()
        S.emit()


def pass_Q(nc, S, C, T, xsrc, lw, QT, GT):
    NST = T // 512
    with ExitStack() as st:
        R = alloc_normT(nc, st, C)
        W = alloc(nc, st, "q_w", [128, 8, 2096], BF16)
        gf = alloc(nc, st, "q_gf", [128, 8], F32)
        q_st = [alloc(nc, st, "q_st%d" % i, [128, 4, 16, 128], BF16) for i in range(2)]
        g_st = [alloc(nc, st, "q_gst%d" % i, [128, 4, 48], F32) for i in range(2)]
        pm = [palloc(nc, st, "q_pm%d" % i, [128, 512], F32) for i in range(4)]
        pgf = palloc(nc, st, "q_pg", [128, 512], F32)
        pg = pgf[:, 0:192].rearrange("p (a g) -> p a g", a=4)
        load_w(S, W, gf, lw["w_in"], lw["gfold"], 0, 2096)
        load_x(S, R, xsrc, 0)
        n = 0
        for s_ in range(NST):
            b = s_ % 2
            if s_ + 1 < NST:
                load_x(S, R, xsrc, s_ + 1)
            hT, hk = norm_T(S, R, s_)
            for hd in range(NH):
                p = pm[n % 4]
                pk = "PS:pm%d" % (n % 4)
                n += 1
                for k in range(8):
                    MM(S, p[:], W[:, k, hd * 128:(hd + 1) * 128], hT[:, k, :], k == 0, k == 7, ["W", hk[k]], [pk])
                S.op("act", lambda e, p=p, hd=hd, b=b: e.mul(out=q_st[b][:, :, hd, :],
                                                            in_=p[:].rearrange("p (a t) -> p a t", a=4), mul=128.0 ** -0.5),
                     [pk], ["qst%d" % b])
            for a in range(4):
                for k in range(8):
                    MM(S, pg[:, a, :], hT[:, k, a * 128:(a + 1) * 128], W[:, k, 2048:2096], k == 0, k == 7, ["W", hk[k]],
                       ["PS:pg"], inc=(k == 7 and a == 3))
            ACT(S, g_st[b][:], pg[:], AF.Sigmoid, ["PS:pg"], ["gst%d" % b])
            DMA(S, "sp", QT[s_ * 4:(s_ + 1) * 4].rearrange("a p (j t) -> p a j t", j=16), q_st[b][:], reads=["qst%d" % b])
            DMA(S, "sp", GT[s_ * 512:(s_ + 1) * 512, :].rearrange("(a p) g -> p a g", p=128), g_st[b][:], reads=["gst%d" % b])
        S.barrier()
        S.emit()


def pass_Z(nc, S, C, T, xsrc, lw, SZG, hz):
    NST = T // 512
    with ExitStack() as st:
        R = alloc_normT(nc, st, C)
        W = alloc(nc, st, "z_w", [128, 8, 3072], BF16)
        gf = alloc(nc, st, "z_gf", [128, 8], F32)
        z_st = [alloc(nc, st, "z_st%d" % i, [128, 4, 3072], BF16) for i in range(2)]
        pm = [palloc(nc, st, "z_pm%d" % i, [128, 512], F32) for i in range(4)]
        c0 = 2096 + hz * 3072
        load_w(S, W, gf, lw["w_in"], lw["gfold"], c0, c0 + 3072)
        load_x(S, R, xsrc, 0)
        n = 0
        for s_ in range(NST):
            b = s_ % 2
            if s_ + 1 < NST:
                load_x(S, R, xsrc, s_ + 1)
            hT, hk = norm_T(S, R, s_)
            for a in range(4):
                for cg in range(6):
                    p = pm[n % 4]
                    pk = "PS:pm%d" % (n % 4)
                    n += 1
                    for k in range(8):
                        MM(S, p[:], hT[:, k, a * 128:(a + 1) * 128], W[:, k, cg * 512:(cg + 1) * 512], k == 0, k == 7,
                           ["W", hk[k]], [pk])
                    ACT(S, z_st[b][:, a, cg * 512:(cg + 1) * 512], p[:], AF.Silu, [pk], ["zst%d" % b])
            DMA(S, "sp", SZG[s_ * 512:(s_ + 1) * 512, hz * 3072:(hz + 1) * 3072].rearrange("(a p) c -> p a c", p=128),
                z_st[b][:], reads=["zst%d" % b])
        S.barrier()
        S.emit()


def pass_ATT(nc, S, C, T, xsrc, xdst, lw, N, QT, GT, SZG, KST, KWT, VS, VW, KCT, VC):
    NQT = T // 128
    NC = T // 16 - 1
    with ExitStack() as st:
        P = alloc_post(nc, st)
        kst = [alloc(nc, st, "at_kst%d" % g, [128, T], BF16) for g in range(2)]
        vsa = [alloc(nc, st, "at_vsa%d" % g, [128, NQT, 129], BF16) for g in range(2)]
        wout = alloc(nc, st, "at_wout", [128, 16, D], BF16)
        gpost = alloc(nc, st, "at_gpost", [128, D], F32)
        kct = [alloc(nc, st, "at_kct%d" % g, [128, 512], BF16) for g in range(2)]
        rc = [alloc(nc, st, "at_rc%d" % g, [128, 4, 161], BF16) for g in range(2)]
        ab = alloc(nc, st, "at_ab", [128, 64, NH], F32)
        abc = alloc(nc, st, "at_abc", [128, 64, NH], F32)
        cm = alloc(nc, st, "at_cm", [128, 17, 128], BF16)
        cdiag = alloc(nc, st, "at_cdiag", [128, 128], BF16)
        cwin = alloc(nc, st, "at_cwin", [128, 128], BF16)
        vm = alloc(nc, st, "at_vm", [128, 256], F32)
        fb = alloc(nc, st, "at_fb", [128, 256], F32)
        mk_all = alloc(nc, st, "at_mkall", [128, NQT, 128], BF16)
        qt_sb = alloc(nc, st, "at_q", [128, NH, 128], BF16)
        szg = alloc(nc, st, "at_szg", [128, ZW], BF16)
        gts = alloc(nc, st, "at_gt", [128, 48], F32)
        xin = alloc(nc, st, "at_xin", [128, D], F32)
        xout = xin
        pT = [alloc(nc, st, "at_pT%d" % i, [128, 8, 128], BF16) for i in range(3)]
        kwin = [alloc(nc, st, "at_kwin%d" % i, [128, 640], BF16) for i in range(2)]
        vwin = [alloc(nc, st, "at_vwin%d" % i, [128, 5, 129], BF16) for i in range(2)]
        ocmp = alloc(nc, st, "at_ocmp", [128, 8, 128], F32)
        impu = alloc(nc, st, "at_impu", [128, 8, 4, 33], F32)
        impn = alloc(nc, st, "at_impn", [128, 4, 33], F32)
        imp = alloc(nc, st, "at_imp", [128, 160], F32)
        impm = alloc(nc, st, "at_impm", [128, 128], F32)
        imp2 = alloc(nc, st, "at_imp2", [128, 128], F32)
        m8 = alloc(nc, st, "at_m8", [128, 16], F32)
        thr = alloc(nc, st, "at_thr", [128, 1], F32)
        mask = alloc(nc, st, "at_mask", [128, 128], BF16)
        mrep = alloc(nc, st, "at_mrep", [128, 8, 64], BF16)
        zc = alloc(nc, st, "at_zc", [128, 8], F32)
        coef = alloc(nc, st, "at_coef", [128, 8], F32)
        osum = alloc(nc, st, "at_osum", [128, 8, 128], F32)
        otmp = alloc(nc, st, "at_otmp", [128, 8, 128], F32)
        oall = alloc(nc, st, "at_oall", [128, 2 * 1024], BF16)
        oT = alloc(nc, st, "at_oT", [128, 16, 128], BF16)
        sT = [palloc(nc, st, "at_sT%d" % i, [128, 1024], F32) for i in range(2)]
        oa = [palloc(nc, st, "at_oa%d" % i, [128, 512], F32) for i in range(3)]
        pmisc = palloc(nc, st, "at_misc", [128, 1024], BF16)

        def oav(hd, w):
            return oa[hd // 3][:, (hd % 3) * 161:(hd % 3) * 161 + w]

        def oak(hd):
            return "PS:oa%d" % (hd // 3)

        for g in range(2):
            DMA(S, "sp", kst[g][:], KST[g], writes=["kst%d" % g])
            S.op("pool", lambda e, g=g: e.memset(vsa[g][:, :, 128:129], 1.0), [], ["vsa1_%d" % g])
            for t4 in range(0, NQT, 16):
                t5 = min(t4 + 16, NQT)
                DMA(S, "sp", vsa[g][:, t4:t5, 0:128], VS[g][t4 * 128:t5 * 128, :].rearrange("(t p) d -> p t d", p=128),
                    writes=["vsa%d" % g])
            DMA(S, "sp", kct[g][:], KCT[g], writes=["kct%d" % g])
            DMA(S, "sp", rc[g][:, :, 0:128], VC[g].rearrange("(t p) d -> p t d", p=128), writes=["rc%d" % g])
            for nt in range(4):
                DMA(S, "pool", rc[g][:, nt, 128:161], N["wov"], writes=["rcw%d" % g])
        for i_ in range(2):
            S.op("pool", lambda e, i_=i_: e.memset(vwin[i_][:, :, 128:129], 1.0), [], ["vwin1_%d" % i_])
        DMA(S, "pool", wout[:], lw["w_out"].rearrange("(j p) c -> p j c", p=128), writes=["wout"])
        DMA(S, "sp", gpost[:], lw["gpost"], writes=["gpost"])
        DMA(S, "sp", ab[:], N["ab"], writes=["ab"])
        DMA(S, "sp", abc[:], N["abc"], writes=["abc"])
        DMA(S, "pool", cm[:], N["cm"], writes=["cm"])
        DMA(S, "pool", cdiag[:], N["cdiag"], writes=["cdiag"])
        DMA(S, "pool", cwin[:], N["cwin"], writes=["cwin"])
        DMA(S, "sp", vm[:], N["vm"], writes=["vm"])
        DMA(S, "sp", fb[:], N["fb"], writes=["fb"])
        consts_k = ["ab", "abc", "cm", "cdiag", "cwin"]
        st_ctr = [0]
        pt_ctr = [0]

        pend_pv = []

        def flush_pv():
            while pend_pv:
                pend_pv.pop(0)()

        def att_step(g, lhsT_k, kkeys, bias_tab, mask_ap, mkeys, rhs_v, vkeys, ncol, first, last):
            si = st_ctr[0] % 2
            st_ctr[0] += 1
            pi = pt_ctr[0] % 3
            pt_ctr[0] += 1
            sk = "PS:sT%d" % si
            for hf in range(2):
                MM(S, sT[si][:, hf * 512:(hf + 1) * 512], lhsT_k,
                   qt_sb[:, g * 8 + hf * 4:g * 8 + hf * 4 + 4, :].rearrange("p h t -> p (h t)"), True, True,
                   kkeys + ["qt"], [sk], inc=(hf == 1))
            for hf in range(2):
                ACT(S, pT[pi][:, hf * 4:(hf + 1) * 4, :], sT[si][:, hf * 512:(hf + 1) * 512].rearrange("p (h t) -> p h t", h=4),
                    AF.Exp, [sk], ["pT%d" % pi])
            TT(S, "dve", pT[pi][:], pT[pi][:], bias_tab[:, g * 8:(g + 1) * 8].unsqueeze(2).to_broadcast([128, 8, 128]),
               ALU.mult, ["pT%d" % pi] + consts_k, ["pT%d" % pi])
            if mask_ap is not None:
                TT(S, "dve", pT[pi][:], pT[pi][:], mask_ap.unsqueeze(1).to_broadcast([128, 8, 128]), ALU.mult,
                   ["pT%d" % pi] + mkeys, ["pT%d" % pi])
            def pv():
                for hh in range(8):
                    first_in_bank = (hh % 3 == 0)
                    S.op("pe", lambda e, hh=hh, fib=first_in_bank: e.matmul(
                        oav(hh, ncol), lhsT=pT[pi][:, hh, :], rhs=rhs_v, start=(first and fib), stop=last,
                        skip_group_check=True), ["pT%d" % pi] + vkeys, [oak(hh)], inc=(hh == 7 or hh % 3 == 2))
            flush_pv()
            pend_pv.append(pv)

        def combine(g, br, src_is_psum, accumulate):
            if src_is_psum:
                for bk in range(3):
                    nh_ = 3 if bk < 2 else 2
                    CP(S, "dve", zc[:, bk * 3:bk * 3 + nh_],
                       oa[bk][:, 0:nh_ * 161].rearrange("p (h c) -> p h c", c=161)[:, :, 128], ["PS:oa%d" % bk], ["zc"])
            TS(S, "dve", zc[:], zc[:], 1e-30, None, ALU.max, None, ["zc"], ["zc"])
            S.op("dve", lambda e: e.reciprocal(out=zc[:], in_=zc[:]), ["zc"], ["zc"])
            gsl = gts[:, (br * 2 + g) * 8:(br * 2 + g) * 8 + 8]
            TT(S, "dve", coef[:], zc[:], gsl, ALU.mult, ["zc", "gts"], ["coef"])
            dst = otmp if accumulate else osum
            dk = "otmp" if accumulate else "osum"
            for hh in range(8):
                src = oav(hh, 128) if src_is_psum else ocmp[:, hh, :]
                sk_ = oak(hh) if src_is_psum else "ocmp"
                zo = ((br * 2 + g) * 8 + hh) * 128
                STT(S, "dve", dst[:, hh, :], src, coef[:, hh:hh + 1], szg[:, zo:zo + 128], ALU.mult, ALU.mult,
                    [sk_, "coef", "szg"], [dk])
            if accumulate:
                TT(S, "pool", osum[:], osum[:], otmp[:], ALU.add, ["osum", "otmp"], ["osum"])

        for qt in range(NQT):
            t0 = qt * 128
            DMA(S, "sp", qt_sb[:].rearrange("p h t -> p (h t)"), QT[qt], writes=["qt"])
            DMA(S, "sp", szg[:], SZG[t0:t0 + 128, :], writes=["szg"])
            DMA(S, "sp", gts[:], GT[t0:t0 + 128, :], writes=["gts"])
            DMA(S, "sp", xin[:], xsrc[t0:t0 + 128, :], writes=["xin"])
            for g in range(2):
                wb = (qt * 2 + g) % 2
                k0 = max(qt - 4, 0)
                nwt = qt - k0 + 1
                DMA(S, "sp", kwin[wb][:, 0:nwt * 128], KWT[g][:, k0 * 128:(qt + 1) * 128], writes=["kwin%d" % wb])
                DMA(S, "sp", vwin[wb][:, 0:nwt, 0:128], VW[g][k0 * 128:(qt + 1) * 128, :].rearrange("(t p) d -> p t d", p=128),
                    writes=["vwin%d" % wb])
                nts = [nt for nt in range(4) if 16 * nt <= qt and nt * 128 < NC]
                S.op("pool", lambda e: e.memset(impu[:], 0.0), [], ["impu"])
                S.op("pool", lambda e: e.memset(ocmp[:], 0.0), [], ["ocmp"])
                for nt in nts:
                    relc = 16 * nt - qt
                    m_ap = cm[:, relc + 16, :] if relc >= -16 else None
                    att_step(g, kct[g][:, nt * 128:(nt + 1) * 128], ["kct%d" % g], abc[:, relc + 63, :], m_ap, ["cm"],
                             rc[g][:, nt, :], ["rc%d" % g, "rcw%d" % g], 161, True, True)
                    flush_pv()
                    for bk in range(3):
                        nh_ = 3 if bk < 2 else 2
                        v3 = oa[bk][:, 0:nh_ * 161].rearrange("p (h c) -> p h c", c=161)
                        TT(S, "dve", ocmp[:, bk * 3:bk * 3 + nh_, :], ocmp[:, bk * 3:bk * 3 + nh_, :], v3[:, :, 0:128], ALU.add,
                           ["ocmp", "PS:oa%d" % bk], ["ocmp"])
                        CP(S, "dve", impu[:, bk * 3:bk * 3 + nh_, nt, :], v3[:, :, 128:161], ["PS:oa%d" % bk], ["impu"])
                S.op("dve", lambda e: e.reduce_sum(out=zc[:], in_=impu[:].rearrange("p h n j -> p h (n j)"),
                                                   axis=mybir.AxisListType.X), ["impu"], ["zc"])
                TS(S, "dve", zc[:], zc[:], 0.5, None, ALU.mult, None, ["zc"], ["zc"])
                combine(g, 0, False, False)
                S.op("pool", lambda e: e.memset(impn[:], 0.0), [], ["impn"])
                for hh in range(8):
                    STT(S, "dve", impn[:], impu[:, hh, :, :], zc[:, hh:hh + 1], impn[:], ALU.mult, ALU.add,
                        ["impu", "zc", "impn"], ["impn"])
                S.op("pool", lambda e: e.memset(imp[:], 0.0), [], ["imp"])
                for nt in nts:
                    TT(S, "dve", imp[:, 32 * nt:32 * nt + 33], imp[:, 32 * nt:32 * nt + 33], impn[:, nt, :], ALU.add,
                       ["imp", "impn"], ["imp"])
                c0 = 127 - 2 * qt
                TT(S, "dve", impm[:], imp[:, 0:128], vm[:, c0:c0 + 128], ALU.mult, ["imp", "vm"], ["impm"])
                TT(S, "dve", impm[:], impm[:], fb[:, c0:c0 + 128], ALU.add, ["impm", "fb"], ["impm"])
                S.op("dve", lambda e: e.memset(impm[:, 0:1], BIG), [], ["impm"])
                S.op("dve", lambda e: e.max(out=m8[:, 0:8], in_=impm[:]), ["impm"], ["m8"])
                S.op("dve", lambda e: e.match_replace(out=imp2[:], in_to_replace=m8[:, 0:8], in_values=impm[:],
                                                      imm_value=-3.0e38), ["impm", "m8"], ["imp2"])
                S.op("dve", lambda e: e.max(out=m8[:, 8:16], in_=imp2[:]), ["imp2"], ["m8"])
                S.op("dve", lambda e: e.tensor_reduce(out=thr[:], in_=m8[:, 8:16], axis=mybir.AxisListType.X, op=ALU.min),
                     ["m8"], ["thr"])
                TS(S, "dve", mask[:], impm[:], thr[:, 0:1], None, ALU.is_ge, None, ["impm", "thr"], ["mask"])
                kb_lo = (max(0, qt - 16) // 4) * 4 if g == 0 else 0
                for kb in range(kb_lo, qt, 4):
                    nk = min(4, qt - kb)
                    CP(S, "dve", mrep[:, 0:2 * nk, :], mask[:, 2 * kb:2 * kb + 2 * nk].unsqueeze(2).to_broadcast([128, 2 * nk, 64]),
                       ["mask"], ["mrep"])
                    si = st_ctr[0] % 2
                    st_ctr[0] += 1
                    for s4 in range(nk):
                        MM(S, sT[si][:, s4 * 128:(s4 + 1) * 128], mrep[:, 2 * s4:2 * s4 + 2, :].rearrange("p a b -> p (a b)"),
                           C.ident[:], True, True, ["mrep"], ["PS:sT%d" % si], inc=(s4 == nk - 1))
                    CP(S, "act", mk_all[:, kb:kb + nk, :], sT[si][:, 0:nk * 128].rearrange("p (k i) -> p k i", i=128),
                       ["PS:sT%d" % si], ["mk_all"])
                kt_lo = max(0, qt - 16) if g == 0 else 0
                for kt in range(kt_lo, qt + 1):
                    m_ap = cdiag[:] if kt == qt else mk_all[:, kt, :]
                    att_step(g, kst[g][:, kt * 128:(kt + 1) * 128], ["kst%d" % g], ab[:, kt - qt + 63, :], m_ap,
                             ["cdiag", "mk_all"], vsa[g][:, kt, :], ["vsa%d" % g, "vsa1_%d" % g], 129, kt == kt_lo, kt == qt)
                flush_pv()
                combine(g, 1, True, True)
                for wi in range(nwt):
                    kt = k0 + wi
                    if kt == qt:
                        m_ap = cdiag[:]
                    elif kt == qt - 4:
                        m_ap = cwin[:]
                    else:
                        m_ap = None
                    att_step(g, kwin[wb][:, wi * 128:(wi + 1) * 128], ["kwin%d" % wb], ab[:, kt - qt + 63, :], m_ap,
                             ["cdiag", "cwin"], vwin[wb][:, wi, :], ["vwin%d" % wb, "vwin1_%d" % wb], 129, wi == 0, wi == nwt - 1)
                flush_pv()
                combine(g, 2, True, True)
                CP(S, "act", oall[:, g * 1024:(g + 1) * 1024], osum[:].rearrange("p h d -> p (h d)"), ["osum"], ["oall%d" % g])
            for half in range(2):
                for jj in range(8):
                    j = half * 8 + jj
                    TR(S, pmisc[:, jj * 128:(jj + 1) * 128], oall[:, j * 128:(j + 1) * 128], C.ident[:], ["oall%d" % (j // 8)],
                       ["PS:misc"], inc=(jj == 7))
                CP(S, "dve", oT[:, half * 8:(half + 1) * 8, :], pmisc[:].rearrange("p (j t) -> p j t", t=128), ["PS:misc"],
                   ["oT%d" % half])
            si = st_ctr[0] % 2
            st_ctr[0] += 1
            py = [sT[si][:, 0:512], sT[si][:, 512:1024]]
            for hh in range(2):
                for j in range(16):
                    MM(S, py[hh], oT[:, j, :], wout[:, j, hh * 512:(hh + 1) * 512], j == 0, j == 15,
                       ["oT%d" % (j // 8), "wout"], ["PS:sT%d" % si])
            post_residual(S, P, py, ["PS:sT%d" % si] * 2, xin, "xin", gpost, xout, "xin")
            DMA(S, "sp", xdst[t0:t0 + 128, :], xout[:], reads=["xin"])
        S.barrier()
        S.emit()
```
